# Optimizing a Trainium2 kernel written in Bass

```python
import math
import jax, jax.numpy as jnp
from jax import lax
import numpy as np

D_MODEL = 2048
BATCH = 8
SEQ = 4096
DEPTH = 1

N_HEADS = 8
HEAD_DIM = 128
N_KV_HEADS = 2
ATTN_WIDTH = N_HEADS * HEAD_DIM
KV_WIDTH = N_KV_HEADS * HEAD_DIM
IDX_HEADS = 16
IDX_DIM = 64
TOPK_MAX = 256
Q_BLOCK = 128
N_BUCKETS = 32
MAX_DISTANCE = 128
POOL_WINDOWS = (2, 4, 8, 16)
N_POOL_GROUPS = 4
POOL_WIDTH = 1024
POOL_GROUP = POOL_WIDTH // N_POOL_GROUPS
PEER_HEADS = 8
PEER_KEYS = 128
PEER_QDIM = 256
PEER_TOPK = 16
N_EXPERTS = PEER_KEYS * PEER_KEYS
PEER_CHUNK = 128
EPS = 1e-6
NEG = -1e30
SPLITS = (ATTN_WIDTH, KV_WIDTH, KV_WIDTH, IDX_HEADS * IDX_DIM, IDX_DIM, IDX_HEADS, POOL_WIDTH, D_MODEL, D_MODEL)
IN_WIDTH = sum(SPLITS)

kernel_name = 'hybrid_dsa_pool_peer_block'


def rms_norm(x, g):
    xf = x.astype(jnp.float32)
    y = xf * lax.rsqrt(jnp.mean(xf * xf, axis=-1, keepdims=True) + EPS)
    return (y * g.astype(jnp.float32)).astype(x.dtype)


def t5_bucket(dist):
    max_exact = N_BUCKETS // 2
    n = jnp.maximum(dist, 0)
    nf = jnp.maximum(n, 1).astype(jnp.float32)
    large = max_exact + (jnp.log(nf / max_exact) / math.log(MAX_DISTANCE / max_exact)
                         * (N_BUCKETS - max_exact)).astype(jnp.int32)
    large = jnp.minimum(large, N_BUCKETS - 1)
    return jnp.where(n < max_exact, n, large)


def dsa_sparse_attention(q, k, v, iq, ik, iw, rel_bias):
    B, S = q.shape[0], q.shape[1]
    k_sel = min(TOPK_MAX, S // 4)
    n_blocks = S // Q_BLOCK
    rep = N_HEADS // N_KV_HEADS
    key_pos = jnp.arange(S)
    scale = HEAD_DIM ** -0.5
    idx_scale = IDX_DIM ** -0.5
    w_scale = IDX_HEADS ** -0.5

    def block(i):
        q0 = i * Q_BLOCK
        qb = lax.dynamic_slice_in_dim(q, q0, Q_BLOCK, axis=1)
        iqb = lax.dynamic_slice_in_dim(iq, q0, Q_BLOCK, axis=1)
        iwb = lax.dynamic_slice_in_dim(iw, q0, Q_BLOCK, axis=1).astype(jnp.float32)
        qpos = q0 + jnp.arange(Q_BLOCK)
        dots = jnp.einsum('bqhd,bsd->bqhs', iqb, ik, preferred_element_type=jnp.float32) * idx_scale
        scores = jnp.einsum('bqh,bqhs->bqs', iwb * w_scale, jax.nn.relu(dots))
        causal = key_pos[None, :] <= qpos[:, None]
        scores = jnp.where(causal[None], scores, -jnp.inf)
        _, sel = lax.top_k(scores, k_sel)
        valid = sel <= qpos[None, :, None]
        kg = jax.vmap(lambda kb, ib: kb[ib])(k, sel)
        vg = jax.vmap(lambda vb, ib: vb[ib])(v, sel)
        qg = qb.reshape(B, Q_BLOCK, N_KV_HEADS, rep, HEAD_DIM)
        logits = jnp.einsum('bqgrd,bqkgd->bqgrk', qg, kg, preferred_element_type=jnp.float32) * scale
        bias = rel_bias[t5_bucket(qpos[None, :, None] - sel)].astype(jnp.float32)
        bias = bias.reshape(B, Q_BLOCK, k_sel, N_KV_HEADS, rep).transpose(0, 1, 3, 4, 2)
        logits = jnp.where(valid[:, :, None, None, :], logits + bias, NEG)
        p = jax.nn.softmax(logits, axis=-1).astype(v.dtype)
        o = jnp.einsum('bqgrk,bqkgd->bqgrd', p, vg)
        return o.reshape(B, Q_BLOCK, ATTN_WIDTH)

    out = lax.map(block, jnp.arange(n_blocks))
    return out.transpose(1, 0, 2, 3).reshape(B, S, ATTN_WIDTH)


def causal_multiscale_pool(p):
    B, S = p.shape[0], p.shape[1]
    pf = p.astype(jnp.float32).reshape(B, S, N_POOL_GROUPS, POOL_GROUP)
    cs = jnp.concatenate([jnp.zeros_like(pf[:, :1]), jnp.cumsum(pf, axis=1)], axis=1)
    t = jnp.arange(S)
    groups = []
    for g, w in enumerate(POOL_WINDOWS):
        start = jnp.maximum(t + 1 - w, 0)
        count = (t + 1 - start).astype(jnp.float32)[None, :, None]
        window_sum = cs[:, t + 1, g] - cs[:, start, g]
        groups.append(window_sum / count - pf[:, :, g])
    return jnp.stack(groups, axis=2)


def token_mixer(h, w_in, g_q, g_k, rel_bias, pool_w, pool_scale, w_attn_br, w_pool_br, w_out):
    B, S = h.shape[0], h.shape[1]
    proj = jnp.einsum('bsd,de->bse', h, w_in)
    offsets = np.cumsum(SPLITS)[:-1].tolist()
    q, k, v, iq, ik, iw, pl, ga, gp = jnp.split(proj, offsets, axis=-1)
    q = rms_norm(q.reshape(B, S, N_HEADS, HEAD_DIM), g_q)
    k = rms_norm(k.reshape(B, S, N_KV_HEADS, HEAD_DIM), g_k)
    v = v.reshape(B, S, N_KV_HEADS, HEAD_DIM)
    iq = iq.reshape(B, S, IDX_HEADS, IDX_DIM)
    attn = dsa_sparse_attention(q, k, v, iq, ik, iw, rel_bias)
    y_attn = jnp.einsum('bse,ed->bsd', attn, w_attn_br)
    pooled = causal_multiscale_pool(pl)
    pooled = jnp.einsum('bsgc,gce->bsge', pooled, pool_w.astype(jnp.float32)).reshape(B, S, POOL_WIDTH)
    pooled = (pooled * pool_scale.astype(jnp.float32)).astype(h.dtype)
    y_pool = jnp.einsum('bse,ed->bsd', pooled, w_pool_br)
    merged = jax.nn.sigmoid(ga) * y_attn + jax.nn.sigmoid(gp) * y_pool
    return jnp.einsum('bsd,de->bse', merged, w_out)


def peer_channel_mixer(h, w_peer_q, peer_keys, peer_u, peer_v):
    B, S, D = h.shape
    q = jnp.einsum('bsd,de->bse', h, w_peer_q).reshape(B, S, PEER_HEADS, 2, PEER_QDIM // 2)
    sub = jnp.einsum('bshpd,hpnd->bshpn', q, peer_keys, preferred_element_type=jnp.float32)
    s_top, i_top = lax.top_k(sub, PEER_TOPK)
    cand = s_top[..., 0, :, None] + s_top[..., 1, None, :]
    cand_idx = i_top[..., 0, :, None] * PEER_KEYS + i_top[..., 1, None, :]
    cand = cand.reshape(B, S, PEER_HEADS, PEER_TOPK * PEER_TOPK)
    cand_idx = cand_idx.reshape(B, S, PEER_HEADS, PEER_TOPK * PEER_TOPK)
    best, pos = lax.top_k(cand, PEER_TOPK)
    experts = jnp.take_along_axis(cand_idx, pos, axis=-1)
    gates = jax.nn.softmax(best, axis=-1)
    n_tok = B * S
    n_chunks = n_tok // PEER_CHUNK
    e_per_tok = PEER_HEADS * PEER_TOPK
    hf = h.reshape(n_chunks, PEER_CHUNK, D)
    ef = experts.reshape(n_chunks, PEER_CHUNK, e_per_tok)
    gf = gates.reshape(n_chunks, PEER_CHUNK, e_per_tok)

    def chunk(args):
        hc, ec, gc = args
        u = peer_u[ec]
        a = jax.nn.gelu(jnp.einsum('cd,ced->ce', hc, u, preferred_element_type=jnp.float32), approximate=False)
        vv = peer_v[ec]
        return jnp.einsum('ce,ced->cd', (a * gc).astype(hc.dtype), vv)

    out = lax.map(chunk, (hf, ef, gf))
    return out.reshape(B, S, D)


def setup_inputs(seed: int = 0) -> dict:
    key = jax.random.key(seed)
    ks = jax.random.split(key, 20)
    f32 = jnp.float32
    nrm = lambda k, shape, s: jax.random.normal(k, shape, f32) * s
    return {
        'x': nrm(ks[0], (BATCH, SEQ, D_MODEL), 1.0),
        'c': nrm(ks[1], (BATCH, D_MODEL), 1.0),
        'w_ada': nrm(ks[2], (DEPTH, D_MODEL, 6 * D_MODEL), 0.5 * D_MODEL ** -0.5),
        'b_ada': nrm(ks[3], (DEPTH, 6 * D_MODEL), 0.01),
        'g_norm1': 1.0 + nrm(ks[4], (DEPTH, D_MODEL), 0.05),
        'g_norm2': 1.0 + nrm(ks[5], (DEPTH, D_MODEL), 0.05),
        'w_in': nrm(ks[6], (DEPTH, D_MODEL, IN_WIDTH), D_MODEL ** -0.5),
        'g_q': 1.0 + nrm(ks[7], (DEPTH, HEAD_DIM), 0.05),
        'g_k': 1.0 + nrm(ks[8], (DEPTH, HEAD_DIM), 0.05),
        'rel_bias': nrm(ks[9], (N_BUCKETS, N_HEADS), 0.5),
        'pool_w': nrm(ks[10], (DEPTH, N_POOL_GROUPS, POOL_GROUP, POOL_GROUP), POOL_GROUP ** -0.5),
        'pool_scale': 1.0 + nrm(ks[11], (DEPTH, POOL_WIDTH), 0.05),
        'w_attn_br': nrm(ks[12], (DEPTH, ATTN_WIDTH, D_MODEL), ATTN_WIDTH ** -0.5),
        'w_pool_br': nrm(ks[13], (DEPTH, POOL_WIDTH, D_MODEL), POOL_WIDTH ** -0.5),
        'w_out': nrm(ks[14], (DEPTH, D_MODEL, D_MODEL), D_MODEL ** -0.5),
        'w_peer_q': nrm(ks[15], (DEPTH, D_MODEL, PEER_HEADS * PEER_QDIM), D_MODEL ** -0.5),
        'peer_keys': nrm(ks[16], (DEPTH, PEER_HEADS, 2, PEER_KEYS, PEER_QDIM // 2), (PEER_QDIM // 2) ** -0.5),
        'peer_u': nrm(ks[17], (DEPTH, N_EXPERTS, D_MODEL), D_MODEL ** -0.5),
        'peer_v': nrm(ks[18], (DEPTH, N_EXPERTS, D_MODEL), PEER_HEADS ** -0.5),
    }


def reference(x, c, w_ada, b_ada, g_norm1, g_norm2, w_in, g_q, g_k, rel_bias, pool_w, pool_scale,
              w_attn_br, w_pool_br, w_out, w_peer_q, peer_keys, peer_u, peer_v):
    c_act = jax.nn.silu(c)
    for l in range(DEPTH):
        mod = jnp.einsum('bd,de->be', c_act, w_ada[l]) + b_ada[l]
        sh1, sc1, gt1, sh2, sc2, gt2 = [m[:, None, :] for m in jnp.split(mod, 6, axis=-1)]
        h = rms_norm(x, g_norm1[l]) * (1.0 + sc1) + sh1
        x = x + gt1 * token_mixer(h, w_in[l], g_q[l], g_k[l], rel_bias, pool_w[l], pool_scale[l],
                                  w_attn_br[l], w_pool_br[l], w_out[l])
        h = rms_norm(x, g_norm2[l]) * (1.0 + sc2) + sh2
        x = x + gt2 * peer_channel_mixer(h, w_peer_q[l], peer_keys[l], peer_u[l], peer_v[l])
    return x
```

```python
import math
from contextlib import ExitStack
import numpy as np
import ml_dtypes
import concourse.bass as bass
import concourse.mybir as mybir
from concourse.bass_utils import run_bass_kernel_spmd

F32 = mybir.dt.float32; BF16 = mybir.dt.bfloat16; I32 = mybir.dt.int32; U32 = mybir.dt.uint32
ALU = mybir.AluOpType; AF = mybir.ActivationFunctionType; AX = mybir.AxisListType

D = 2048; S = 4096; T = 256; NSUB = T // 128; NTILES = S // T
INW = 7760
C_Q, C_K, C_V, C_IQ, C_IK, C_IW, C_PL, C_GA, C_GP = 0, 1024, 1280, 1536, 2560, 2624, 2640, 3664, 5712
CH_Q = 0; CH_K = 8; CH_V = 10; CH_IQ = 12; CH_IK = 20; CH_IW = 21; CH_PL = 22; CH_GA = 30; CH_GP = 46
CH_AB = 62; CH_PB = 70; CH_PW = 78; CH_WO = 79; CH_PQ = 95; CH_KEYS = 111; NCH = 112
EPS = 1e-6
EPOCH = 20000
NDS = 8
NEGBIG = -3.0e38


class Buf:
    __slots__ = ("name", "lw", "rd")

    def __init__(self, name):
        self.name = name; self.lw = None; self.rd = {}


class Eng:
    def __init__(self, idx, name, h):
        self.idx = idx; self.name = name; self.h = h; self.seq = 0; self.sems = []; self.waited = {}
        self.dcount = 0; self.dvals = [0] * NDS; self.dsems = None


class Sched:
    def __init__(self, nc, stack):
        self.nc = nc; self.stack = stack; self.E = {}
        for i, (n, h) in enumerate([("pe", nc.tensor), ("act", nc.scalar), ("dve", nc.vector),
                                    ("pool", nc.gpsimd), ("sp", nc.sync)]):
            self.E[n] = Eng(i, n, h)
        self.elist = list(self.E.values()); self.dsem_list = []

    def _esem(self, E, ep):
        while len(E.sems) <= ep:
            E.sems.append(self.stack.enter_context(self.nc.semaphore(f"e_{E.name}_{len(E.sems)}")))
        return E.sems[ep]

    def _wait(self, E, ev):
        kind, k, v = ev
        key = (kind, k)
        if E.waited.get(key, 0) >= v:
            return
        E.waited[key] = v
        if kind == "e":
            E2 = self.elist[k]; ep, val = divmod(v - 1, EPOCH)
            E.h.wait_ge(self._esem(E2, ep), val + 1)
        else:
            E.h.wait_ge(self.dsem_list[k], v)

    def op(self, en, fn, reads=(), writes=()):
        E = self.E[en]; deps = []
        for r in reads:
            if r.lw is not None and not (en == "pe" and r.lw[0] == "e" and r.lw[1] == E.idx):
                deps.append(r.lw)
        pe = en == "pe"
        for w in writes:
            if w.lw is not None and not (pe and w.lw[0] == "e" and w.lw[1] == E.idx):
                deps.append(w.lw)
            for ev in w.rd.values():
                if not (pe and ev[0] == "e" and ev[1] == E.idx):
                    deps.append(ev)
        for ev in deps:
            self._wait(E, ev)
        inst = fn(E.h)
        E.seq += 1; ep, _ = divmod(E.seq - 1, EPOCH)
        inst.then_inc(self._esem(E, ep), 1)
        ev = ("e", E.idx, E.seq)
        for r in reads:
            r.rd[("e", E.idx)] = ev
        for w in writes:
            w.lw = ev; w.rd = {}
        return inst

    def dma(self, qn, out, in_, reads=(), writes=(), indirect=None, **kw):
        Q = self.E[qn]
        if Q.dsems is None:
            Q.dsems = []
            for i in range(NDS):
                sem = self.stack.enter_context(self.nc.semaphore(f"d_{qn}_{i}"))
                Q.dsems.append(len(self.dsem_list)); self.dsem_list.append(sem)
        slot = Q.dcount % NDS; Q.dcount += 1
        k = Q.dsems[slot]; pv = Q.dvals[slot]
        if pv > 0:
            self._wait(Q, ("d", k, pv))
        deps = []
        for r in reads:
            if r.lw is not None:
                deps.append(r.lw)
        for w in writes:
            if w.lw is not None:
                deps.append(w.lw)
            deps.extend(w.rd.values())
        for ev in deps:
            self._wait(Q, ev)
        if indirect is not None:
            inst = Q.h.indirect_dma_start(out=out, out_offset=None, in_=in_, in_offset=indirect)
        else:
            inst = Q.h.dma_start(out=out, in_=in_, **kw)
        nv = pv + 16; Q.dvals[slot] = nv
        inst.then_inc(self.dsem_list[k], 16)
        ev = ("d", k, nv)
        for r in reads:
            r.rd[("d", k)] = ev
        for w in writes:
            w.lw = ev; w.rd = {}

    def barrier(self):
        evs = [("e", E.idx, E.seq) for E in self.elist if E.seq > 0]
        for Q in self.elist:
            if Q.dsems is not None:
                for slot in range(NDS):
                    if Q.dvals[slot] > 0:
                        evs.append(("d", Q.dsems[slot], Q.dvals[slot]))
        for E in self.elist:
            for ev in evs:
                if not (ev[0] == "e" and ev[1] == E.idx):
                    self._wait(E, ev)

    def drain_dmas(self, en="sp"):
        E = self.E[en]
        for Q in self.elist:
            if Q.dsems is not None:
                for slot in range(NDS):
                    if Q.dvals[slot] > 0:
                        self._wait(E, ("d", Q.dsems[slot], Q.dvals[slot]))


def t5_bucket_np(n):
    n = np.asarray(n)
    nf = np.maximum(n, 1).astype(np.float32)
    large = 16 + (np.log(nf / np.float32(16)) / np.float32(math.log(8.0)) * np.float32(16)).astype(np.int32)
    large = np.minimum(large, 31)
    return np.where(n < 16, n, large)


def host_consts():
    c = {}
    eye = np.eye(128, dtype=np.float32)
    c["identb"] = eye.astype(ml_dtypes.bfloat16)
    c["identbig"] = (eye * 32768.0).astype(ml_dtypes.bfloat16)
    c["identf"] = eye
    c["antif"] = np.ascontiguousarray(eye[::-1])
    q = np.arange(128)[:, None]; s = np.arange(128)[None, :]
    c["cmask"] = np.where(s <= q, 0.0, -1.0e30).astype(np.float32)
    c["onesb"] = np.ones((128, 128), dtype=ml_dtypes.bfloat16)
    oh = np.zeros((33, 384), dtype=np.float32)
    for j in range(383):
        dist = j - 127
        if dist >= 0:
            oh[int(t5_bucket_np(dist)), j] = 1.0
        else:
            oh[32, j] = -30000.0
    c["oh2"] = oh
    c["iota16"] = np.tile(np.arange(16, dtype=np.float32)[None, :], (128, 1))
    invc = np.zeros((128, 4, 16), dtype=np.float32)
    for g, w in enumerate((2, 4, 8, 16)):
        for t in range(16):
            invc[:, g, t] = 1.0 / min(t + 1, w)
    c["invc"] = invc
    c["pow2"] = np.tile((2.0 ** -(np.arange(32, dtype=np.float64) + 1)).astype(np.float32)[None, :], (128, 1))
    return c


CONST_SPECS = [("identb", [128, 128], BF16), ("identbig", [128, 128], BF16), ("identf", [128, 128], F32),
               ("antif", [128, 128], F32), ("cmask", [128, 128], F32), ("onesb", [128, 128], BF16),
               ("oh2", [33, 384], F32), ("iota16", [128, 16], F32), ("invc", [128, 4, 16], F32), ("pow2", [128, 32], F32)]


def build(NT=NTILES, dbg=None, do_mix=True, do_peer=True, tile_list=None):
    dbg = dbg or {}
    nc = bass.Bass("TRN2", target_bir_lowering=False)
    stack = ExitStack()
    dt = lambda name, shape, dtype, kind="ExternalInput": nc.dram_tensor(name, shape, dtype, kind=kind)
    x_h = dt("x", [S, D], F32); cT_h = dt("cT", [128, 16], F32)
    wada_h = dt("w_ada", [D, 6 * D], F32); bada_h = dt("b_ada", [1, 6 * D], F32)
    g1_h = dt("g1", [1, D], F32); g2_h = dt("g2", [1, D], F32)
    win_h = dt("w_in", [D, INW], F32); gq_h = dt("gq", [128, 1], F32); gk_h = dt("gk", [128, 1], F32)
    relb_h = dt("rel_bias", [32, 8], F32); poolw_h = dt("pool_w", [4, 256, 256], F32)
    pscale_h = dt("pscale", [128, 8], F32)
    wab_h = dt("w_attn_br", [1024, D], F32); wpb_h = dt("w_pool_br", [1024, D], F32)
    wout_h = dt("w_out", [D, D], F32); wpq_h = dt("w_peer_q", [D, D], F32)
    keys_h = dt("peer_keys", [16, 128, 128], F32)
    pu_h = dt("peer_u", [16384, D], F32); pv_h = dt("peer_v", [16384, D], F32)
    cst_h = {n: dt(n, sh, ty) for n, sh, ty in CONST_SPECS}
    out_h = dt("out", [S, D], F32, kind="ExternalOutput")
    wsc_h = dt("wsc", [NCH, 128, 2048], BF16, kind="Internal")
    modv_h = dt("modv", [1, 6 * D], F32, kind="Internal")
    fd_h = dt("fd", [8, 384], F32, kind="Internal")
    uv_h = dt("uvtab", [16384, 2 * D], BF16, kind="Internal")
    dbg_h = {n: dt("dbg_" + n, sh, ty, kind="ExternalOutput") for n, (sh, ty) in dbg.items()}

    sb = lambda name, shape, dtype: stack.enter_context(nc.sbuf_tensor("s_" + name, shape, dtype))
    ps = lambda name, shape, dtype: stack.enter_context(nc.psum_tensor(name, shape, dtype))
    sc = Sched(nc, stack)
    op = sc.op; dma = sc.dma
    AP = bass.AP

    kT = sb("kT", [128, 2, S], BF16); B_kT = Buf("kT")
    vaug = sb("vaug", [128, 32, 2, 130], BF16); B_v = Buf("vaug")
    ikT = sb("ikT", [128, S], BF16); B_ik = Buf("ikT")
    xres = sb("xres", [128, NSUB, D], F32); B_x = [Buf(f"x{j}") for j in range(NSUB)]
    hTM = sb("hTM", [128, NSUB, D], BF16); B_hTM = Buf("hTM")
    hT = sb("hT", [128, 16, T], BF16); B_hT = Buf("hT")
    NW = 6
    wb_all = sb("wb_all", [128, NW * 2048], BF16)
    wb = [wb_all[:, i * 2048:(i + 1) * 2048] for i in range(NW)]; B_wb = [Buf(f"wb{i}") for i in range(NW)]
    identb = sb("identb", [128, 128], BF16); identbig = sb("identbig", [128, 128], BF16)
    identf = sb("identf", [128, 128], F32); antif = sb("antif", [128, 128], F32)
    cmask = sb("cmask", [128, 128], F32); onesb = sb("onesb", [128, 128], BF16)
    iota16 = sb("iota16", [128, 16], F32); invc = sb("invc", [128, 4, 16], F32); pow2 = sb("pow2", [128, 32], F32)
    B_cst = Buf("cst")
    biasT = sb("biasT", [128, 8, 2, 2, 128], BF16); B_bias = Buf("biasT")
    gqs = sb("gqs", [128, 2], F32); B_gqs = Buf("gqs")
    pscale = sb("pscale", [128, 8], F32)
    epst = sb("epst", [128, 1], F32)
    halo = sb("halo", [128, 8, 16], F32); B_halo = Buf("halo")
    stat = sb("stat", [128, 16], F32); B_stat = Buf("stat")
    ARENA = 96 * 1024
    arena = sb("arena", [128, ARENA // 4], F32)

    def av(off, shape, dtype):
        n = int(np.prod(shape)); esz = 4 if dtype in (F32, I32, U32) else 2
        assert off % 4 == 0 and off + n * esz <= ARENA, (off, shape)
        a = arena[:, off // 4:(off + n * esz) // 4]
        if dtype != F32:
            a = a.bitcast(dtype)
        if len(shape) == 2:
            a = a.rearrange("p (a b) -> p a b", a=shape[0])
        elif len(shape) == 3:
            a = a.rearrange("p (a b c) -> p a b c", a=shape[0], b=shape[1])
        elif len(shape) == 4:
            a = a.rearrange("p (a b c d) -> p a b c d", a=shape[0], b=shape[1], c=shape[2])
        return a

    class Lay:
        def __init__(self):
            self.off = 0

        def take(self, shape, dtype, name):
            n = int(np.prod(shape)); esz = 4 if dtype in (F32, I32, U32) else 2
            v = av(self.off, shape, dtype)
            self.off += (n * esz + 31) // 32 * 32
            return v, Buf(name)

    pb = [ps(f"pb{i}", [128, 512], F32) for i in range(8)]; B_pb = [Buf(f"pb{i}") for i in range(8)]

    wslot_ctr = [0]

    def load_chunk(ch):
        i = wslot_ctr[0] % NW; wslot_ctr[0] += 1
        dma("sp", wb[i], wsc_h[ch], reads=[B_wsc[ch]], writes=[B_wb[i]])
        return wb[i], B_wb[i]

    def mm(out, lhsT, rhs, start, stop, reads, writes):
        return op("pe", lambda e: e.matmul(out, lhsT, rhs, start=start, stop=stop), reads=reads, writes=writes)

    def bcast_row(h, off, n=D, parts=128):
        return AP(tensor=h, offset=off, ap=[[0, parts], [1, n]])

    B_wsc = [Buf(f"wsc{i}") for i in range(NCH)]
    B_modv = Buf("modv"); B_fd = Buf("fd")
    B_dbg = {n: Buf("dbg_" + n) for n in dbg}

    def dump(name, src_ap, src_bufs):
        if name in dbg_h:
            dma("sp", dbg_h[name].ap(), src_ap, reads=src_bufs, writes=[B_dbg[name]])

    for n, t in [("identb", identb), ("identbig", identbig), ("identf", identf), ("antif", antif),
                 ("cmask", cmask), ("onesb", onesb), ("iota16", iota16), ("invc", invc), ("pow2", pow2)]:
        dma("sp", t[:], cst_h[n].ap(), writes=[B_cst])
    dma("sp", pscale[:, :], pscale_h.ap(), writes=[B_cst])
    dma("sp", gqs[:, 0:1], gq_h.ap(), writes=[B_gqs])
    dma("sp", gqs[:, 1:2], gk_h.ap(), writes=[B_gqs])
    op("dve", lambda e: e.memset(epst[:, :], EPS), writes=[B_cst])
    op("dve", lambda e: e.tensor_scalar(gqs[:, 0:1], gqs[:, 0:1], float(128 ** -0.5), None, ALU.mult),
       reads=[B_gqs], writes=[B_gqs])
    op("dve", lambda e: e.memset(halo[:, :, :], 0.0), writes=[B_halo])
    op("pool", lambda e: e.memset(vaug[:, :, :, 128:130], 1.0), writes=[B_v])

    def cast_store(ch, loads):
        i = wslot_ctr[0] % NW; wslot_ctr[0] += 1
        for (o, src) in loads:
            dma("pool", o(wb[i]), src, writes=[B_wb[i]])
        dma("sp", wsc_h[ch], wb[i], reads=[B_wb[i]], writes=[B_wsc[ch]])

    def wsrc(h, ncolsW, c0, nk, ncols):
        return AP(tensor=h, offset=c0, ap=[[ncolsW, 128], [128 * ncolsW, nk], [1, ncols]])

    def v3(nk, ncols, c_lo=0, c_n=None):
        c_n = ncols if c_n is None else c_n
        return lambda w: w[:, 0:nk * ncols].rearrange("p (k c) -> p k c", k=nk)[:, :, c_lo:c_lo + c_n]

    def std_chunks(ch0, h, ncolsW, c0, n):
        for i in range(n):
            cast_store(ch0 + i, [(v3(16, 128), wsrc(h, ncolsW, c0 + i * 128, 16, 128))])

    std_chunks(CH_Q, win_h, INW, C_Q, 8); std_chunks(CH_K, win_h, INW, C_K, 2)
    std_chunks(CH_V, win_h, INW, C_V, 2); std_chunks(CH_IQ, win_h, INW, C_IQ, 8)
    cast_store(CH_IK, [(v3(16, 128, 0, 64), wsrc(win_h, INW, C_IK, 16, 64)),
                       (v3(16, 128, 64, 64), wsrc(win_h, INW, C_IK, 16, 64))])
    cast_store(CH_IW, [(v3(16, 16), wsrc(win_h, INW, C_IW, 16, 16))])
    std_chunks(CH_PL, win_h, INW, C_PL, 8); std_chunks(CH_GA, win_h, INW, C_GA, 16)
    std_chunks(CH_GP, win_h, INW, C_GP, 16)
    for i in range(8):
        cast_store(CH_AB + i, [(v3(8, 256), wsrc(wab_h, D, i * 256, 8, 256))])
        cast_store(CH_PB + i, [(v3(8, 256), wsrc(wpb_h, D, i * 256, 8, 256))])
    cast_store(CH_PW, [((lambda w, g=g: w[:, g * 512:(g + 1) * 512].rearrange("p (k c) -> p k c", k=2)),
                        AP(tensor=poolw_h, offset=g * 65536, ap=[[256, 128], [32768, 2], [1, 256]]))
                       for g in range(4)])
    std_chunks(CH_WO, wout_h, D, 0, 16); std_chunks(CH_PQ, wpq_h, D, 0, 16)

    L = Lay()
    wst = []; B_wst = []
    for i in range(2):
        v, b = L.take([16, 256], BF16, f"wst{i}"); wst.append(v); B_wst.append(b)
    modrow, B_modrow = L.take([1, 256], F32, "modrow")
    oh2v_full, B_oh2 = L.take([384], F32, "oh2")
    cact, B_cact = L.take([16], F32, "cact"); cactb, B_cactb = L.take([16], BF16, "cactb")
    keysn, B_keysn = L.take([16, 128], F32, "keysn")
    rb33, B_rb = L.take([8], F32, "rb33")
    rb31, B_rb31 = L.take([8], F32, "rb31")
    fdsb, B_fdsb = L.take([384], F32, "fdsb")
    hank, B_hank = L.take([2, 128], F32, "hank")
    brow, B_brow = L.take([256], F32, "brow")
    grow, B_grow = L.take([2, 2048], F32, "grow")

    dma("sp", keysn[:, :, :], AP(tensor=keys_h, offset=0, ap=[[128, 128], [16384, 16], [1, 128]]), writes=[B_keysn])
    ki = wslot_ctr[0] % NW; wslot_ctr[0] += 1
    for hp in range(16):
        bank = 2 + (hp % 2)
        op("pe", lambda e, hp=hp, bank=bank: e.transpose(pb[bank][:, 0:128], keysn[:, hp, :], identf[:, :]),
           reads=[B_keysn, B_cst], writes=[B_pb[bank]])
        op("act", lambda e, hp=hp, bank=bank: e.activation(wb[ki][:, hp * 128:(hp + 1) * 128], pb[bank][:, 0:128], AF.Copy),
           reads=[B_pb[bank]], writes=[B_wb[ki]])
    dma("sp", wsc_h[CH_KEYS], wb[ki], reads=[B_wb[ki]], writes=[B_wsc[CH_KEYS]])

    dma("sp", cact[:, :], cT_h.ap(), writes=[B_cact])
    op("act", lambda e: e.activation(cactb[:, :], cact[:, :], AF.Silu), reads=[B_cact], writes=[B_cactb])
    dma("sp", grow[0:1, 0, :], g1_h.ap(), writes=[B_grow])
    dma("sp", grow[0:1, 1, :], g2_h.ap(), writes=[B_grow])
    CGW = 256
    for cg in range(6 * D // CGW):
        i = cg % 2
        dma("pool", wst[i][:, :, :], AP(tensor=wada_h, offset=cg * CGW, ap=[[6 * D, 128], [128 * 6 * D, 16], [1, CGW]]),
            writes=[B_wst[i]])
        dma("sp", brow[0:1, :], AP(tensor=bada_h, offset=cg * CGW, ap=[[0, 1], [1, CGW]]), writes=[B_brow])
        for kc in range(16):
            mm(pb[0][0:1, 0:CGW], cactb[:, kc:kc + 1], wst[i][:, kc, :], kc == 0, kc == 15,
               reads=[B_cactb, B_wst[i]], writes=[B_pb[0]])
        op("dve", lambda e: e.tensor_tensor(modrow[0:1, 0, :], pb[0][0:1, 0:CGW], brow[0:1, :], ALU.add),
           reads=[B_pb[0], B_brow], writes=[B_modrow])
        seg = (cg * CGW) // D
        if seg in (1, 4):
            gsel = 0 if seg == 1 else 1
            cs = (cg * CGW) % D
            op("dve", lambda e, gsel=gsel, cs=cs: e.scalar_tensor_tensor(
                modrow[0:1, 0, :], modrow[0:1, 0, :], 1.0, grow[0:1, gsel, cs:cs + CGW], ALU.add, ALU.mult),
               reads=[B_modrow, B_grow], writes=[B_modrow])
        dma("sp", AP(tensor=modv_h, offset=cg * CGW, ap=[[0, 1], [1, CGW]]), modrow[0:1, 0, :],
            reads=[B_modrow], writes=[B_modv])
    MV_SH1, MV_S1, MV_GT1, MV_SH2, MV_S2, MV_GT2 = [i * D for i in range(6)]

    op("dve", lambda e: e.memset(rb33[0:33, :], 1.0), writes=[B_rb])
    dma("sp", rb33[0:32, :], relb_h.ap(), reads=[], writes=[B_rb])
    dma("sp", rb31[0:32, :], AP(tensor=relb_h, offset=31 * 8, ap=[[0, 32], [1, 8]]), writes=[B_rb31])
    op("dve", lambda e: e.tensor_tensor(rb33[0:32, :], rb33[0:32, :], rb31[0:32, :], ALU.subtract),
       reads=[B_rb, B_rb31], writes=[B_rb])
    oh2v = oh2v_full[0:33, :]
    dma("sp", oh2v, cst_h["oh2"].ap(), writes=[B_oh2])
    mm(pb[1][0:8, 0:384], rb33[0:33, :], oh2v, True, True, reads=[B_rb, B_oh2], writes=[B_pb[1]])
    op("act", lambda e: e.activation(fdsb[0:8, :], pb[1][0:8, 0:384], AF.Copy), reads=[B_pb[1]], writes=[B_fdsb])
    dma("sp", fd_h.ap(), fdsb[0:8, :], reads=[B_fdsb], writes=[B_fd])
    for h in range(8):
        for dl in range(2):
            k = (h * 2 + dl) % 2
            dma("sp", hank[:, k, :], AP(tensor=fd_h, offset=h * 384 + 128 * dl, ap=[[1, 128], [1, 128]]),
                reads=[B_fd], writes=[B_hank])
            bank = 2 + k
            mm(pb[bank][:, 0:128], hank[:, k, :], antif[:, :], True, True, reads=[B_hank, B_cst], writes=[B_pb[bank]])
            op("act", lambda e, h=h, dl=dl, bank=bank: e.activation(biasT[:, h, dl, 0, :], pb[bank][:, 0:128], AF.Copy),
               reads=[B_pb[bank]], writes=[B_bias])
            op("dve", lambda e, h=h, dl=dl, bank=bank: e.tensor_tensor(
                biasT[:, h, dl, 1, :], pb[bank][:, 0:128], biasT[:, h, dl, 0, :], ALU.subtract),
               reads=[B_pb[bank], B_bias], writes=[B_bias])
    dump("biasT", biasT[:, :, :, :, :], [B_bias])
    dump("modv", modv_h.ap(), [B_modv])

    sc.barrier()
    B_uv = Buf("uvtab")
    LU = Lay(); stg = []; B_stg = []
    for i in range(4):
        v_, b_ = LU.take([4, D], BF16, f"stg{i}"); stg.append(v_); B_stg.append(b_)
    uctr = 0
    for blk in range(32):
        for half, th in ((0, pu_h), (1, pv_h)):
            k = uctr % 4; uctr += 1
            dma("pool", stg[k][:, :, :], AP(tensor=th, offset=blk * 512 * D, ap=[[4 * D, 128], [D, 4], [1, D]]),
                writes=[B_stg[k]])
            dma("sp", AP(tensor=uv_h, offset=blk * 512 * 2 * D + half * D, ap=[[4 * 2 * D, 128], [2 * D, 4], [1, D]]),
                stg[k][:, :, :], reads=[B_stg[k]], writes=[B_uv])
    sc.barrier()

    LA = Lay()
    bcA, B_bcA = LA.take([D], F32, "bcA"); bcB, B_bcB = LA.take([D], F32, "bcB")
    qT, B_qT = LA.take([8, T], BF16, "qT"); iqT, B_iqT = LA.take([8, T], BF16, "iqT")
    attnT, B_attnT = LA.take([8, T], BF16, "attnT")
    acc, B_acc = LA.take([S], F32, "acc"); mneg, B_mneg = LA.take([S], BF16, "mneg")
    rl = []; B_rl = []; pT = []; B_pT = []
    for i in range(2):
        v, b = LA.take([512], BF16, f"rl{i}"); rl.append(v); B_rl.append(b)
        v, b = LA.take([512], BF16, f"pT{i}"); pT.append(v); B_pT.append(b)
    attn_tm, B_atm = LA.take([1024], BF16, "attn_tm")
    iw, B_iw = LA.take([NSUB, 16], F32, "iw")
    m8, B_m8 = LA.take([8], F32, "m8")
    sqt, B_sqt = LA.take([T], BF16, "sqt"); rtq, B_rtq = LA.take([T], F32, "rtq")
    junk8, B_junk8 = LA.take([S // 2], BF16, "junk8"); junk8 = junk8.bitcast(mybir.dt.uint8)
    rdn, _ = LA.take([4], F32, "rdn"); B_rdn = [Buf("rdn0"), Buf("rdn1")]
    bs, B_bs = LA.take([8], F32, "bs"); Wt, B_Wt = LA.take([32], F32, "Wt"); W2t, B_W2t = LA.take([32], F32, "W2t")
    plb = []; B_plb = []
    for i in range(2):
        v, b = LA.take([16 + T], F32, f"plb{i}"); plb.append(v); B_plb.append(b)
    ptmp = []; B_ptmp = []
    for i in range(2):
        v, b = LA.take([16 + T], F32, f"ptmp{i}"); ptmp.append(v); B_ptmp.append(b)
    praw, B_praw = LA.take([8, T], BF16, "praw"); pooledT, B_pooled = LA.take([8, T], BF16, "pooledT")
    sga, B_sga = LA.take([16, T], BF16, "sga"); sgp, B_sgp = LA.take([16, T], BF16, "sgp")
    LB = Lay()
    LB.take([D], F32, "bcA"); LB.take([D], F32, "bcB")
    LB.take([8, T], BF16, "qT_"); LB.take([8, T], BF16, "iqT_"); LB.take([8, T], BF16, "attnT_")
    mergedT, B_mrg = LB.take([16, T], BF16, "mergedT")
    sg = []; B_sg = []
    for i in range(4):
        v, b = LB.take([T], F32, f"sg{i}"); sg.append(v); B_sg.append(b)
    rtmp, B_rtmp = LB.take([NSUB, 128], F32, "rtmp")
    assert LB.off <= 2 * 4 * D + 3 * 8 * T * 2 + 4 * S, LB.off
    LP = Lay()
    LP.take([D], F32, "bcA"); LP.take([D], F32, "bcB")
    mtop, B_mtop = LP.take([16, 16], F32, "mtop"); ixu, B_ixu = LP.take([16, 16], U32, "ixu")
    ixf, B_ixf = LP.take([16, 16], F32, "ixf")
    best, B_best = LP.take([8, 16], F32, "best"); posu, B_posu = LP.take([8, 16], U32, "posu")
    pa_u, B_pau = LP.take([128], U32, "pa_u"); pb_u, B_pbu = LP.take([128], U32, "pb_u")
    pa_f, B_paf = LP.take([128], F32, "pa_f"); pb_f, B_pbf = LP.take([128], F32, "pb_f")
    ia, B_ia = LP.take([128], F32, "ia"); ib, B_ib = LP.take([128], F32, "ib")
    eidf, B_eidf = LP.take([128], F32, "eidf")
    gsum, B_gsum = LP.take([8], F32, "gsum")
    eid = []; B_eid = []; gate = []; B_gate = []
    for j in range(NSUB):
        v, b = LP.take([128], I32, f"eid{j}"); eid.append(v); B_eid.append(b)
        v, b = LP.take([8, 16], F32, f"gate{j}"); gate.append(v); B_gate.append(b)
    dots, B_dots = LP.take([128], F32, "dots"); coef, B_coef = LP.take([128], F32, "coef")
    LPa = Lay(); LPa.off = LP.off
    qpT, B_qpT = LPa.take([16, T], BF16, "qpT")
    sub, B_sub = LPa.take([16, 128], F32, "sub")
    cand, B_cand = LPa.take([8, 256], F32, "cand")
    ohb, B_ohb = LPa.take([128, 16], F32, "ohb")
    LPb = Lay(); LPb.off = LPa.off
    prod = []; B_prod = []
    for i in range(2):
        v_, b_ = LPb.take([D], BF16, f"prod{i}"); prod.append(v_); B_prod.append(b_)
    NG = 8
    gb = []; B_gb = []
    gb.append(av(0, [2 * D], BF16)); B_gb.append([B_bcA])
    for i in range(1, 4):
        v_, b_ = LPb.take([2 * D], BF16, f"gb{i}"); gb.append(v_); B_gb.append([b_])
    for i in range(3):
        gb.append(wb_all[:, i * 2 * D:(i + 1) * 2 * D]); B_gb.append([B_wb[2 * i], B_wb[2 * i + 1]])
    gb.append(hT[:, :, :].rearrange("p k t -> p (k t)")); B_gb.append([B_hT])
    NPAR = 4
    dotg = []; B_dotg = []; gact = []; B_gact = []; dg = []; B_dg = []
    for i in range(NPAR):
        v_, b_ = LPb.take([2], F32, f"dotg{i}"); dotg.append(v_); B_dotg.append(b_)
        v_, b_ = LPb.take([2], F32, f"gact{i}"); gact.append(v_); B_gact.append(b_)
        v_, b_ = LPb.take([2, 128], BF16, f"dg{i}"); dg.append(v_); B_dg.append([Buf(f"dg{i}_0"), Buf(f"dg{i}_1")])
    rt2, B_rt2 = LPb.take([512], F32, "rt2")

    def flat(v):
        return v

    def rmsnorm_to_hT(j, s_off, b_off, first):
        c0 = 0 if first else 4
        op("act", lambda e: e.activation(hTM[:, j, :], xres[:, j, :], AF.Square, accum_out=stat[:, c0 + j:c0 + j + 1]),
           reads=[B_x[j]], writes=[B_hTM, B_stat])
        op("act", lambda e: e.activation(stat[:, 8 + j:9 + j], stat[:, c0 + j:c0 + j + 1], AF.Sqrt,
                                         bias=epst[:, 0:1], scale=1.0 / D), reads=[B_stat, B_cst], writes=[B_stat])
        op("dve", lambda e: e.reciprocal(stat[:, c0 + j:c0 + j + 1], stat[:, 8 + j:9 + j]), reads=[B_stat], writes=[B_stat])
        op("dve", lambda e: e.scalar_tensor_tensor(hTM[:, j, :], xres[:, j, :], stat[:, c0 + j:c0 + j + 1], bcA_ap(),
                                                   ALU.mult, ALU.mult), reads=[B_x[j], B_stat, B_bcA], writes=[B_hTM])
        op("dve", lambda e: e.tensor_tensor(hTM[:, j, :], hTM[:, j, :], bcB_ap(), ALU.add), reads=[B_hTM, B_bcB], writes=[B_hTM])
        for half in range(2):
            bank = 2 + half
            pbb = pb[bank][:, :].bitcast(BF16)
            for k8 in range(8):
                kc = half * 8 + k8
                op("pe", lambda e, kc=kc, k8=k8, pbb=pbb: e.transpose(pbb[:, k8 * 128:(k8 + 1) * 128],
                                                                       hTM[:, j, kc * 128:(kc + 1) * 128], identb[:, :]),
                   reads=[B_hTM, B_cst], writes=[B_pb[bank]])
            eng = "act" if half == 0 else "dve"
            src = pbb.rearrange("p (k t) -> p k t", k=8)
            dst = hT[:, half * 8:(half + 1) * 8, j * 128:(j + 1) * 128]
            if eng == "act":
                op("act", lambda e, src=src, dst=dst: e.activation(dst, src, AF.Copy), reads=[B_pb[bank]], writes=[B_hT])
            else:
                op("dve", lambda e, src=src, dst=dst: e.tensor_copy(dst, src), reads=[B_pb[bank]], writes=[B_hT])

    def bcA_ap():
        return arena[:, 0:D]

    def bcB_ap():
        return arena[:, D:2 * D]

    def load_bc(which, off):
        ap_ = bcA_ap() if which == 0 else bcB_ap()
        dma("sp", ap_, bcast_row(modv_h, off), reads=[B_modv], writes=[B_bcA if which == 0 else B_bcB])

    pbsel = [0]

    def next_bank01():
        pbsel[0] ^= 1
        return pbsel[0]

    def proj_fm(ch, nk, rhs_fn, rhs_bufs, ncols=T, col0=0, ldw=None):
        w, bw = ldw if ldw is not None else load_chunk(ch)
        bank = next_bank01()
        for kc in range(nk):
            lhsT = w[:, kc * 128:(kc + 1) * 128]
            mm(pb[bank][:, 0:ncols], lhsT, rhs_fn(kc), kc == 0, kc == nk - 1, reads=[bw] + rhs_bufs, writes=[B_pb[bank]])
        return bank

    hT_rhs = lambda kc: hT[:, kc, :]

    for ti in (tile_list if tile_list is not None else range(NT)):
        t0 = ti * T
        for j in range(NSUB):
            dma("sp", xres[:, j, :], x_h[t0 + j * 128:t0 + (j + 1) * 128, :], writes=[B_x[j]])
        load_bc(0, MV_S1); load_bc(1, MV_SH1)
        for j in range(NSUB):
            rmsnorm_to_hT(j, MV_S1, MV_SH1, True)
        if ti == 0:
            dump("hT", hT[:, :, :], [B_hT])

        if do_mix:
            for c in range(10):
                isq = c < 8
                bank = proj_fm((CH_Q + c) if isq else (CH_K + c - 8), 16, hT_rhs, [B_hT])
                op("act", lambda e, bank=bank: e.activation(sqt, pb[bank][:, 0:T], AF.Square),
                   reads=[B_pb[bank]], writes=[B_sqt])
                mm(pb[2][:, 0:T], onesb[:, :], sqt, True, True, reads=[B_cst, B_sqt], writes=[B_pb[2]])
                op("act", lambda e: e.activation(rtq, pb[2][:, 0:T], AF.Sqrt, bias=epst[:, 0:1], scale=1.0 / 128),
                   reads=[B_pb[2], B_cst], writes=[B_rtq])
                op("dve", lambda e: e.reciprocal(rtq, rtq), reads=[B_rtq], writes=[B_rtq])
                if isq:
                    dst = qT[:, c, :]; bd = B_qT; gcol = 0
                else:
                    dst = kT[:, c - 8, t0:t0 + T]; bd = B_kT; gcol = 1
                op("dve", lambda e, bank=bank, dst=dst, gcol=gcol: e.scalar_tensor_tensor(
                    dst, pb[bank][:, 0:T], gqs[:, gcol:gcol + 1], rtq, ALU.mult, ALU.mult),
                   reads=[B_pb[bank], B_gqs, B_rtq], writes=[bd])
            for c in range(8):
                bank = proj_fm(CH_IQ + c, 16, hT_rhs, [B_hT])
                op("act", lambda e, bank=bank, c=c: e.activation(iqT[:, c, :], pb[bank][:, 0:T], AF.Copy),
                   reads=[B_pb[bank]], writes=[B_iqT])
            bank = proj_fm(CH_IK, 16, hT_rhs, [B_hT])
            op("act", lambda e, bank=bank: e.activation(ikT[:, t0:t0 + T], pb[bank][:, 0:T], AF.Copy),
               reads=[B_pb[bank]], writes=[B_ik])
            for g in range(2):
                w, bw = load_chunk(CH_V + g)
                for j in range(NSUB):
                    bank = next_bank01()
                    for kc in range(16):
                        mm(pb[bank][:, 0:128], hT[:, kc, j * 128:(j + 1) * 128], w[:, kc * 128:(kc + 1) * 128],
                           kc == 0, kc == 15, reads=[B_hT, bw], writes=[B_pb[bank]])
                    op("act", lambda e, bank=bank, g=g, j=j: e.activation(vaug[:, ti * NSUB + j, g, 0:128], pb[bank][:, 0:128], AF.Copy),
                       reads=[B_pb[bank]], writes=[B_v])
            w, bw = load_chunk(CH_IW)
            for j in range(NSUB):
                bank = next_bank01()
                for kc in range(16):
                    mm(pb[bank][:, 0:16], hT[:, kc, j * 128:(j + 1) * 128], w[:, kc * 16:(kc + 1) * 16],
                       kc == 0, kc == 15, reads=[B_hT, bw], writes=[B_pb[bank]])
                op("act", lambda e, bank=bank, j=j: e.activation(iw[:, j, :], pb[bank][:, 0:16], AF.Copy),
                   reads=[B_pb[bank]], writes=[B_iw])
            if ti == 0:
                dump("qT", qT[:, :, :], [B_qT]); dump("kT", kT[:, :, 0:T], [B_kT]); dump("iw", iw[:, :, :], [B_iw])

            def indexer(j, ti=ti):
                qi = ti * NSUB + j; Lk = (qi + 1) * 128
                qs = slice(j * 128, (j + 1) * 128)
                use_mask = qi >= 2
                for ih in range(16):
                    c = ih // 2; p0 = (ih % 2) * 64
                    for s0 in range(0, Lk, 512):
                        n = min(512, Lk - s0)
                        bank = next_bank01(); r = (ih + s0 // 512) % 2
                        mm(pb[bank][:, 0:n], iqT[p0:p0 + 64, c, qs], ikT[p0:p0 + 64, s0:s0 + n], True, True,
                           reads=[B_iqT, B_ik], writes=[B_pb[bank]])
                        op("act", lambda e, bank=bank, r=r, n=n: e.activation(rl[r][:, 0:n], pb[bank][:, 0:n], AF.Relu),
                           reads=[B_pb[bank]], writes=[B_rl[r]])
                        last = (s0 + n == Lk)
                        if ih == 0:
                            nb = n - 128 if last else n
                            if nb > 0:
                                op("dve", lambda e, r=r, s0=s0, nb=nb, j=j: e.tensor_scalar(
                                    acc[:, s0:s0 + nb], rl[r][:, 0:nb], iw[:, j, 0:1], None, ALU.mult),
                                   reads=[B_rl[r], B_iw], writes=[B_acc])
                            if last:
                                op("dve", lambda e, r=r, s0=s0, n=n, j=j: e.scalar_tensor_tensor(
                                    acc[:, s0 + n - 128:s0 + n], rl[r][:, n - 128:n], iw[:, j, 0:1], cmask[:, :],
                                    ALU.mult, ALU.add), reads=[B_rl[r], B_iw, B_cst], writes=[B_acc])
                        else:
                            op("dve", lambda e, r=r, s0=s0, n=n, j=j, ih=ih: e.scalar_tensor_tensor(
                                acc[:, s0:s0 + n], rl[r][:, 0:n], iw[:, j, ih:ih + 1], acc[:, s0:s0 + n],
                                ALU.mult, ALU.add), reads=[B_rl[r], B_iw, B_acc], writes=[B_acc])
                if qi == 2:
                    dump("acc", acc[:, 0:384], [B_acc])
            def topk(j, ti=ti):
                qi = ti * NSUB + j; Lk = (qi + 1) * 128
                qs = slice(j * 128, (j + 1) * 128)
                use_mask = qi >= 2
                if use_mask:
                    KB = 24
                    op("dve", lambda e: e.max(out=m8, in_=acc[:, 0:Lk]), reads=[B_acc], writes=[B_m8])
                    op("dve", lambda e: e.tensor_reduce(out=bs[:, 4:5], in_=acc[:, 0:Lk - 128], axis=AX.X, op=ALU.min),
                       reads=[B_acc], writes=[B_bs])
                    op("dve", lambda e: e.tensor_tensor(bs[:, 5:6], m8[:, 0:1], bs[:, 4:5], ALU.subtract), reads=[B_m8, B_bs], writes=[B_bs])
                    op("dve", lambda e: e.tensor_scalar(Wt[:, :], pow2[:, :], bs[:, 5:6], None, ALU.mult), reads=[B_cst, B_bs], writes=[B_Wt])
                    op("dve", lambda e: e.tensor_scalar(W2t[:, :], Wt[:, :], 2.0, None, ALU.mult), reads=[B_Wt], writes=[B_W2t])
                    op("dve", lambda e: e.tensor_tensor(bs[:, 0:1], bs[:, 4:5], Wt[:, 0:1], ALU.add), reads=[B_bs, B_Wt], writes=[B_bs])
                    for kb in range(KB):
                        op("dve", lambda e: e.tensor_scalar(junk8[:, 0:Lk], acc[:, 0:Lk], bs[:, 0:1], None, ALU.is_ge, ALU.add,
                                                            accum_out=bs[:, 1:2]), reads=[B_acc, B_bs], writes=[B_junk8, B_bs])
                        op("dve", lambda e, kb=kb: e.tensor_scalar(bs[:, 2:3], bs[:, 1:2], 256.0, W2t[:, kb + 1:kb + 2], ALU.is_ge, ALU.mult),
                           reads=[B_bs, B_W2t], writes=[B_bs])
                        op("dve", lambda e, kb=kb: e.scalar_tensor_tensor(bs[:, 0:1], bs[:, 2:3], Wt[:, kb + 1:kb + 2], bs[:, 0:1],
                                                                          ALU.subtract, ALU.add), reads=[B_bs, B_Wt], writes=[B_bs])
                    op("dve", lambda e: e.tensor_tensor(bs[:, 3:4], bs[:, 0:1], Wt[:, KB:KB + 1], ALU.subtract), reads=[B_bs, B_Wt], writes=[B_bs])
                    op("dve", lambda e: e.tensor_scalar(mneg[:, 0:Lk], acc[:, 0:Lk], bs[:, 3:4], -1.0, ALU.is_lt, ALU.mult),
                       reads=[B_acc, B_bs], writes=[B_mneg])
                    if qi == 2:
                        dump("mneg", mneg[:, 0:384], [B_mneg])
            def attention(j, ti=ti):
                qi = ti * NSUB + j; Lk = (qi + 1) * 128
                qs = slice(j * 128, (j + 1) * 128)
                use_mask = qi >= 2
                for h in range(8):
                    g = h // 4
                    pvb = 6 + (h % 2)
                    for grp in range(0, qi + 1, 4):
                        scs = list(range(grp, min(grp + 4, qi + 1)))
                        lbk = 4 + ((grp // 4) % 2); pr = (grp // 4) % 2
                        for sc_ in scs:
                            col = (sc_ - grp) * 128
                            o = pb[lbk][:, col:col + 128]
                            extra = (1 if use_mask else 0) + (2 if sc_ >= qi - 1 else 0)
                            mm(o, kT[:, g, sc_ * 128:(sc_ + 1) * 128], qT[:, h, qs], True, extra == 0,
                               reads=[B_kT, B_qT], writes=[B_pb[lbk]])
                            if use_mask:
                                extra -= 1
                                mm(o, mneg[:, sc_ * 128:(sc_ + 1) * 128], identbig[:, :], False, extra == 0,
                                   reads=[B_mneg, B_cst], writes=[B_pb[lbk]])
                            if sc_ >= qi - 1:
                                dl = qi - sc_
                                mm(o, biasT[:, h, dl, 0, :], identb[:, :], False, False, reads=[B_bias, B_cst], writes=[B_pb[lbk]])
                                mm(o, biasT[:, h, dl, 1, :], identb[:, :], False, True, reads=[B_bias, B_cst], writes=[B_pb[lbk]])
                        ncol = len(scs) * 128
                        op("act", lambda e, lbk=lbk, pr=pr, ncol=ncol: e.activation(pT[pr][:, 0:ncol], pb[lbk][:, 0:ncol], AF.Exp),
                           reads=[B_pb[lbk]], writes=[B_pT[pr]])
                        for sc_ in scs:
                            col = (sc_ - grp) * 128
                            mm(pb[pvb][:, 0:129], pT[pr][:, col:col + 128], vaug[:, sc_, g, 0:129], sc_ == 0, sc_ == qi,
                               reads=[B_pT[pr], B_v], writes=[B_pb[pvb]])
                    ra = h % 2
                    op("act", lambda e, pvb=pvb, ra=ra: e.activation(rdn[:, ra:ra + 1], pb[pvb][:, 128:129], AF.Ln),
                       reads=[B_pb[pvb]], writes=[B_rdn[ra]])
                    op("act", lambda e, ra=ra: e.activation(rdn[:, ra:ra + 1], rdn[:, ra:ra + 1], AF.Exp, scale=-1.0),
                       reads=[B_rdn[ra]], writes=[B_rdn[ra]])
                    op("act", lambda e, pvb=pvb, h=h, ra=ra: e.activation(attn_tm[:, h * 128:(h + 1) * 128], pb[pvb][:, 0:128], AF.Copy,
                                                                          scale=rdn[:, ra:ra + 1]),
                       reads=[B_pb[pvb], B_rdn[ra]], writes=[B_atm])
                pbb = pb[3][:, :].bitcast(BF16)
                for h in range(8):
                    op("pe", lambda e, h=h: e.transpose(pbb[:, h * 128:(h + 1) * 128], attn_tm[:, h * 128:(h + 1) * 128], identb[:, :]),
                       reads=[B_atm, B_cst], writes=[B_pb[3]])
                op("act", lambda e, j=j: e.activation(attnT[:, :, j * 128:(j + 1) * 128], pbb.rearrange("p (k t) -> p k t", k=8), AF.Copy),
                   reads=[B_pb[3]], writes=[B_attnT])

            def pool_block(ti=ti):
                for c in range(8):
                    g = c // 2; wdw = (2, 4, 8, 16)[g]
                    bank = proj_fm(CH_PL + c, 16, hT_rhs, [B_hT])
                    pbuf = plb[c % 2]; bpl = B_plb[c % 2]
                    op("act", lambda e, bank=bank, pbuf=pbuf: e.activation(pbuf[:, 16:16 + T], pb[bank][:, 0:T], AF.Copy),
                       reads=[B_pb[bank]], writes=[bpl])
                    op("pool", lambda e, pbuf=pbuf, c=c: e.tensor_copy(pbuf[:, 0:16], halo[:, c, :]),
                       reads=[B_halo], writes=[bpl])
                    op("pool", lambda e, pbuf=pbuf, c=c: e.tensor_copy(halo[:, c, :], pbuf[:, T:T + 16]),
                       reads=[bpl], writes=[B_halo])
                    cur = pbuf; bcur = bpl; k = 1; st = 0; lo = 1
                    while k < wdw:
                        nxt = ptmp[st % 2]; bn = B_ptmp[st % 2]
                        op("pool", lambda e, cur=cur, nxt=nxt, k=k, lo=lo: e.tensor_tensor(
                            nxt[:, lo:16 + T], cur[:, lo:16 + T], cur[:, lo - k:16 + T - k], ALU.add),
                           reads=[bcur], writes=[bn])
                        cur = nxt; bcur = bn; k *= 2; lo += k; st += 1
                    oth = ptmp[st % 2]; both = B_ptmp[st % 2]
                    op("pool", lambda e, cur=cur, oth=oth, wdw=wdw: e.tensor_scalar(oth[:, 16:16 + T], cur[:, 16:16 + T], 1.0 / wdw, 0.0, ALU.mult, ALU.add),
                       reads=[bcur], writes=[both])
                    op("pool", lambda e, oth=oth, pbuf=pbuf, c=c: e.tensor_tensor(praw[:, c, :], oth[:, 16:16 + T], pbuf[:, 16:16 + T], ALU.subtract),
                       reads=[both, bpl], writes=[B_praw])
                    if ti == 0:
                        op("pool", lambda e, cur=cur, oth=oth, g=g: e.tensor_tensor(oth[:, 16:32], cur[:, 16:32], invc[:, g, :], ALU.mult),
                           reads=[bcur, B_cst], writes=[both])
                        op("pool", lambda e, oth=oth, pbuf=pbuf, c=c: e.tensor_tensor(praw[:, c, 0:16], oth[:, 16:32], pbuf[:, 16:32], ALU.subtract),
                           reads=[both, bpl, B_praw], writes=[B_praw])
                w, bw = load_chunk(CH_PW)
                for g in range(4):
                    for eh in range(2):
                        bank = next_bank01()
                        for kc in range(2):
                            lhsT = w[:, g * 512 + kc * 256 + eh * 128: g * 512 + kc * 256 + eh * 128 + 128]
                            mm(pb[bank][:, 0:T], lhsT, praw[:, 2 * g + kc, :], kc == 0, kc == 1, reads=[bw, B_praw], writes=[B_pb[bank]])
                        op("act", lambda e, bank=bank, g=g, eh=eh: e.activation(pooledT[:, 2 * g + eh, :], pb[bank][:, 0:T], AF.Copy,
                                                                                scale=pscale[:, 2 * g + eh:2 * g + eh + 1]),
                           reads=[B_pb[bank], B_cst], writes=[B_pooled])
                if ti == 0:
                    dump("pooledT", pooledT[:, :, :], [B_pooled])


            def gates(c_lo, c_hi):
                for c in range(c_lo, c_hi):
                    bank = proj_fm(CH_GA + c, 16, hT_rhs, [B_hT])
                    op("act", lambda e, bank=bank, c=c: e.activation(sga[:, c, :], pb[bank][:, 0:T], AF.Sigmoid),
                       reads=[B_pb[bank]], writes=[B_sga])
                    bank = proj_fm(CH_GP + c, 16, hT_rhs, [B_hT])
                    op("act", lambda e, bank=bank, c=c: e.activation(sgp[:, c, :], pb[bank][:, 0:T], AF.Sigmoid),
                       reads=[B_pb[bank]], writes=[B_sgp])

            indexer(0); pool_block(); gates(0, 8); topk(0)
            for j in range(1, NSUB):
                indexer(j); attention(j - 1)
                if j == 1:
                    gates(8, 16)
                topk(j)
            attention(NSUB - 1)
            if ti == 0:
                dump("attnT", attnT[:, :, :], [B_attnT])

            sc.barrier()
            wab = wpbk = None
            for c in range(16):
                if c % 2 == 0:
                    wab = load_chunk(CH_AB + c // 2); wpbk = load_chunk(CH_PB + c // 2)
                col0 = (c % 2) * 128
                for kc in range(8):
                    mm(pb[4][:, 0:T], wab[0][:, kc * 256 + col0:kc * 256 + col0 + 128], attnT[:, kc, :], kc == 0, kc == 7,
                       reads=[wab[1], B_attnT], writes=[B_pb[4]])
                for kc in range(8):
                    mm(pb[5][:, 0:T], wpbk[0][:, kc * 256 + col0:kc * 256 + col0 + 128], pooledT[:, kc, :], kc == 0, kc == 7,
                       reads=[wpbk[1], B_pooled], writes=[B_pb[5]])
                sa = (c % 2) * 2
                op("dve", lambda e, sa=sa, c=c: e.tensor_tensor(sg[sa], sga[:, c, :], pb[4][:, 0:T], ALU.mult),
                   reads=[B_sga, B_pb[4]], writes=[B_sg[sa]])
                op("dve", lambda e, sa=sa, c=c: e.tensor_tensor(sg[sa + 1], sgp[:, c, :], pb[5][:, 0:T], ALU.mult),
                   reads=[B_sgp, B_pb[5]], writes=[B_sg[sa + 1]])
                op("dve", lambda e, sa=sa, c=c: e.tensor_tensor(mergedT[:, c, :], sg[sa], sg[sa + 1], ALU.add),
                   reads=[B_sg[sa], B_sg[sa + 1]], writes=[B_mrg])
            if ti == 0:
                dump("mergedT", mergedT[:, :, :], [B_mrg])

            load_bc(0, MV_GT1)
            for cc in range(16):
                w, bw = load_chunk(CH_WO + cc)
                bank = next_bank01()
                for j in range(NSUB):
                    for kc in range(16):
                        mm(pb[bank][:, j * 128:(j + 1) * 128], mergedT[:, kc, j * 128:(j + 1) * 128], w[:, kc * 128:(kc + 1) * 128],
                           kc == 0, kc == 15, reads=[B_mrg, bw], writes=[B_pb[bank]])
                gt = AP(tensor=arena, offset=cc * 128,
                        ap=[[ARENA // 4, 128], [0, NSUB], [1, 128]])
                op("dve", lambda e, bank=bank, gt=gt: e.tensor_tensor(
                    rtmp[:, :, :], pb[bank][:, 0:T].rearrange("p (j c) -> p j c", j=NSUB), gt, ALU.mult),
                   reads=[B_pb[bank], B_bcA], writes=[B_rtmp])
                op("dve", lambda e, cc=cc: e.tensor_tensor(xres[:, :, cc * 128:(cc + 1) * 128], xres[:, :, cc * 128:(cc + 1) * 128],
                                                           rtmp[:, :, :], ALU.add), reads=[B_rtmp] + B_x, writes=B_x)
        if ti == NT - 1:
            dump("x1", xres[:, :, :], B_x)
        sc.barrier()

        if do_peer:
            load_bc(0, MV_S2); load_bc(1, MV_SH2)
            for j in range(NSUB):
                rmsnorm_to_hT(j, MV_S2, MV_SH2, False)
            for c in range(16):
                bank = proj_fm(CH_PQ + c, 16, hT_rhs, [B_hT])
                op("act", lambda e, bank=bank, c=c: e.activation(qpT[:, c, :], pb[bank][:, 0:T], AF.Copy),
                   reads=[B_pb[bank]], writes=[B_qpT])
            wk, bwk = load_chunk(CH_KEYS)
            deferred = []; defer_on = [False]

            def dop(en, fn, reads=(), writes=()):
                if defer_on[0]:
                    deferred.append((en, fn, list(reads), list(writes)))
                else:
                    op(en, fn, reads=reads, writes=writes)

            def flush(n):
                while deferred and n > 0:
                    en, fn, r_, w_ = deferred.pop(0)
                    op(en, fn, reads=r_, writes=w_); n -= 1
            for j in range(NSUB):
                for hp in range(16):
                    bank = 4 + hp // 4
                    mm(pb[bank][:, (hp % 4) * 128:(hp % 4 + 1) * 128], qpT[:, hp, j * 128:(j + 1) * 128],
                       wk[:, hp * 128:(hp + 1) * 128], True, True, reads=[B_qpT, bwk], writes=[B_pb[bank]])
                for b4 in range(4):
                    op("act", lambda e, b4=b4: e.activation(sub[:, b4 * 4:(b4 + 1) * 4, :],
                                                            pb[4 + b4][:, :].rearrange("p (a n) -> p a n", a=4), AF.Copy),
                       reads=[B_pb[4 + b4]], writes=[B_sub])
                defer_on[0] = (NSUB > 1 and j == NSUB - 1)
                for hp in range(16):
                    sv = sub[:, hp, :]
                    dop("dve", lambda e, sv=sv, hp=hp: e.max(out=mtop[:, hp, 0:8], in_=sv), reads=[B_sub], writes=[B_mtop])
                    dop("dve", lambda e, sv=sv, hp=hp: e.max_index(out=ixu[:, hp, 0:8], in_max=mtop[:, hp, 0:8], in_values=sv),
                       reads=[B_sub, B_mtop], writes=[B_ixu])
                    dop("dve", lambda e, sv=sv, hp=hp: e.match_replace(out=sv, in_to_replace=mtop[:, hp, 0:8], in_values=sv, imm_value=-1.0e30),
                       reads=[B_sub, B_mtop], writes=[B_sub])
                    dop("dve", lambda e, sv=sv, hp=hp: e.max(out=mtop[:, hp, 8:16], in_=sv), reads=[B_sub], writes=[B_mtop])
                    dop("dve", lambda e, sv=sv, hp=hp: e.max_index(out=ixu[:, hp, 8:16], in_max=mtop[:, hp, 8:16], in_values=sv),
                       reads=[B_sub, B_mtop], writes=[B_ixu])
                dop("dve", lambda e: e.tensor_copy(ixf[:, :, :], ixu[:, :, :]), reads=[B_ixu], writes=[B_ixf])
                mt_t = mtop.tensor; mt_off = mtop.offset; PST = ARENA // 4
                s1b = AP(tensor=mt_t, offset=mt_off, ap=[[PST, 128], [32, 8], [1, 16], [0, 16]])
                s2b = AP(tensor=mt_t, offset=mt_off + 16, ap=[[PST, 128], [32, 8], [0, 16], [1, 16]])
                candv = cand[:, :, :].rearrange("p h (a b) -> p h a b", a=16)
                dop("dve", lambda e: e.tensor_tensor(candv, s1b, s2b, ALU.add), reads=[B_mtop], writes=[B_cand])
                for h in range(8):
                    cv = cand[:, h, :]
                    dop("dve", lambda e, cv=cv, h=h: e.max(out=best[:, h, 0:8], in_=cv), reads=[B_cand], writes=[B_best])
                    dop("dve", lambda e, cv=cv, h=h: e.max_index(out=posu[:, h, 0:8], in_max=best[:, h, 0:8], in_values=cv),
                       reads=[B_cand, B_best], writes=[B_posu])
                    dop("dve", lambda e, cv=cv, h=h: e.match_replace(out=cv, in_to_replace=best[:, h, 0:8], in_values=cv, imm_value=-1.0e30),
                       reads=[B_cand, B_best], writes=[B_cand])
                    dop("dve", lambda e, cv=cv, h=h: e.max(out=best[:, h, 8:16], in_=cv), reads=[B_cand], writes=[B_best])
                    dop("dve", lambda e, cv=cv, h=h: e.max_index(out=posu[:, h, 8:16], in_max=best[:, h, 8:16], in_values=cv),
                       reads=[B_cand, B_best], writes=[B_posu])
                bt_t = best.tensor; bt_off = best.offset
                b0 = AP(tensor=bt_t, offset=bt_off, ap=[[PST, 128], [16, 8], [0, 16]])
                gj = gate[j]
                dop("dve", lambda e, gj=gj: e.tensor_tensor(gj[:, :, :], best[:, :, :], b0, ALU.subtract), reads=[B_best], writes=[B_gate[j]])
                dop("act", lambda e, gj=gj: e.activation(gj[:, :, :], gj[:, :, :], AF.Exp), reads=[B_gate[j]], writes=[B_gate[j]])
                dop("dve", lambda e, gj=gj: e.tensor_reduce(out=gsum[:, :], in_=gj[:, :, :], axis=AX.X, op=ALU.add),
                   reads=[B_gate[j]], writes=[B_gsum])
                dop("dve", lambda e: e.reciprocal(gsum[:, :], gsum[:, :]), reads=[B_gsum], writes=[B_gsum])
                gs_b = AP(tensor=gsum.tensor, offset=gsum.offset, ap=[[PST, 128], [1, 8], [0, 16]])
                dop("dve", lambda e, gj=gj: e.tensor_tensor(gj[:, :, :], gj[:, :, :], gs_b, ALU.mult), reads=[B_gate[j], B_gsum], writes=[B_gate[j]])
                posf = posu[:, :, :].rearrange("p h r -> p (h r)")
                dop("dve", lambda e: e.tensor_single_scalar(pa_u[:, :], posf, 4, ALU.logical_shift_right), reads=[B_posu], writes=[B_pau])
                dop("dve", lambda e: e.tensor_single_scalar(pb_u[:, :], posf, 15, ALU.bitwise_and), reads=[B_posu], writes=[B_pbu])
                dop("dve", lambda e: e.tensor_copy(pa_f[:, :], pa_u[:, :]), reads=[B_pau], writes=[B_paf])
                dop("dve", lambda e: e.tensor_copy(pb_f[:, :], pb_u[:, :]), reads=[B_pbu], writes=[B_pbf])
                io_b = iota16[:, :].unsqueeze(1).to_broadcast([128, 128, 16])
                for (pf, Bpf, half, dst, Bdst) in ((pa_f, B_paf, 0, ia, B_ia), (pb_f, B_pbf, 1, ib, B_ib)):
                    pfb = pf[:, :].unsqueeze(2).to_broadcast([128, 128, 16])
                    dop("dve", lambda e, pfb=pfb: e.tensor_tensor(ohb[:, :, :], pfb, io_b, ALU.is_equal), reads=[Bpf, B_cst], writes=[B_ohb])
                    ixb = AP(tensor=ixf.tensor, offset=ixf.offset + 16 * half, ap=[[PST, 128], [32, 8], [0, 16], [1, 16]])
                    oh4 = ohb[:, :, :].rearrange("p (h r) a -> p h r a", h=8)
                    dop("dve", lambda e, oh4=oh4, ixb=ixb: e.tensor_tensor(oh4, oh4, ixb, ALU.mult), reads=[B_ohb, B_ixf], writes=[B_ohb])
                    dop("dve", lambda e, dst=dst: e.tensor_reduce(out=dst[:, :], in_=ohb[:, :, :], axis=AX.X, op=ALU.add),
                       reads=[B_ohb], writes=[Bdst])
                dop("dve", lambda e: e.scalar_tensor_tensor(eidf[:, :], ia[:, :], 128.0, ib[:, :], ALU.mult, ALU.add),
                   reads=[B_ia, B_ib], writes=[B_eidf])
                dop("dve", lambda e, j=j: e.tensor_copy(eid[j][:, :], eidf[:, :]), reads=[B_eidf], writes=[B_eid[j]])
                defer_on[0] = False
                if ti == 0 and j == 0:
                    dump("eid", eid[0][:, :], [B_eid[0]]); dump("gate", gate[0][:, :, :], [B_gate[0]])

            load_bc(1, MV_GT2)
            GS = 2; NGRP = 128 // GS
            glist = [(j, g) for j in range(NSUB) for g in range(NGRP)]
            kof = {}
            gctr = 0

            def stage_A(idx):
                j, g = glist[idx]; par = idx % NPAR
                nonlocal_k = []
                for i in range(GS):
                    slot = g * GS + i
                    k = (idx * GS + i) % NG
                    nonlocal_k.append(k)
                    dma("pool", gb[k], uv_h.ap(), reads=[B_eid[j], B_uv], writes=B_gb[k],
                        indirect=bass.IndirectOffsetOnAxis(ap=eid[j][:, slot:slot + 1], axis=0))
                    pk = (idx * GS + i) % 2
                    op("dve", lambda e, k=k, j=j, pk=pk: e.tensor_tensor(prod[pk][:, :], hTM[:, j, :], gb[k][:, 0:D], ALU.mult),
                       reads=[B_hTM] + B_gb[k], writes=[B_prod[pk]])
                    op("act", lambda e, i=i, par=par, pk=pk: e.activation(prod[pk][:, :], prod[pk][:, :], AF.Copy,
                                                                          accum_out=dotg[par][:, i:i + 1]),
                       reads=[B_prod[pk]], writes=[B_prod[pk], B_dotg[par]])
                kof[idx] = nonlocal_k
                op("act", lambda e, par=par: e.activation(gact[par][:, :], dotg[par][:, :], AF.Gelu),
                   reads=[B_dotg[par]], writes=[B_gact[par]])

            def stage_C(idx):
                j, g = glist[idx]; par = idx % NPAR
                gflat = gate[j][:, :, :].rearrange("p h r -> p (h r)")
                for i in range(GS):
                    slot = g * GS + i; k = kof[idx][i]
                    op("dve", lambda e, i=i, par=par, slot=slot, gflat=gflat: e.tensor_scalar(
                        dg[par][:, i, :], identb[:, :], gact[par][:, i:i + 1], gflat[:, slot:slot + 1], ALU.mult, ALU.mult),
                       reads=[B_cst, B_gact[par], B_gate[j]], writes=[B_dg[par][i]])
                    for q4 in range(4):
                        mm(pb[q4][:, :], dg[par][:, i, :], gb[k][:, D + q4 * 512:D + (q4 + 1) * 512], slot == 0, slot == 127,
                           reads=[B_dg[par][i]] + B_gb[k], writes=[B_pb[q4]])
                if g == NGRP - 1:
                    for q4 in range(4):
                        op("dve", lambda e, q4=q4: e.tensor_tensor(rt2[:, :], pb[q4][:, :], arena[:, D + q4 * 512:D + (q4 + 1) * 512], ALU.mult),
                           reads=[B_pb[q4], B_bcB], writes=[B_rt2])
                        op("dve", lambda e, q4=q4, j=j: e.tensor_tensor(xres[:, j, q4 * 512:(q4 + 1) * 512], xres[:, j, q4 * 512:(q4 + 1) * 512],
                                                                        rt2[:, :], ALU.add), reads=[B_x[j], B_rt2], writes=[B_x[j]])

            for idx in range(len(glist) + 1):
                if idx < len(glist):
                    if glist[idx][0] > 0:
                        flush(10 ** 9)
                    stage_A(idx)
                if idx >= 1:
                    stage_C(idx - 1)
                flush(3)
            flush(10 ** 9)
        for j in range(NSUB):
            dma("sp", out_h[t0 + j * 128:t0 + (j + 1) * 128, :], xres[:, j, :], reads=[B_x[j]])
        sc.barrier()

    sc.drain_dmas("sp")
    return nc, stack


def _noop():
    pass


_CACHE = {}


def make_in_maps(inputs):
    cst = host_consts()
    f = lambda a: np.ascontiguousarray(np.asarray(a, dtype=np.float32))
    x = f(inputs["x"]); c = f(inputs["c"])
    shared = {
        "w_ada": f(inputs["w_ada"][0]), "b_ada": f(inputs["b_ada"][0]).reshape(1, -1),
        "g1": f(inputs["g_norm1"][0]).reshape(1, -1), "g2": f(inputs["g_norm2"][0]).reshape(1, -1),
        "w_in": f(inputs["w_in"][0]), "gq": f(inputs["g_q"][0]).reshape(128, 1), "gk": f(inputs["g_k"][0]).reshape(128, 1),
        "rel_bias": f(inputs["rel_bias"]), "pool_w": f(inputs["pool_w"][0]),
        "pscale": np.ascontiguousarray(f(inputs["pool_scale"][0]).reshape(8, 128).T),
        "w_attn_br": f(inputs["w_attn_br"][0]), "w_pool_br": f(inputs["w_pool_br"][0]),
        "w_out": f(inputs["w_out"][0]), "w_peer_q": f(inputs["w_peer_q"][0]),
        "peer_keys": f(inputs["peer_keys"][0]).reshape(16, 128, 128),
        "peer_u": f(inputs["peer_u"][0]), "peer_v": f(inputs["peer_v"][0]),
    }
    shared.update(cst)
    maps = []
    for b in range(x.shape[0]):
        m = dict(shared)
        m["x"] = x[b]
        m["cT"] = np.ascontiguousarray(c[b].reshape(16, 128).T)
        maps.append(m)
    return maps


def kernel(**inputs):
    nc, stack = build()
    maps = make_in_maps(inputs)
    res = run_bass_kernel_spmd(nc, maps, core_ids=list(range(8)))
    out = np.stack([np.asarray(r["out"], dtype=np.float32) for r in res.results], axis=0)
    return out
```

```python
import math
from contextlib import ExitStack
import numpy as np
import ml_dtypes
import concourse.bass as bass
import concourse.mybir as mybir
from concourse.bass_utils import run_bass_kernel_spmd

F32 = mybir.dt.float32; BF16 = mybir.dt.bfloat16; I32 = mybir.dt.int32; U32 = mybir.dt.uint32
ALU = mybir.AluOpType; AF = mybir.ActivationFunctionType; AX = mybir.AxisListType

D = 2048; S = 4096; T = 256; NSUB = T // 128; NTILES = S // T
INW = 7760
C_Q, C_K, C_V, C_IQ, C_IK, C_IW, C_PL, C_GA, C_GP = 0, 1024, 1280, 1536, 2560, 2624, 2640, 3664, 5712
CH_Q = 0; CH_K = 8; CH_V = 10; CH_IQ = 12; CH_IK = 20; CH_IW = 21; CH_PL = 22; CH_GA = 30; CH_GP = 46
CH_AB = 62; CH_PB = 70; CH_PW = 78; CH_WO = 79; CH_PQ = 95; CH_KEYS = 111; NCH = 112
EPS = 1e-6
EPOCH = 20000
NDS = 8
NEGBIG = -3.0e38


class Buf:
    __slots__ = ("name", "lw", "rd")

    def __init__(self, name):
        self.name = name; self.lw = None; self.rd = {}


class Eng:
    def __init__(self, idx, name, h):
        self.idx = idx; self.name = name; self.h = h; self.seq = 0; self.sems = []; self.waited = {}
        self.dcount = 0; self.dvals = [0] * NDS; self.dsems = None


class Sched:
    def __init__(self, nc, stack):
        self.nc = nc; self.stack = stack; self.E = {}
        for i, (n, h) in enumerate([("pe", nc.tensor), ("act", nc.scalar), ("dve", nc.vector),
                                    ("pool", nc.gpsimd), ("sp", nc.sync)]):
            self.E[n] = Eng(i, n, h)
        self.elist = list(self.E.values()); self.dsem_list = []

    def _esem(self, E, ep):
        while len(E.sems) <= ep:
            E.sems.append(self.stack.enter_context(self.nc.semaphore(f"e_{E.name}_{len(E.sems)}")))
        return E.sems[ep]

    def _wait(self, E, ev):
        kind, k, v = ev
        key = (kind, k)
        if E.waited.get(key, 0) >= v:
            return
        E.waited[key] = v
        if kind == "e":
            E2 = self.elist[k]; ep, val = divmod(v - 1, EPOCH)
            E.h.wait_ge(self._esem(E2, ep), val + 1)
        else:
            E.h.wait_ge(self.dsem_list[k], v)

    def op(self, en, fn, reads=(), writes=()):
        E = self.E[en]; deps = []
        for r in reads:
            if r.lw is not None and not (en == "pe" and r.lw[0] == "e" and r.lw[1] == E.idx):
                deps.append(r.lw)
        pe = en == "pe"
        for w in writes:
            if w.lw is not None and not (pe and w.lw[0] == "e" and w.lw[1] == E.idx):
                deps.append(w.lw)
            for ev in w.rd.values():
                if not (pe and ev[0] == "e" and ev[1] == E.idx):
                    deps.append(ev)
        for ev in deps:
            self._wait(E, ev)
        inst = fn(E.h)
        E.seq += 1; ep, _ = divmod(E.seq - 1, EPOCH)
        inst.then_inc(self._esem(E, ep), 1)
        ev = ("e", E.idx, E.seq)
        for r in reads:
            r.rd[("e", E.idx)] = ev
        for w in writes:
            w.lw = ev; w.rd = {}
        return inst

    def dma(self, qn, out, in_, reads=(), writes=(), indirect=None, **kw):
        Q = self.E[qn]
        if Q.dsems is None:
            Q.dsems = []
            for i in range(NDS):
                sem = self.stack.enter_context(self.nc.semaphore(f"d_{qn}_{i}"))
                Q.dsems.append(len(self.dsem_list)); self.dsem_list.append(sem)
        slot = Q.dcount % NDS; Q.dcount += 1
        k = Q.dsems[slot]; pv = Q.dvals[slot]
        if pv > 0:
            self._wait(Q, ("d", k, pv))
        deps = []
        for r in reads:
            if r.lw is not None:
                deps.append(r.lw)
        for w in writes:
            if w.lw is not None:
                deps.append(w.lw)
            deps.extend(w.rd.values())
        for ev in deps:
            self._wait(Q, ev)
        if indirect is not None:
            inst = Q.h.indirect_dma_start(out=out, out_offset=None, in_=in_, in_offset=indirect)
        else:
            inst = Q.h.dma_start(out=out, in_=in_, **kw)
        nv = pv + 16; Q.dvals[slot] = nv
        inst.then_inc(self.dsem_list[k], 16)
        ev = ("d", k, nv)
        for r in reads:
            r.rd[("d", k)] = ev
        for w in writes:
            w.lw = ev; w.rd = {}

    def barrier(self):
        evs = [("e", E.idx, E.seq) for E in self.elist if E.seq > 0]
        for Q in self.elist:
            if Q.dsems is not None:
                for slot in range(NDS):
                    if Q.dvals[slot] > 0:
                        evs.append(("d", Q.dsems[slot], Q.dvals[slot]))
        for E in self.elist:
            for ev in evs:
                if not (ev[0] == "e" and ev[1] == E.idx):
                    self._wait(E, ev)

    def drain_dmas(self, en="sp"):
        E = self.E[en]
        for Q in self.elist:
            if Q.dsems is not None:
                for slot in range(NDS):
                    if Q.dvals[slot] > 0:
                        self._wait(E, ("d", Q.dsems[slot], Q.dvals[slot]))


def t5_bucket_np(n):
    n = np.asarray(n)
    nf = np.maximum(n, 1).astype(np.float32)
    large = 16 + (np.log(nf / np.float32(16)) / np.float32(math.log(8.0)) * np.float32(16)).astype(np.int32)
    large = np.minimum(large, 31)
    return np.where(n < 16, n, large)


def host_consts():
    c = {}
    eye = np.eye(128, dtype=np.float32)
    c["identb"] = eye.astype(ml_dtypes.bfloat16)
    c["identbig"] = (eye * 32768.0).astype(ml_dtypes.bfloat16)
    c["identf"] = eye
    c["antif"] = np.ascontiguousarray(eye[::-1])
    q = np.arange(128)[:, None]; s = np.arange(128)[None, :]
    c["cmask"] = np.where(s <= q, 0.0, -1.0e30).astype(np.float32)
    c["onesb"] = np.ones((128, 128), dtype=ml_dtypes.bfloat16)
    oh = np.zeros((33, 384), dtype=np.float32)
    for j in range(383):
        dist = j - 127
        if dist >= 0:
            oh[int(t5_bucket_np(dist)), j] = 1.0
        else:
            oh[32, j] = -30000.0
    c["oh2"] = oh
    c["iota16"] = np.tile(np.arange(16, dtype=np.float32)[None, :], (128, 1))
    invc = np.zeros((128, 4, 16), dtype=np.float32)
    for g, w in enumerate((2, 4, 8, 16)):
        for t in range(16):
            invc[:, g, t] = 1.0 / min(t + 1, w)
    c["invc"] = invc
    c["pow2"] = np.tile((2.0 ** -(np.arange(32, dtype=np.float64) + 1)).astype(np.float32)[None, :], (128, 1))
    return c


CONST_SPECS = [("identb", [128, 128], BF16), ("identbig", [128, 128], BF16), ("identf", [128, 128], F32),
               ("antif", [128, 128], F32), ("cmask", [128, 128], F32), ("onesb", [128, 128], BF16),
               ("oh2", [33, 384], F32), ("iota16", [128, 16], F32), ("invc", [128, 4, 16], F32), ("pow2", [128, 32], F32)]


def build(NT=NTILES, dbg=None, do_mix=True, do_peer=True, tile_list=None):
    dbg = dbg or {}
    nc = bass.Bass("TRN2", target_bir_lowering=False)
    stack = ExitStack()
    dt = lambda name, shape, dtype, kind="ExternalInput": nc.dram_tensor(name, shape, dtype, kind=kind)
    x_h = dt("x", [S, D], F32); cT_h = dt("cT", [128, 16], F32)
    wada_h = dt("w_ada", [D, 6 * D], F32); bada_h = dt("b_ada", [1, 6 * D], F32)
    g1_h = dt("g1", [1, D], F32); g2_h = dt("g2", [1, D], F32)
    win_h = dt("w_in", [D, INW], F32); gq_h = dt("gq", [128, 1], F32); gk_h = dt("gk", [128, 1], F32)
    relb_h = dt("rel_bias", [32, 8], F32); poolw_h = dt("pool_w", [4, 256, 256], F32)
    pscale_h = dt("pscale", [128, 8], F32)
    wab_h = dt("w_attn_br", [1024, D], F32); wpb_h = dt("w_pool_br", [1024, D], F32)
    wout_h = dt("w_out", [D, D], F32); wpq_h = dt("w_peer_q", [D, D], F32)
    keys_h = dt("peer_keys", [16, 128, 128], F32)
    pu_h = dt("peer_u", [16384, D], F32); pv_h = dt("peer_v", [16384, D], F32)
    cst_h = {n: dt(n, sh, ty) for n, sh, ty in CONST_SPECS}
    out_h = dt("out", [S, D], F32, kind="ExternalOutput")
    wsc_h = dt("wsc", [NCH, 128, 2048], BF16, kind="Internal")
    modv_h = dt("modv", [1, 6 * D], F32, kind="Internal")
    fd_h = dt("fd", [8, 384], F32, kind="Internal")
    uv_h = dt("uvtab", [16384, 2 * D], BF16, kind="Internal")
    dbg_h = {n: dt("dbg_" + n, sh, ty, kind="ExternalOutput") for n, (sh, ty) in dbg.items()}

    sb = lambda name, shape, dtype: stack.enter_context(nc.sbuf_tensor("s_" + name, shape, dtype))
    ps = lambda name, shape, dtype: stack.enter_context(nc.psum_tensor(name, shape, dtype))
    sc = Sched(nc, stack)
    op = sc.op; dma = sc.dma
    AP = bass.AP

    kT = sb("kT", [128, 2, S], BF16); B_kT = Buf("kT")
    vaug = sb("vaug", [128, 32, 2, 130], BF16); B_v = Buf("vaug")
    ikT = sb("ikT", [128, S], BF16); B_ik = Buf("ikT")
    xres = sb("xres", [128, NSUB, D], F32); B_x = [Buf(f"x{j}") for j in range(NSUB)]
    hTM = sb("hTM", [128, NSUB, D], BF16); B_hTM = Buf("hTM")
    hT = sb("hT", [128, 16, T], BF16); B_hT = Buf("hT")
    NW = 6
    wb_all = sb("wb_all", [128, NW * 2048], BF16)
    wb = [wb_all[:, i * 2048:(i + 1) * 2048] for i in range(NW)]; B_wb = [Buf(f"wb{i}") for i in range(NW)]
    identb = sb("identb", [128, 128], BF16); identbig = sb("identbig", [128, 128], BF16)
    identf = sb("identf", [128, 128], F32); antif = sb("antif", [128, 128], F32)
    cmask = sb("cmask", [128, 128], F32); onesb = sb("onesb", [128, 128], BF16)
    iota16 = sb("iota16", [128, 16], F32); invc = sb("invc", [128, 4, 16], F32); pow2 = sb("pow2", [128, 32], F32)
    B_cst = Buf("cst")
    biasT = sb("biasT", [128, 8, 2, 2, 128], BF16); B_bias = Buf("biasT")
    gqs = sb("gqs", [128, 2], F32); B_gqs = Buf("gqs")
    pscale = sb("pscale", [128, 8], F32)
    epst = sb("epst", [128, 1], F32)
    halo = sb("halo", [128, 8, 16], F32); B_halo = Buf("halo")
    stat = sb("stat", [128, 16], F32); B_stat = Buf("stat")
    ARENA = 96 * 1024
    arena = sb("arena", [128, ARENA // 4], F32)

    def av(off, shape, dtype):
        n = int(np.prod(shape)); esz = 4 if dtype in (F32, I32, U32) else 2
        assert off % 4 == 0 and off + n * esz <= ARENA, (off, shape)
        a = arena[:, off // 4:(off + n * esz) // 4]
        if dtype != F32:
            a = a.bitcast(dtype)
        if len(shape) == 2:
            a = a.rearrange("p (a b) -> p a b", a=shape[0])
        elif len(shape) == 3:
            a = a.rearrange("p (a b c) -> p a b c", a=shape[0], b=shape[1])
        elif len(shape) == 4:
            a = a.rearrange("p (a b c d) -> p a b c d", a=shape[0], b=shape[1], c=shape[2])
        return a

    class Lay:
        def __init__(self):
            self.off = 0

        def take(self, shape, dtype, name):
            n = int(np.prod(shape)); esz = 4 if dtype in (F32, I32, U32) else 2
            v = av(self.off, shape, dtype)
            self.off += (n * esz + 31) // 32 * 32
            return v, Buf(name)

    pb = [ps(f"pb{i}", [128, 512], F32) for i in range(8)]; B_pb = [Buf(f"pb{i}") for i in range(8)]

    wslot_ctr = [0]

    def load_chunk(ch):
        i = wslot_ctr[0] % NW; wslot_ctr[0] += 1
        dma("sp", wb[i], wsc_h[ch], reads=[B_wsc[ch]], writes=[B_wb[i]])
        return wb[i], B_wb[i]

    def mm(out, lhsT, rhs, start, stop, reads, writes):
        return op("pe", lambda e: e.matmul(out, lhsT, rhs, start=start, stop=stop), reads=reads, writes=writes)

    def bcast_row(h, off, n=D, parts=128):
        return AP(tensor=h, offset=off, ap=[[0, parts], [1, n]])

    B_wsc = [Buf(f"wsc{i}") for i in range(NCH)]
    B_modv = Buf("modv"); B_fd = Buf("fd")
    B_dbg = {n: Buf("dbg_" + n) for n in dbg}

    def dump(name, src_ap, src_bufs):
        if name in dbg_h:
            dma("sp", dbg_h[name].ap(), src_ap, reads=src_bufs, writes=[B_dbg[name]])

    for n, t in [("identb", identb), ("identbig", identbig), ("identf", identf), ("antif", antif),
                 ("cmask", cmask), ("onesb", onesb), ("iota16", iota16), ("invc", invc), ("pow2", pow2)]:
        dma("sp", t[:], cst_h[n].ap(), writes=[B_cst])
    dma("sp", pscale[:, :], pscale_h.ap(), writes=[B_cst])
    dma("sp", gqs[:, 0:1], gq_h.ap(), writes=[B_gqs])
    dma("sp", gqs[:, 1:2], gk_h.ap(), writes=[B_gqs])
    op("dve", lambda e: e.memset(epst[:, :], EPS), writes=[B_cst])
    op("dve", lambda e: e.tensor_scalar(gqs[:, 0:1], gqs[:, 0:1], float(128 ** -0.5), None, ALU.mult),
       reads=[B_gqs], writes=[B_gqs])
    op("dve", lambda e: e.memset(halo[:, :, :], 0.0), writes=[B_halo])
    op("pool", lambda e: e.memset(vaug[:, :, :, 128:130], 1.0), writes=[B_v])

    def cast_store(ch, loads):
        i = wslot_ctr[0] % NW; wslot_ctr[0] += 1
        for (o, src) in loads:
            dma("pool", o(wb[i]), src, writes=[B_wb[i]])
        dma("sp", wsc_h[ch], wb[i], reads=[B_wb[i]], writes=[B_wsc[ch]])

    def wsrc(h, ncolsW, c0, nk, ncols):
        return AP(tensor=h, offset=c0, ap=[[ncolsW, 128], [128 * ncolsW, nk], [1, ncols]])

    def v3(nk, ncols, c_lo=0, c_n=None):
        c_n = ncols if c_n is None else c_n
        return lambda w: w[:, 0:nk * ncols].rearrange("p (k c) -> p k c", k=nk)[:, :, c_lo:c_lo + c_n]

    def std_chunks(ch0, h, ncolsW, c0, n):
        for i in range(n):
            cast_store(ch0 + i, [(v3(16, 128), wsrc(h, ncolsW, c0 + i * 128, 16, 128))])

    std_chunks(CH_Q, win_h, INW, C_Q, 8); std_chunks(CH_K, win_h, INW, C_K, 2)
    std_chunks(CH_V, win_h, INW, C_V, 2); std_chunks(CH_IQ, win_h, INW, C_IQ, 8)
    cast_store(CH_IK, [(v3(16, 128, 0, 64), wsrc(win_h, INW, C_IK, 16, 64)),
                       (v3(16, 128, 64, 64), wsrc(win_h, INW, C_IK, 16, 64))])
    cast_store(CH_IW, [(v3(16, 16), wsrc(win_h, INW, C_IW, 16, 16))])
    std_chunks(CH_PL, win_h, INW, C_PL, 8); std_chunks(CH_GA, win_h, INW, C_GA, 16)
    std_chunks(CH_GP, win_h, INW, C_GP, 16)
    for i in range(8):
        cast_store(CH_AB + i, [(v3(8, 256), wsrc(wab_h, D, i * 256, 8, 256))])
        cast_store(CH_PB + i, [(v3(8, 256), wsrc(wpb_h, D, i * 256, 8, 256))])
    cast_store(CH_PW, [((lambda w, g=g: w[:, g * 512:(g + 1) * 512].rearrange("p (k c) -> p k c", k=2)),
                        AP(tensor=poolw_h, offset=g * 65536, ap=[[256, 128], [32768, 2], [1, 256]]))
                       for g in range(4)])
    std_chunks(CH_WO, wout_h, D, 0, 16); std_chunks(CH_PQ, wpq_h, D, 0, 16)

    L = Lay()
    wst = []; B_wst = []
    for i in range(2):
        v, b = L.take([16, 256], BF16, f"wst{i}"); wst.append(v); B_wst.append(b)
    modrow, B_modrow = L.take([1, 256], F32, "modrow")
    oh2v_full, B_oh2 = L.take([384], F32, "oh2")
    cact, B_cact = L.take([16], F32, "cact"); cactb, B_cactb = L.take([16], BF16, "cactb")
    keysn, B_keysn = L.take([16, 128], F32, "keysn")
    rb33, B_rb = L.take([8], F32, "rb33")
    rb31, B_rb31 = L.take([8], F32, "rb31")
    fdsb, B_fdsb = L.take([384], F32, "fdsb")
    hank, B_hank = L.take([2, 128], F32, "hank")
    brow, B_brow = L.take([256], F32, "brow")
    grow, B_grow = L.take([2, 2048], F32, "grow")

    dma("sp", keysn[:, :, :], AP(tensor=keys_h, offset=0, ap=[[128, 128], [16384, 16], [1, 128]]), writes=[B_keysn])
    ki = wslot_ctr[0] % NW; wslot_ctr[0] += 1
    for hp in range(16):
        bank = 2 + (hp % 2)
        op("pe", lambda e, hp=hp, bank=bank: e.transpose(pb[bank][:, 0:128], keysn[:, hp, :], identf[:, :]),
           reads=[B_keysn, B_cst], writes=[B_pb[bank]])
        op("act", lambda e, hp=hp, bank=bank: e.activation(wb[ki][:, hp * 128:(hp + 1) * 128], pb[bank][:, 0:128], AF.Copy),
           reads=[B_pb[bank]], writes=[B_wb[ki]])
    dma("sp", wsc_h[CH_KEYS], wb[ki], reads=[B_wb[ki]], writes=[B_wsc[CH_KEYS]])

    dma("sp", cact[:, :], cT_h.ap(), writes=[B_cact])
    op("act", lambda e: e.activation(cactb[:, :], cact[:, :], AF.Silu), reads=[B_cact], writes=[B_cactb])
    dma("sp", grow[0:1, 0, :], g1_h.ap(), writes=[B_grow])
    dma("sp", grow[0:1, 1, :], g2_h.ap(), writes=[B_grow])
    CGW = 256
    for cg in range(6 * D // CGW):
        i = cg % 2
        dma("pool", wst[i][:, :, :], AP(tensor=wada_h, offset=cg * CGW, ap=[[6 * D, 128], [128 * 6 * D, 16], [1, CGW]]),
            writes=[B_wst[i]])
        dma("sp", brow[0:1, :], AP(tensor=bada_h, offset=cg * CGW, ap=[[0, 1], [1, CGW]]), writes=[B_brow])
        for kc in range(16):
            mm(pb[0][0:1, 0:CGW], cactb[:, kc:kc + 1], wst[i][:, kc, :], kc == 0, kc == 15,
               reads=[B_cactb, B_wst[i]], writes=[B_pb[0]])
        op("dve", lambda e: e.tensor_tensor(modrow[0:1, 0, :], pb[0][0:1, 0:CGW], brow[0:1, :], ALU.add),
           reads=[B_pb[0], B_brow], writes=[B_modrow])
        seg = (cg * CGW) // D
        if seg in (1, 4):
            gsel = 0 if seg == 1 else 1
            cs = (cg * CGW) % D
            op("dve", lambda e, gsel=gsel, cs=cs: e.scalar_tensor_tensor(
                modrow[0:1, 0, :], modrow[0:1, 0, :], 1.0, grow[0:1, gsel, cs:cs + CGW], ALU.add, ALU.mult),
               reads=[B_modrow, B_grow], writes=[B_modrow])
        dma("sp", AP(tensor=modv_h, offset=cg * CGW, ap=[[0, 1], [1, CGW]]), modrow[0:1, 0, :],
            reads=[B_modrow], writes=[B_modv])
    MV_SH1, MV_S1, MV_GT1, MV_SH2, MV_S2, MV_GT2 = [i * D for i in range(6)]

    op("dve", lambda e: e.memset(rb33[0:33, :], 1.0), writes=[B_rb])
    dma("sp", rb33[0:32, :], relb_h.ap(), reads=[], writes=[B_rb])
    dma("sp", rb31[0:32, :], AP(tensor=relb_h, offset=31 * 8, ap=[[0, 32], [1, 8]]), writes=[B_rb31])
    op("dve", lambda e: e.tensor_tensor(rb33[0:32, :], rb33[0:32, :], rb31[0:32, :], ALU.subtract),
       reads=[B_rb, B_rb31], writes=[B_rb])
    oh2v = oh2v_full[0:33, :]
    dma("sp", oh2v, cst_h["oh2"].ap(), writes=[B_oh2])
    mm(pb[1][0:8, 0:384], rb33[0:33, :], oh2v, True, True, reads=[B_rb, B_oh2], writes=[B_pb[1]])
    op("act", lambda e: e.activation(fdsb[0:8, :], pb[1][0:8, 0:384], AF.Copy), reads=[B_pb[1]], writes=[B_fdsb])
    dma("sp", fd_h.ap(), fdsb[0:8, :], reads=[B_fdsb], writes=[B_fd])
    for h in range(8):
        for dl in range(2):
            k = (h * 2 + dl) % 2
            dma("sp", hank[:, k, :], AP(tensor=fd_h, offset=h * 384 + 128 * dl, ap=[[1, 128], [1, 128]]),
                reads=[B_fd], writes=[B_hank])
            bank = 2 + k
            mm(pb[bank][:, 0:128], hank[:, k, :], antif[:, :], True, True, reads=[B_hank, B_cst], writes=[B_pb[bank]])
            op("act", lambda e, h=h, dl=dl, bank=bank: e.activation(biasT[:, h, dl, 0, :], pb[bank][:, 0:128], AF.Copy),
               reads=[B_pb[bank]], writes=[B_bias])
            op("dve", lambda e, h=h, dl=dl, bank=bank: e.tensor_tensor(
                biasT[:, h, dl, 1, :], pb[bank][:, 0:128], biasT[:, h, dl, 0, :], ALU.subtract),
               reads=[B_pb[bank], B_bias], writes=[B_bias])
    dump("biasT", biasT[:, :, :, :, :], [B_bias])
    dump("modv", modv_h.ap(), [B_modv])

    sc.barrier()
    B_uv = Buf("uvtab")
    LU = Lay(); stg = []; B_stg = []
    for i in range(4):
        v_, b_ = LU.take([4, D], BF16, f"stg{i}"); stg.append(v_); B_stg.append(b_)
    uctr = 0
    for blk in range(32):
        for half, th in ((0, pu_h), (1, pv_h)):
            k = uctr % 4; uctr += 1
            dma("pool", stg[k][:, :, :], AP(tensor=th, offset=blk * 512 * D, ap=[[4 * D, 128], [D, 4], [1, D]]),
                writes=[B_stg[k]])
            dma("sp", AP(tensor=uv_h, offset=blk * 512 * 2 * D + half * D, ap=[[4 * 2 * D, 128], [2 * D, 4], [1, D]]),
                stg[k][:, :, :], reads=[B_stg[k]], writes=[B_uv])
    sc.barrier()

    LA = Lay()
    bcA, B_bcA = LA.take([D], F32, "bcA"); bcB, B_bcB = LA.take([D], F32, "bcB")
    qT, B_qT = LA.take([8, T], BF16, "qT"); iqT, B_iqT = LA.take([8, T], BF16, "iqT")
    attnT, B_attnT = LA.take([8, T], BF16, "attnT")
    acc, B_acc = LA.take([S], F32, "acc"); mneg, B_mneg = LA.take([S], BF16, "mneg")
    rl = []; B_rl = []; pT = []; B_pT = []
    for i in range(2):
        v, b = LA.take([512], BF16, f"rl{i}"); rl.append(v); B_rl.append(b)
        v, b = LA.take([512], BF16, f"pT{i}"); pT.append(v); B_pT.append(b)
    attn_tm, B_atm = LA.take([1024], BF16, "attn_tm")
    iw, B_iw = LA.take([NSUB, 16], F32, "iw")
    m8, B_m8 = LA.take([8], F32, "m8")
    sqt, B_sqt = LA.take([T], BF16, "sqt"); rtq, B_rtq = LA.take([T], F32, "rtq")
    junk8, B_junk8 = LA.take([S // 2], BF16, "junk8"); junk8 = junk8.bitcast(mybir.dt.uint8)
    rdn, _ = LA.take([4], F32, "rdn"); B_rdn = [Buf("rdn0"), Buf("rdn1")]
    bs, B_bs = LA.take([8], F32, "bs"); Wt, B_Wt = LA.take([32], F32, "Wt"); W2t, B_W2t = LA.take([32], F32, "W2t")
    plb = []; B_plb = []
    for i in range(2):
        v, b = LA.take([16 + T], F32, f"plb{i}"); plb.append(v); B_plb.append(b)
    ptmp = []; B_ptmp = []
    for i in range(2):
        v, b = LA.take([16 + T], F32, f"ptmp{i}"); ptmp.append(v); B_ptmp.append(b)
    praw, B_praw = LA.take([8, T], BF16, "praw"); pooledT, B_pooled = LA.take([8, T], BF16, "pooledT")
    sga, B_sga = LA.take([16, T], BF16, "sga"); sgp, B_sgp = LA.take([16, T], BF16, "sgp")
    qsb = []; B_qsb = []
    for i in range(2):
        v, b = LA.take([T], BF16, f"qsb{i}"); qsb.append(v); B_qsb.append(b)
    LB = Lay()
    LB.take([D], F32, "bcA"); LB.take([D], F32, "bcB")
    LB.take([8, T], BF16, "qT_"); LB.take([8, T], BF16, "iqT_"); LB.take([8, T], BF16, "attnT_")
    mergedT, B_mrg = LB.take([16, T], BF16, "mergedT")
    sg = []; B_sg = []
    for i in range(4):
        v, b = LB.take([T], F32, f"sg{i}"); sg.append(v); B_sg.append(b)
    rtmp, B_rtmp = LB.take([NSUB, 128], F32, "rtmp")
    assert LB.off <= 2 * 4 * D + 3 * 8 * T * 2 + 4 * S, LB.off
    LP = Lay()
    LP.take([D], F32, "bcA"); LP.take([D], F32, "bcB")
    mtop, B_mtop = LP.take([16, 16], F32, "mtop"); ixu, B_ixu = LP.take([16, 16], U32, "ixu")
    ixf, B_ixf = LP.take([16, 16], F32, "ixf")
    best, B_best = LP.take([8, 16], F32, "best"); posu, B_posu = LP.take([8, 16], U32, "posu")
    pa_u, B_pau = LP.take([128], U32, "pa_u"); pb_u, B_pbu = LP.take([128], U32, "pb_u")
    pa_f, B_paf = LP.take([128], F32, "pa_f"); pb_f, B_pbf = LP.take([128], F32, "pb_f")
    ia, B_ia = LP.take([128], F32, "ia"); ib, B_ib = LP.take([128], F32, "ib")
    eidf, B_eidf = LP.take([128], F32, "eidf")
    gsum, B_gsum = LP.take([8], F32, "gsum")
    eid = []; B_eid = []; gate = []; B_gate = []
    for j in range(NSUB):
        v, b = LP.take([128], I32, f"eid{j}"); eid.append(v); B_eid.append(b)
        v, b = LP.take([8, 16], F32, f"gate{j}"); gate.append(v); B_gate.append(b)
    dots, B_dots = LP.take([128], F32, "dots"); coef, B_coef = LP.take([128], F32, "coef")
    LPa = Lay(); LPa.off = LP.off
    qpT, B_qpT = LPa.take([16, T], BF16, "qpT")
    sub, B_sub = LPa.take([16, 128], F32, "sub")
    cand, B_cand = LPa.take([8, 256], F32, "cand")
    ohb, B_ohb = LPa.take([128, 16], F32, "ohb")
    LPb = Lay(); LPb.off = LPa.off
    prod = []; B_prod = []
    for i in range(2):
        v_, b_ = LPb.take([D], BF16, f"prod{i}"); prod.append(v_); B_prod.append(b_)
    NG = 8
    gb = []; B_gb = []
    gb.append(av(0, [2 * D], BF16)); B_gb.append([B_bcA])
    for i in range(1, 4):
        v_, b_ = LPb.take([2 * D], BF16, f"gb{i}"); gb.append(v_); B_gb.append([b_])
    for i in range(3):
        gb.append(wb_all[:, i * 2 * D:(i + 1) * 2 * D]); B_gb.append([B_wb[2 * i], B_wb[2 * i + 1]])
    gb.append(hT[:, :, :].rearrange("p k t -> p (k t)")); B_gb.append([B_hT])
    NPAR = 4
    dotg = []; B_dotg = []; gact = []; B_gact = []; dg = []; B_dg = []
    for i in range(NPAR):
        v_, b_ = LPb.take([2], F32, f"dotg{i}"); dotg.append(v_); B_dotg.append(b_)
        v_, b_ = LPb.take([2], F32, f"gact{i}"); gact.append(v_); B_gact.append(b_)
        v_, b_ = LPb.take([2, 128], BF16, f"dg{i}"); dg.append(v_); B_dg.append([Buf(f"dg{i}_0"), Buf(f"dg{i}_1")])
    rt2, B_rt2 = LPb.take([512], F32, "rt2")

    def flat(v):
        return v

    def rmsnorm_to_hT(j, s_off, b_off, first):
        c0 = 0 if first else 4
        op("act", lambda e: e.activation(hTM[:, j, :], xres[:, j, :], AF.Square, accum_out=stat[:, c0 + j:c0 + j + 1]),
           reads=[B_x[j]], writes=[B_hTM, B_stat])
        op("act", lambda e: e.activation(stat[:, 8 + j:9 + j], stat[:, c0 + j:c0 + j + 1], AF.Sqrt,
                                         bias=epst[:, 0:1], scale=1.0 / D), reads=[B_stat, B_cst], writes=[B_stat])
        op("dve", lambda e: e.reciprocal(stat[:, c0 + j:c0 + j + 1], stat[:, 8 + j:9 + j]), reads=[B_stat], writes=[B_stat])
        op("dve", lambda e: e.scalar_tensor_tensor(hTM[:, j, :], xres[:, j, :], stat[:, c0 + j:c0 + j + 1], bcA_ap(),
                                                   ALU.mult, ALU.mult), reads=[B_x[j], B_stat, B_bcA], writes=[B_hTM])
        op("dve", lambda e: e.tensor_tensor(hTM[:, j, :], hTM[:, j, :], bcB_ap(), ALU.add), reads=[B_hTM, B_bcB], writes=[B_hTM])
        for half in range(2):
            bank = 2 + half
            pbb = pb[bank][:, :].bitcast(BF16)
            for k8 in range(8):
                kc = half * 8 + k8
                op("pe", lambda e, kc=kc, k8=k8, pbb=pbb: e.transpose(pbb[:, k8 * 128:(k8 + 1) * 128],
                                                                       hTM[:, j, kc * 128:(kc + 1) * 128], identb[:, :]),
                   reads=[B_hTM, B_cst], writes=[B_pb[bank]])
            eng = "act" if half == 0 else "dve"
            src = pbb.rearrange("p (k t) -> p k t", k=8)
            dst = hT[:, half * 8:(half + 1) * 8, j * 128:(j + 1) * 128]
            if eng == "act":
                op("act", lambda e, src=src, dst=dst: e.activation(dst, src, AF.Copy), reads=[B_pb[bank]], writes=[B_hT])
            else:
                op("dve", lambda e, src=src, dst=dst: e.tensor_copy(dst, src), reads=[B_pb[bank]], writes=[B_hT])

    def bcA_ap():
        return arena[:, 0:D]

    def bcB_ap():
        return arena[:, D:2 * D]

    def load_bc(which, off):
        ap_ = bcA_ap() if which == 0 else bcB_ap()
        dma("sp", ap_, bcast_row(modv_h, off), reads=[B_modv], writes=[B_bcA if which == 0 else B_bcB])

    pbsel = [0]

    def next_bank01():
        pbsel[0] ^= 1
        return pbsel[0]

    def proj_fm(ch, nk, rhs_fn, rhs_bufs, ncols=T, col0=0, ldw=None):
        w, bw = ldw if ldw is not None else load_chunk(ch)
        bank = next_bank01()
        for kc in range(nk):
            lhsT = w[:, kc * 128:(kc + 1) * 128]
            mm(pb[bank][:, 0:ncols], lhsT, rhs_fn(kc), kc == 0, kc == nk - 1, reads=[bw] + rhs_bufs, writes=[B_pb[bank]])
        return bank

    hT_rhs = lambda kc: hT[:, kc, :]

    for ti in (tile_list if tile_list is not None else range(NT)):
        t0 = ti * T
        for j in range(NSUB):
            dma("sp", xres[:, j, :], x_h[t0 + j * 128:t0 + (j + 1) * 128, :], writes=[B_x[j]])
        load_bc(0, MV_S1); load_bc(1, MV_SH1)
        for j in range(NSUB):
            rmsnorm_to_hT(j, MV_S1, MV_SH1, True)
        if ti == 0:
            dump("hT", hT[:, :, :], [B_hT])

        if do_mix:
            for c in range(8):
                bank = proj_fm(CH_IQ + c, 16, hT_rhs, [B_hT])
                op("act", lambda e, bank=bank, c=c: e.activation(iqT[:, c, :], pb[bank][:, 0:T], AF.Copy),
                   reads=[B_pb[bank]], writes=[B_iqT])
            bank = proj_fm(CH_IK, 16, hT_rhs, [B_hT])
            op("act", lambda e, bank=bank: e.activation(ikT[:, t0:t0 + T], pb[bank][:, 0:T], AF.Copy),
               reads=[B_pb[bank]], writes=[B_ik])
            w, bw = load_chunk(CH_IW)
            for j in range(NSUB):
                bank = next_bank01()
                for kc in range(16):
                    mm(pb[bank][:, 0:16], hT[:, kc, j * 128:(j + 1) * 128], w[:, kc * 16:(kc + 1) * 16],
                       kc == 0, kc == 15, reads=[B_hT, bw], writes=[B_pb[bank]])
                op("act", lambda e, bank=bank, j=j: e.activation(iw[:, j, :], pb[bank][:, 0:16], AF.Copy),
                   reads=[B_pb[bank]], writes=[B_iw])

            def qkv_block(ti=ti, t0=t0):
                for c in range(10):
                    isq = c < 8
                    bank = proj_fm((CH_Q + c) if isq else (CH_K + c - 8), 16, hT_rhs, [B_hT])
                    op("act", lambda e, bank=bank: e.activation(sqt, pb[bank][:, 0:T], AF.Square),
                       reads=[B_pb[bank]], writes=[B_sqt])
                    mm(pb[2][:, 0:T], onesb[:, :], sqt, True, True, reads=[B_cst, B_sqt], writes=[B_pb[2]])
                    op("act", lambda e: e.activation(rtq, pb[2][:, 0:T], AF.Ln, bias=epst[:, 0:1], scale=1.0 / 128),
                       reads=[B_pb[2], B_cst], writes=[B_rtq])
                    op("act", lambda e: e.activation(rtq, rtq, AF.Exp, scale=-0.5), reads=[B_rtq], writes=[B_rtq])
                    if isq:
                        dst = qT[:, c, :]; bd = B_qT; gcol = 0
                    else:
                        dst = kT[:, c - 8, t0:t0 + T]; bd = B_kT; gcol = 1
                    qk = c % 2
                    op("act", lambda e, bank=bank, gcol=gcol, qk=qk: e.activation(qsb[qk], pb[bank][:, 0:T], AF.Copy,
                                                                                 scale=gqs[:, gcol:gcol + 1]),
                       reads=[B_pb[bank], B_gqs], writes=[B_qsb[qk]])
                    op("pool", lambda e, dst=dst, qk=qk: e.tensor_tensor(dst, qsb[qk], rtq, ALU.mult),
                       reads=[B_qsb[qk], B_rtq], writes=[bd])
                for g in range(2):
                    w, bw = load_chunk(CH_V + g)
                    for j in range(NSUB):
                        bank = next_bank01()
                        for kc in range(16):
                            mm(pb[bank][:, 0:128], hT[:, kc, j * 128:(j + 1) * 128], w[:, kc * 128:(kc + 1) * 128],
                               kc == 0, kc == 15, reads=[B_hT, bw], writes=[B_pb[bank]])
                        op("act", lambda e, bank=bank, g=g, j=j: e.activation(vaug[:, ti * NSUB + j, g, 0:128], pb[bank][:, 0:128], AF.Copy),
                           reads=[B_pb[bank]], writes=[B_v])
            if ti == 0:
                dump("iw", iw[:, :, :], [B_iw])

            def indexer(j, ti=ti):
                qi = ti * NSUB + j; Lk = (qi + 1) * 128
                qs = slice(j * 128, (j + 1) * 128)
                use_mask = qi >= 2
                for ih in range(16):
                    c = ih // 2; p0 = (ih % 2) * 64
                    for s0 in range(0, Lk, 512):
                        n = min(512, Lk - s0)
                        bank = next_bank01(); r = (ih + s0 // 512) % 2
                        mm(pb[bank][:, 0:n], iqT[p0:p0 + 64, c, qs], ikT[p0:p0 + 64, s0:s0 + n], True, True,
                           reads=[B_iqT, B_ik], writes=[B_pb[bank]])
                        op("act", lambda e, bank=bank, r=r, n=n: e.activation(rl[r][:, 0:n], pb[bank][:, 0:n], AF.Relu),
                           reads=[B_pb[bank]], writes=[B_rl[r]])
                        last = (s0 + n == Lk)
                        if ih == 0:
                            nb = n - 128 if last else n
                            if nb > 0:
                                op("dve", lambda e, r=r, s0=s0, nb=nb, j=j: e.tensor_scalar(
                                    acc[:, s0:s0 + nb], rl[r][:, 0:nb], iw[:, j, 0:1], None, ALU.mult),
                                   reads=[B_rl[r], B_iw], writes=[B_acc])
                            if last:
                                op("dve", lambda e, r=r, s0=s0, n=n, j=j: e.scalar_tensor_tensor(
                                    acc[:, s0 + n - 128:s0 + n], rl[r][:, n - 128:n], iw[:, j, 0:1], cmask[:, :],
                                    ALU.mult, ALU.add), reads=[B_rl[r], B_iw, B_cst], writes=[B_acc])
                        else:
                            op("dve", lambda e, r=r, s0=s0, n=n, j=j, ih=ih: e.scalar_tensor_tensor(
                                acc[:, s0:s0 + n], rl[r][:, 0:n], iw[:, j, ih:ih + 1], acc[:, s0:s0 + n],
                                ALU.mult, ALU.add), reads=[B_rl[r], B_iw, B_acc], writes=[B_acc])
                if qi == 2:
                    dump("acc", acc[:, 0:384], [B_acc])
            def topk(j, ti=ti):
                qi = ti * NSUB + j; Lk = (qi + 1) * 128
                qs = slice(j * 128, (j + 1) * 128)
                use_mask = qi >= 2
                if use_mask:
                    KB = 24
                    op("dve", lambda e: e.max(out=m8, in_=acc[:, 0:Lk]), reads=[B_acc], writes=[B_m8])
                    op("dve", lambda e: e.tensor_reduce(out=bs[:, 4:5], in_=acc[:, 0:Lk - 128], axis=AX.X, op=ALU.min),
                       reads=[B_acc], writes=[B_bs])
                    op("dve", lambda e: e.tensor_tensor(bs[:, 5:6], m8[:, 0:1], bs[:, 4:5], ALU.subtract), reads=[B_m8, B_bs], writes=[B_bs])
                    op("dve", lambda e: e.tensor_scalar(Wt[:, :], pow2[:, :], bs[:, 5:6], None, ALU.mult), reads=[B_cst, B_bs], writes=[B_Wt])
                    op("dve", lambda e: e.tensor_scalar(W2t[:, :], Wt[:, :], 2.0, None, ALU.mult), reads=[B_Wt], writes=[B_W2t])
                    op("dve", lambda e: e.tensor_tensor(bs[:, 0:1], bs[:, 4:5], Wt[:, 0:1], ALU.add), reads=[B_bs, B_Wt], writes=[B_bs])
                    for kb in range(KB):
                        op("dve", lambda e: e.tensor_scalar(junk8[:, 0:Lk], acc[:, 0:Lk], bs[:, 0:1], None, ALU.is_ge, ALU.add,
                                                            accum_out=bs[:, 1:2]), reads=[B_acc, B_bs], writes=[B_junk8, B_bs])
                        op("dve", lambda e, kb=kb: e.tensor_scalar(bs[:, 2:3], bs[:, 1:2], 256.0, W2t[:, kb + 1:kb + 2], ALU.is_ge, ALU.mult),
                           reads=[B_bs, B_W2t], writes=[B_bs])
                        op("dve", lambda e, kb=kb: e.scalar_tensor_tensor(bs[:, 0:1], bs[:, 2:3], Wt[:, kb + 1:kb + 2], bs[:, 0:1],
                                                                          ALU.subtract, ALU.add), reads=[B_bs, B_Wt], writes=[B_bs])
                    op("dve", lambda e: e.tensor_tensor(bs[:, 3:4], bs[:, 0:1], Wt[:, KB:KB + 1], ALU.subtract), reads=[B_bs, B_Wt], writes=[B_bs])
                    op("dve", lambda e: e.tensor_scalar(mneg[:, 0:Lk], acc[:, 0:Lk], bs[:, 3:4], -1.0, ALU.is_lt, ALU.mult),
                       reads=[B_acc, B_bs], writes=[B_mneg])
                    if qi == 2:
                        dump("mneg", mneg[:, 0:384], [B_mneg])
            def attention(j, ti=ti):
                qi = ti * NSUB + j; Lk = (qi + 1) * 128
                qs = slice(j * 128, (j + 1) * 128)
                use_mask = qi >= 2
                for h in range(8):
                    g = h // 4
                    pvb = 6 + (h % 2)
                    for grp in range(0, qi + 1, 4):
                        scs = list(range(grp, min(grp + 4, qi + 1)))
                        lbk = 4 + ((grp // 4) % 2); pr = (grp // 4) % 2
                        for sc_ in scs:
                            col = (sc_ - grp) * 128
                            o = pb[lbk][:, col:col + 128]
                            extra = (1 if use_mask else 0) + (2 if sc_ >= qi - 1 else 0)
                            mm(o, kT[:, g, sc_ * 128:(sc_ + 1) * 128], qT[:, h, qs], True, extra == 0,
                               reads=[B_kT, B_qT], writes=[B_pb[lbk]])
                            if use_mask:
                                extra -= 1
                                mm(o, mneg[:, sc_ * 128:(sc_ + 1) * 128], identbig[:, :], False, extra == 0,
                                   reads=[B_mneg, B_cst], writes=[B_pb[lbk]])
                            if sc_ >= qi - 1:
                                dl = qi - sc_
                                mm(o, biasT[:, h, dl, 0, :], identb[:, :], False, False, reads=[B_bias, B_cst], writes=[B_pb[lbk]])
                                mm(o, biasT[:, h, dl, 1, :], identb[:, :], False, True, reads=[B_bias, B_cst], writes=[B_pb[lbk]])
                        ncol = len(scs) * 128
                        op("act", lambda e, lbk=lbk, pr=pr, ncol=ncol: e.activation(pT[pr][:, 0:ncol], pb[lbk][:, 0:ncol], AF.Exp),
                           reads=[B_pb[lbk]], writes=[B_pT[pr]])
                        for sc_ in scs:
                            col = (sc_ - grp) * 128
                            mm(pb[pvb][:, 0:129], pT[pr][:, col:col + 128], vaug[:, sc_, g, 0:129], sc_ == 0, sc_ == qi,
                               reads=[B_pT[pr], B_v], writes=[B_pb[pvb]])
                    ra = h % 2
                    op("act", lambda e, pvb=pvb, ra=ra: e.activation(rdn[:, ra:ra + 1], pb[pvb][:, 128:129], AF.Ln),
                       reads=[B_pb[pvb]], writes=[B_rdn[ra]])
                    op("act", lambda e, ra=ra: e.activation(rdn[:, ra:ra + 1], rdn[:, ra:ra + 1], AF.Exp, scale=-1.0),
                       reads=[B_rdn[ra]], writes=[B_rdn[ra]])
                    op("act", lambda e, pvb=pvb, h=h, ra=ra: e.activation(attn_tm[:, h * 128:(h + 1) * 128], pb[pvb][:, 0:128], AF.Copy,
                                                                          scale=rdn[:, ra:ra + 1]),
                       reads=[B_pb[pvb], B_rdn[ra]], writes=[B_atm])
                pbb = pb[3][:, :].bitcast(BF16)
                for h in range(8):
                    op("pe", lambda e, h=h: e.transpose(pbb[:, h * 128:(h + 1) * 128], attn_tm[:, h * 128:(h + 1) * 128], identb[:, :]),
                       reads=[B_atm, B_cst], writes=[B_pb[3]])
                op("act", lambda e, j=j: e.activation(attnT[:, :, j * 128:(j + 1) * 128], pbb.rearrange("p (k t) -> p k t", k=8), AF.Copy),
                   reads=[B_pb[3]], writes=[B_attnT])

            def pool_block(ti=ti):
                for c in range(8):
                    g = c // 2; wdw = (2, 4, 8, 16)[g]
                    bank = proj_fm(CH_PL + c, 16, hT_rhs, [B_hT])
                    pbuf = plb[c % 2]; bpl = B_plb[c % 2]
                    op("act", lambda e, bank=bank, pbuf=pbuf: e.activation(pbuf[:, 16:16 + T], pb[bank][:, 0:T], AF.Copy),
                       reads=[B_pb[bank]], writes=[bpl])
                    op("pool", lambda e, pbuf=pbuf, c=c: e.tensor_copy(pbuf[:, 0:16], halo[:, c, :]),
                       reads=[B_halo], writes=[bpl])
                    op("pool", lambda e, pbuf=pbuf, c=c: e.tensor_copy(halo[:, c, :], pbuf[:, T:T + 16]),
                       reads=[bpl], writes=[B_halo])
                    cur = pbuf; bcur = bpl; k = 1; st = 0; lo = 1
                    while k < wdw:
                        nxt = ptmp[st % 2]; bn = B_ptmp[st % 2]
                        op("pool", lambda e, cur=cur, nxt=nxt, k=k, lo=lo: e.tensor_tensor(
                            nxt[:, lo:16 + T], cur[:, lo:16 + T], cur[:, lo - k:16 + T - k], ALU.add),
                           reads=[bcur], writes=[bn])
                        cur = nxt; bcur = bn; k *= 2; lo += k; st += 1
                    oth = ptmp[st % 2]; both = B_ptmp[st % 2]
                    op("pool", lambda e, cur=cur, oth=oth, wdw=wdw: e.tensor_scalar(oth[:, 16:16 + T], cur[:, 16:16 + T], 1.0 / wdw, 0.0, ALU.mult, ALU.add),
                       reads=[bcur], writes=[both])
                    op("pool", lambda e, oth=oth, pbuf=pbuf, c=c: e.tensor_tensor(praw[:, c, :], oth[:, 16:16 + T], pbuf[:, 16:16 + T], ALU.subtract),
                       reads=[both, bpl], writes=[B_praw])
                    if ti == 0:
                        op("pool", lambda e, cur=cur, oth=oth, g=g: e.tensor_tensor(oth[:, 16:32], cur[:, 16:32], invc[:, g, :], ALU.mult),
                           reads=[bcur, B_cst], writes=[both])
                        op("pool", lambda e, oth=oth, pbuf=pbuf, c=c: e.tensor_tensor(praw[:, c, 0:16], oth[:, 16:32], pbuf[:, 16:32], ALU.subtract),
                           reads=[both, bpl, B_praw], writes=[B_praw])
                w, bw = load_chunk(CH_PW)
                for g in range(4):
                    for eh in range(2):
                        bank = next_bank01()
                        for kc in range(2):
                            lhsT = w[:, g * 512 + kc * 256 + eh * 128: g * 512 + kc * 256 + eh * 128 + 128]
                            mm(pb[bank][:, 0:T], lhsT, praw[:, 2 * g + kc, :], kc == 0, kc == 1, reads=[bw, B_praw], writes=[B_pb[bank]])
                        op("act", lambda e, bank=bank, g=g, eh=eh: e.activation(pooledT[:, 2 * g + eh, :], pb[bank][:, 0:T], AF.Copy,
                                                                                scale=pscale[:, 2 * g + eh:2 * g + eh + 1]),
                           reads=[B_pb[bank], B_cst], writes=[B_pooled])
                if ti == 0:
                    dump("pooledT", pooledT[:, :, :], [B_pooled])


            def gates(c_lo, c_hi):
                for c in range(c_lo, c_hi):
                    bank = proj_fm(CH_GA + c, 16, hT_rhs, [B_hT])
                    op("act", lambda e, bank=bank, c=c: e.activation(sga[:, c, :], pb[bank][:, 0:T], AF.Sigmoid),
                       reads=[B_pb[bank]], writes=[B_sga])
                    bank = proj_fm(CH_GP + c, 16, hT_rhs, [B_hT])
                    op("act", lambda e, bank=bank, c=c: e.activation(sgp[:, c, :], pb[bank][:, 0:T], AF.Sigmoid),
                       reads=[B_pb[bank]], writes=[B_sgp])

            indexer(0); qkv_block(); pool_block(); gates(0, 8); topk(0)
            if ti == 0:
                dump("qT", qT[:, :, :], [B_qT]); dump("kT", kT[:, :, 0:T], [B_kT])
            for j in range(1, NSUB):
                indexer(j); attention(j - 1)
                if j == 1:
                    gates(8, 16)
                topk(j)
            attention(NSUB - 1)
            if ti == 0:
                dump("attnT", attnT[:, :, :], [B_attnT])

            sc.barrier()
            wab = wpbk = None
            for c in range(16):
                if c % 2 == 0:
                    wab = load_chunk(CH_AB + c // 2); wpbk = load_chunk(CH_PB + c // 2)
                col0 = (c % 2) * 128
                for kc in range(8):
                    mm(pb[4][:, 0:T], wab[0][:, kc * 256 + col0:kc * 256 + col0 + 128], attnT[:, kc, :], kc == 0, kc == 7,
                       reads=[wab[1], B_attnT], writes=[B_pb[4]])
                for kc in range(8):
                    mm(pb[5][:, 0:T], wpbk[0][:, kc * 256 + col0:kc * 256 + col0 + 128], pooledT[:, kc, :], kc == 0, kc == 7,
                       reads=[wpbk[1], B_pooled], writes=[B_pb[5]])
                sa = (c % 2) * 2
                op("dve", lambda e, sa=sa, c=c: e.tensor_tensor(sg[sa], sga[:, c, :], pb[4][:, 0:T], ALU.mult),
                   reads=[B_sga, B_pb[4]], writes=[B_sg[sa]])
                op("dve", lambda e, sa=sa, c=c: e.tensor_tensor(sg[sa + 1], sgp[:, c, :], pb[5][:, 0:T], ALU.mult),
                   reads=[B_sgp, B_pb[5]], writes=[B_sg[sa + 1]])
                op("dve", lambda e, sa=sa, c=c: e.tensor_tensor(mergedT[:, c, :], sg[sa], sg[sa + 1], ALU.add),
                   reads=[B_sg[sa], B_sg[sa + 1]], writes=[B_mrg])
            if ti == 0:
                dump("mergedT", mergedT[:, :, :], [B_mrg])

            load_bc(0, MV_GT1)
            for cc in range(16):
                w, bw = load_chunk(CH_WO + cc)
                bank = next_bank01()
                for j in range(NSUB):
                    for kc in range(16):
                        mm(pb[bank][:, j * 128:(j + 1) * 128], mergedT[:, kc, j * 128:(j + 1) * 128], w[:, kc * 128:(kc + 1) * 128],
                           kc == 0, kc == 15, reads=[B_mrg, bw], writes=[B_pb[bank]])
                gt = AP(tensor=arena, offset=cc * 128,
                        ap=[[ARENA // 4, 128], [0, NSUB], [1, 128]])
                op("dve", lambda e, bank=bank, gt=gt: e.tensor_tensor(
                    rtmp[:, :, :], pb[bank][:, 0:T].rearrange("p (j c) -> p j c", j=NSUB), gt, ALU.mult),
                   reads=[B_pb[bank], B_bcA], writes=[B_rtmp])
                op("dve", lambda e, cc=cc: e.tensor_tensor(xres[:, :, cc * 128:(cc + 1) * 128], xres[:, :, cc * 128:(cc + 1) * 128],
                                                           rtmp[:, :, :], ALU.add), reads=[B_rtmp] + B_x, writes=B_x)
        if ti == NT - 1:
            dump("x1", xres[:, :, :], B_x)
        sc.barrier()

        if do_peer:
            load_bc(0, MV_S2); load_bc(1, MV_SH2)
            for j in range(NSUB):
                rmsnorm_to_hT(j, MV_S2, MV_SH2, False)
            for c in range(16):
                bank = proj_fm(CH_PQ + c, 16, hT_rhs, [B_hT])
                op("act", lambda e, bank=bank, c=c: e.activation(qpT[:, c, :], pb[bank][:, 0:T], AF.Copy),
                   reads=[B_pb[bank]], writes=[B_qpT])
            wk, bwk = load_chunk(CH_KEYS)
            deferred = []; defer_on = [False]

            def dop(en, fn, reads=(), writes=()):
                if defer_on[0]:
                    deferred.append((en, fn, list(reads), list(writes)))
                else:
                    op(en, fn, reads=reads, writes=writes)

            def flush(n):
                while deferred and n > 0:
                    en, fn, r_, w_ = deferred.pop(0)
                    op(en, fn, reads=r_, writes=w_); n -= 1
            for j in range(NSUB):
                for hp in range(16):
                    bank = 4 + hp // 4
                    mm(pb[bank][:, (hp % 4) * 128:(hp % 4 + 1) * 128], qpT[:, hp, j * 128:(j + 1) * 128],
                       wk[:, hp * 128:(hp + 1) * 128], True, True, reads=[B_qpT, bwk], writes=[B_pb[bank]])
                for b4 in range(4):
                    op("act", lambda e, b4=b4: e.activation(sub[:, b4 * 4:(b4 + 1) * 4, :],
                                                            pb[4 + b4][:, :].rearrange("p (a n) -> p a n", a=4), AF.Copy),
                       reads=[B_pb[4 + b4]], writes=[B_sub])
                defer_on[0] = (NSUB > 1 and j == NSUB - 1)
                for hp in range(16):
                    sv = sub[:, hp, :]
                    dop("dve", lambda e, sv=sv, hp=hp: e.max(out=mtop[:, hp, 0:8], in_=sv), reads=[B_sub], writes=[B_mtop])
                    dop("dve", lambda e, sv=sv, hp=hp: e.max_index(out=ixu[:, hp, 0:8], in_max=mtop[:, hp, 0:8], in_values=sv),
                       reads=[B_sub, B_mtop], writes=[B_ixu])
                    dop("dve", lambda e, sv=sv, hp=hp: e.match_replace(out=sv, in_to_replace=mtop[:, hp, 0:8], in_values=sv, imm_value=-1.0e30),
                       reads=[B_sub, B_mtop], writes=[B_sub])
                    dop("dve", lambda e, sv=sv, hp=hp: e.max(out=mtop[:, hp, 8:16], in_=sv), reads=[B_sub], writes=[B_mtop])
                    dop("dve", lambda e, sv=sv, hp=hp: e.max_index(out=ixu[:, hp, 8:16], in_max=mtop[:, hp, 8:16], in_values=sv),
                       reads=[B_sub, B_mtop], writes=[B_ixu])
                dop("dve", lambda e: e.tensor_copy(ixf[:, :, :], ixu[:, :, :]), reads=[B_ixu], writes=[B_ixf])
                mt_t = mtop.tensor; mt_off = mtop.offset; PST = ARENA // 4
                s1b = AP(tensor=mt_t, offset=mt_off, ap=[[PST, 128], [32, 8], [1, 16], [0, 16]])
                s2b = AP(tensor=mt_t, offset=mt_off + 16, ap=[[PST, 128], [32, 8], [0, 16], [1, 16]])
                candv = cand[:, :, :].rearrange("p h (a b) -> p h a b", a=16)
                dop("dve", lambda e: e.tensor_tensor(candv, s1b, s2b, ALU.add), reads=[B_mtop], writes=[B_cand])
                for h in range(8):
                    cv = cand[:, h, :]
                    dop("dve", lambda e, cv=cv, h=h: e.max(out=best[:, h, 0:8], in_=cv), reads=[B_cand], writes=[B_best])
                    dop("dve", lambda e, cv=cv, h=h: e.max_index(out=posu[:, h, 0:8], in_max=best[:, h, 0:8], in_values=cv),
                       reads=[B_cand, B_best], writes=[B_posu])
                    dop("dve", lambda e, cv=cv, h=h: e.match_replace(out=cv, in_to_replace=best[:, h, 0:8], in_values=cv, imm_value=-1.0e30),
                       reads=[B_cand, B_best], writes=[B_cand])
                    dop("dve", lambda e, cv=cv, h=h: e.max(out=best[:, h, 8:16], in_=cv), reads=[B_cand], writes=[B_best])
                    dop("dve", lambda e, cv=cv, h=h: e.max_index(out=posu[:, h, 8:16], in_max=best[:, h, 8:16], in_values=cv),
                       reads=[B_cand, B_best], writes=[B_posu])
                bt_t = best.tensor; bt_off = best.offset
                b0 = AP(tensor=bt_t, offset=bt_off, ap=[[PST, 128], [16, 8], [0, 16]])
                gj = gate[j]
                dop("dve", lambda e, gj=gj: e.tensor_tensor(gj[:, :, :], best[:, :, :], b0, ALU.subtract), reads=[B_best], writes=[B_gate[j]])
                dop("act", lambda e, gj=gj: e.activation(gj[:, :, :], gj[:, :, :], AF.Exp), reads=[B_gate[j]], writes=[B_gate[j]])
                dop("dve", lambda e, gj=gj: e.tensor_reduce(out=gsum[:, :], in_=gj[:, :, :], axis=AX.X, op=ALU.add),
                   reads=[B_gate[j]], writes=[B_gsum])
                dop("dve", lambda e: e.reciprocal(gsum[:, :], gsum[:, :]), reads=[B_gsum], writes=[B_gsum])
                gs_b = AP(tensor=gsum.tensor, offset=gsum.offset, ap=[[PST, 128], [1, 8], [0, 16]])
                dop("dve", lambda e, gj=gj: e.tensor_tensor(gj[:, :, :], gj[:, :, :], gs_b, ALU.mult), reads=[B_gate[j], B_gsum], writes=[B_gate[j]])
                posf = posu[:, :, :].rearrange("p h r -> p (h r)")
                dop("dve", lambda e: e.tensor_single_scalar(pa_u[:, :], posf, 4, ALU.logical_shift_right), reads=[B_posu], writes=[B_pau])
                dop("dve", lambda e: e.tensor_single_scalar(pb_u[:, :], posf, 15, ALU.bitwise_and), reads=[B_posu], writes=[B_pbu])
                dop("dve", lambda e: e.tensor_copy(pa_f[:, :], pa_u[:, :]), reads=[B_pau], writes=[B_paf])
                dop("dve", lambda e: e.tensor_copy(pb_f[:, :], pb_u[:, :]), reads=[B_pbu], writes=[B_pbf])
                io_b = iota16[:, :].unsqueeze(1).to_broadcast([128, 128, 16])
                for (pf, Bpf, half, dst, Bdst) in ((pa_f, B_paf, 0, ia, B_ia), (pb_f, B_pbf, 1, ib, B_ib)):
                    pfb = pf[:, :].unsqueeze(2).to_broadcast([128, 128, 16])
                    dop("dve", lambda e, pfb=pfb: e.tensor_tensor(ohb[:, :, :], pfb, io_b, ALU.is_equal), reads=[Bpf, B_cst], writes=[B_ohb])
                    ixb = AP(tensor=ixf.tensor, offset=ixf.offset + 16 * half, ap=[[PST, 128], [32, 8], [0, 16], [1, 16]])
                    oh4 = ohb[:, :, :].rearrange("p (h r) a -> p h r a", h=8)
                    dop("dve", lambda e, oh4=oh4, ixb=ixb: e.tensor_tensor(oh4, oh4, ixb, ALU.mult), reads=[B_ohb, B_ixf], writes=[B_ohb])
                    dop("dve", lambda e, dst=dst: e.tensor_reduce(out=dst[:, :], in_=ohb[:, :, :], axis=AX.X, op=ALU.add),
                       reads=[B_ohb], writes=[Bdst])
                dop("dve", lambda e: e.scalar_tensor_tensor(eidf[:, :], ia[:, :], 128.0, ib[:, :], ALU.mult, ALU.add),
                   reads=[B_ia, B_ib], writes=[B_eidf])
                dop("dve", lambda e, j=j: e.tensor_copy(eid[j][:, :], eidf[:, :]), reads=[B_eidf], writes=[B_eid[j]])
                defer_on[0] = False
                if ti == 0 and j == 0:
                    dump("eid", eid[0][:, :], [B_eid[0]]); dump("gate", gate[0][:, :, :], [B_gate[0]])

            load_bc(1, MV_GT2)
            GS = 2; NGRP = 128 // GS
            glist = [(j, g) for j in range(NSUB) for g in range(NGRP)]
            kof = {}
            gctr = 0

            def stage_A(idx):
                j, g = glist[idx]; par = idx % NPAR
                nonlocal_k = []
                for i in range(GS):
                    slot = g * GS + i
                    k = (idx * GS + i) % NG
                    nonlocal_k.append(k)
                    dma("pool", gb[k], uv_h.ap(), reads=[B_eid[j], B_uv], writes=B_gb[k],
                        indirect=bass.IndirectOffsetOnAxis(ap=eid[j][:, slot:slot + 1], axis=0))
                    pk = (idx * GS + i) % 2
                    op("dve", lambda e, k=k, j=j, pk=pk: e.tensor_tensor(prod[pk][:, :], hTM[:, j, :], gb[k][:, 0:D], ALU.mult),
                       reads=[B_hTM] + B_gb[k], writes=[B_prod[pk]])
                    op("act", lambda e, i=i, par=par, pk=pk: e.activation(prod[pk][:, :], prod[pk][:, :], AF.Copy,
                                                                          accum_out=dotg[par][:, i:i + 1]),
                       reads=[B_prod[pk]], writes=[B_prod[pk], B_dotg[par]])
                kof[idx] = nonlocal_k
                op("act", lambda e, par=par: e.activation(gact[par][:, :], dotg[par][:, :], AF.Gelu),
                   reads=[B_dotg[par]], writes=[B_gact[par]])

            def stage_C(idx):
                j, g = glist[idx]; par = idx % NPAR
                gflat = gate[j][:, :, :].rearrange("p h r -> p (h r)")
                for i in range(GS):
                    slot = g * GS + i; k = kof[idx][i]
                    op("dve", lambda e, i=i, par=par, slot=slot, gflat=gflat: e.tensor_scalar(
                        dg[par][:, i, :], identb[:, :], gact[par][:, i:i + 1], gflat[:, slot:slot + 1], ALU.mult, ALU.mult),
                       reads=[B_cst, B_gact[par], B_gate[j]], writes=[B_dg[par][i]])
                    for q4 in range(4):
                        mm(pb[q4][:, :], dg[par][:, i, :], gb[k][:, D + q4 * 512:D + (q4 + 1) * 512], slot == 0, slot == 127,
                           reads=[B_dg[par][i]] + B_gb[k], writes=[B_pb[q4]])
                if g == NGRP - 1:
                    for q4 in range(4):
                        op("dve", lambda e, q4=q4: e.tensor_tensor(rt2[:, :], pb[q4][:, :], arena[:, D + q4 * 512:D + (q4 + 1) * 512], ALU.mult),
                           reads=[B_pb[q4], B_bcB], writes=[B_rt2])
                        op("dve", lambda e, q4=q4, j=j: e.tensor_tensor(xres[:, j, q4 * 512:(q4 + 1) * 512], xres[:, j, q4 * 512:(q4 + 1) * 512],
                                                                        rt2[:, :], ALU.add), reads=[B_x[j], B_rt2], writes=[B_x[j]])

            for idx in range(len(glist) + 1):
                if idx < len(glist):
                    if glist[idx][0] > 0:
                        flush(10 ** 9)
                    stage_A(idx)
                if idx >= 1:
                    stage_C(idx - 1)
                flush(3)
            flush(10 ** 9)
        for j in range(NSUB):
            dma("sp", out_h[t0 + j * 128:t0 + (j + 1) * 128, :], xres[:, j, :], reads=[B_x[j]])
        sc.barrier()

    sc.drain_dmas("sp")
    return nc, stack


def _noop():
    pass


_CACHE = {}


def make_in_maps(inputs):
    cst = host_consts()
    f = lambda a: np.ascontiguousarray(np.asarray(a, dtype=np.float32))
    x = f(inputs["x"]); c = f(inputs["c"])
    shared = {
        "w_ada": f(inputs["w_ada"][0]), "b_ada": f(inputs["b_ada"][0]).reshape(1, -1),
        "g1": f(inputs["g_norm1"][0]).reshape(1, -1), "g2": f(inputs["g_norm2"][0]).reshape(1, -1),
        "w_in": f(inputs["w_in"][0]), "gq": f(inputs["g_q"][0]).reshape(128, 1), "gk": f(inputs["g_k"][0]).reshape(128, 1),
        "rel_bias": f(inputs["rel_bias"]), "pool_w": f(inputs["pool_w"][0]),
        "pscale": np.ascontiguousarray(f(inputs["pool_scale"][0]).reshape(8, 128).T),
        "w_attn_br": f(inputs["w_attn_br"][0]), "w_pool_br": f(inputs["w_pool_br"][0]),
        "w_out": f(inputs["w_out"][0]), "w_peer_q": f(inputs["w_peer_q"][0]),
        "peer_keys": f(inputs["peer_keys"][0]).reshape(16, 128, 128),
        "peer_u": f(inputs["peer_u"][0]), "peer_v": f(inputs["peer_v"][0]),
    }
    shared.update(cst)
    maps = []
    for b in range(x.shape[0]):
        m = dict(shared)
        m["x"] = x[b]
        m["cT"] = np.ascontiguousarray(c[b].reshape(16, 128).T)
        maps.append(m)
    return maps


def kernel(**inputs):
    nc, stack = build()
    maps = make_in_maps(inputs)
    res = run_bass_kernel_spmd(nc, maps, core_ids=list(range(8)))
    out = np.stack([np.asarray(r["out"], dtype=np.float32) for r in res.results], axis=0)
    return out
```

```python
import math
from contextlib import ExitStack
import numpy as np
import ml_dtypes
import concourse.bass as bass
import concourse.mybir as mybir
from concourse.bass_utils import run_bass_kernel_spmd

F32 = mybir.dt.float32; BF16 = mybir.dt.bfloat16; I32 = mybir.dt.int32; U32 = mybir.dt.uint32
ALU = mybir.AluOpType; AF = mybir.ActivationFunctionType; AX = mybir.AxisListType

D = 2048; S = 4096; T = 256; NSUB = T // 128; NTILES = S // T
INW = 7760
C_Q, C_K, C_V, C_IQ, C_IK, C_IW, C_PL, C_GA, C_GP = 0, 1024, 1280, 1536, 2560, 2624, 2640, 3664, 5712
CH_Q = 0; CH_K = 8; CH_V = 10; CH_IQ = 12; CH_IK = 20; CH_IW = 21; CH_PL = 22; CH_GA = 30; CH_GP = 46
CH_AB = 62; CH_PB = 70; CH_PW = 78; CH_WO = 79; CH_PQ = 95; CH_KEYS = 111; NCH = 112
EPS = 1e-6
EPOCH = 20000
NDS = 8
NEGBIG = -3.0e38


class Buf:
    __slots__ = ("name", "lw", "rd")

    def __init__(self, name):
        self.name = name; self.lw = None; self.rd = {}


class Eng:
    def __init__(self, idx, name, h):
        self.idx = idx; self.name = name; self.h = h; self.seq = 0; self.sems = []; self.waited = {}
        self.dcount = 0; self.dvals = [0] * NDS; self.dsems = None


class Sched:
    def __init__(self, nc, stack):
        self.nc = nc; self.stack = stack; self.E = {}
        for i, (n, h) in enumerate([("pe", nc.tensor), ("act", nc.scalar), ("dve", nc.vector),
                                    ("pool", nc.gpsimd), ("sp", nc.sync)]):
            self.E[n] = Eng(i, n, h)
        self.elist = list(self.E.values()); self.dsem_list = []

    def _esem(self, E, ep):
        while len(E.sems) <= ep:
            E.sems.append(self.stack.enter_context(self.nc.semaphore(f"e_{E.name}_{len(E.sems)}")))
        return E.sems[ep]

    def _wait(self, E, ev):
        kind, k, v = ev
        key = (kind, k)
        if E.waited.get(key, 0) >= v:
            return
        E.waited[key] = v
        if kind == "e":
            E2 = self.elist[k]; ep, val = divmod(v - 1, EPOCH)
            E.h.wait_ge(self._esem(E2, ep), val + 1)
        else:
            E.h.wait_ge(self.dsem_list[k], v)

    def op(self, en, fn, reads=(), writes=()):
        E = self.E[en]; deps = []
        for r in reads:
            if r.lw is not None and not (en == "pe" and r.lw[0] == "e" and r.lw[1] == E.idx):
                deps.append(r.lw)
        pe = en == "pe"
        for w in writes:
            if w.lw is not None and not (pe and w.lw[0] == "e" and w.lw[1] == E.idx):
                deps.append(w.lw)
            for ev in w.rd.values():
                if not (pe and ev[0] == "e" and ev[1] == E.idx):
                    deps.append(ev)
        for ev in deps:
            self._wait(E, ev)
        inst = fn(E.h)
        E.seq += 1; ep, _ = divmod(E.seq - 1, EPOCH)
        inst.then_inc(self._esem(E, ep), 1)
        ev = ("e", E.idx, E.seq)
        for r in reads:
            r.rd[("e", E.idx)] = ev
        for w in writes:
            w.lw = ev; w.rd = {}
        return inst

    def dma(self, qn, out, in_, reads=(), writes=(), indirect=None, **kw):
        Q = self.E[qn]
        if Q.dsems is None:
            Q.dsems = []
            for i in range(NDS):
                sem = self.stack.enter_context(self.nc.semaphore(f"d_{qn}_{i}"))
                Q.dsems.append(len(self.dsem_list)); self.dsem_list.append(sem)
        slot = Q.dcount % NDS; Q.dcount += 1
        k = Q.dsems[slot]; pv = Q.dvals[slot]
        if pv > 0:
            self._wait(Q, ("d", k, pv))
        deps = []
        for r in reads:
            if r.lw is not None:
                deps.append(r.lw)
        for w in writes:
            if w.lw is not None:
                deps.append(w.lw)
            deps.extend(w.rd.values())
        for ev in deps:
            self._wait(Q, ev)
        if indirect is not None:
            inst = Q.h.indirect_dma_start(out=out, out_offset=None, in_=in_, in_offset=indirect)
        else:
            inst = Q.h.dma_start(out=out, in_=in_, **kw)
        nv = pv + 16; Q.dvals[slot] = nv
        inst.then_inc(self.dsem_list[k], 16)
        ev = ("d", k, nv)
        for r in reads:
            r.rd[("d", k)] = ev
        for w in writes:
            w.lw = ev; w.rd = {}

    def barrier(self):
        evs = [("e", E.idx, E.seq) for E in self.elist if E.seq > 0]
        for Q in self.elist:
            if Q.dsems is not None:
                for slot in range(NDS):
                    if Q.dvals[slot] > 0:
                        evs.append(("d", Q.dsems[slot], Q.dvals[slot]))
        for E in self.elist:
            for ev in evs:
                if not (ev[0] == "e" and ev[1] == E.idx):
                    self._wait(E, ev)

    def drain_dmas(self, en="sp"):
        E = self.E[en]
        for Q in self.elist:
            if Q.dsems is not None:
                for slot in range(NDS):
                    if Q.dvals[slot] > 0:
                        self._wait(E, ("d", Q.dsems[slot], Q.dvals[slot]))


def t5_bucket_np(n):
    n = np.asarray(n)
    nf = np.maximum(n, 1).astype(np.float32)
    large = 16 + (np.log(nf / np.float32(16)) / np.float32(math.log(8.0)) * np.float32(16)).astype(np.int32)
    large = np.minimum(large, 31)
    return np.where(n < 16, n, large)


def host_consts():
    c = {}
    eye = np.eye(128, dtype=np.float32)
    c["identb"] = eye.astype(ml_dtypes.bfloat16)
    c["identbig"] = (eye * 32768.0).astype(ml_dtypes.bfloat16)
    c["identf"] = eye
    c["antif"] = np.ascontiguousarray(eye[::-1])
    q = np.arange(128)[:, None]; s = np.arange(128)[None, :]
    c["cmask"] = np.where(s <= q, 0.0, -1.0e30).astype(np.float32)
    c["onesb"] = np.ones((128, 128), dtype=ml_dtypes.bfloat16)
    oh = np.zeros((33, 384), dtype=np.float32)
    for j in range(383):
        dist = j - 127
        if dist >= 0:
            oh[int(t5_bucket_np(dist)), j] = 1.0
        else:
            oh[32, j] = -30000.0
    c["oh2"] = oh
    c["iota16"] = np.tile(np.arange(16, dtype=np.float32)[None, :], (128, 1))
    invc = np.zeros((128, 4, 16), dtype=np.float32)
    for g, w in enumerate((2, 4, 8, 16)):
        for t in range(16):
            invc[:, g, t] = 1.0 / min(t + 1, w)
    c["invc"] = invc
    c["pow2"] = np.tile((2.0 ** -(np.arange(32, dtype=np.float64) + 1)).astype(np.float32)[None, :], (128, 1))
    return c


CONST_SPECS = [("identb", [128, 128], BF16), ("identbig", [128, 128], BF16), ("identf", [128, 128], F32),
               ("antif", [128, 128], F32), ("cmask", [128, 128], F32), ("onesb", [128, 128], BF16),
               ("oh2", [33, 384], F32), ("iota16", [128, 16], F32), ("invc", [128, 4, 16], F32), ("pow2", [128, 32], F32)]


def build(NT=NTILES, dbg=None, do_mix=True, do_peer=True, tile_list=None):
    dbg = dbg or {}
    nc = bass.Bass("TRN2", target_bir_lowering=False)
    stack = ExitStack()
    dt = lambda name, shape, dtype, kind="ExternalInput": nc.dram_tensor(name, shape, dtype, kind=kind)
    x_h = dt("x", [S, D], F32); cT_h = dt("cT", [128, 16], F32)
    wada_h = dt("w_ada", [D, 6 * D], F32); bada_h = dt("b_ada", [1, 6 * D], F32)
    g1_h = dt("g1", [1, D], F32); g2_h = dt("g2", [1, D], F32)
    win_h = dt("w_in", [D, INW], F32); gq_h = dt("gq", [128, 1], F32); gk_h = dt("gk", [128, 1], F32)
    relb_h = dt("rel_bias", [32, 8], F32); poolw_h = dt("pool_w", [4, 256, 256], F32)
    pscale_h = dt("pscale", [128, 8], F32)
    wab_h = dt("w_attn_br", [1024, D], F32); wpb_h = dt("w_pool_br", [1024, D], F32)
    wout_h = dt("w_out", [D, D], F32); wpq_h = dt("w_peer_q", [D, D], F32)
    keys_h = dt("peer_keys", [16, 128, 128], F32)
    pu_h = dt("peer_u", [16384, D], F32); pv_h = dt("peer_v", [16384, D], F32)
    cst_h = {n: dt(n, sh, ty) for n, sh, ty in CONST_SPECS}
    out_h = dt("out", [S, D], F32, kind="ExternalOutput")
    wsc_h = dt("wsc", [NCH, 128, 2048], BF16, kind="Internal")
    modv_h = dt("modv", [1, 6 * D], F32, kind="Internal")
    fd_h = dt("fd", [8, 384], F32, kind="Internal")
    uv_h = dt("uvtab", [16384, 2 * D], BF16, kind="Internal")
    dbg_h = {n: dt("dbg_" + n, sh, ty, kind="ExternalOutput") for n, (sh, ty) in dbg.items()}

    sb = lambda name, shape, dtype: stack.enter_context(nc.sbuf_tensor("s_" + name, shape, dtype))
    ps = lambda name, shape, dtype: stack.enter_context(nc.psum_tensor(name, shape, dtype))
    sc = Sched(nc, stack)
    op = sc.op; dma = sc.dma
    AP = bass.AP

    kT = sb("kT", [128, 2, S], BF16); B_kT = Buf("kT")
    vaug = sb("vaug", [128, 32, 2, 130], BF16); B_v = Buf("vaug")
    ikT = sb("ikT", [128, S], BF16); B_ik = Buf("ikT")
    xres = sb("xres", [128, NSUB, D], F32); B_x = [Buf(f"x{j}") for j in range(NSUB)]
    hTM = sb("hTM", [128, NSUB, D], BF16); B_hTM = Buf("hTM")
    hT = sb("hT", [128, 16, T], BF16); B_hT = Buf("hT")
    NW = 6
    wb_all = sb("wb_all", [128, NW * 2048], BF16)
    wb = [wb_all[:, i * 2048:(i + 1) * 2048] for i in range(NW)]; B_wb = [Buf(f"wb{i}") for i in range(NW)]
    identb = sb("identb", [128, 128], BF16); identbig = sb("identbig", [128, 128], BF16)
    identf = sb("identf", [128, 128], F32); antif = sb("antif", [128, 128], F32)
    cmask = sb("cmask", [128, 128], F32); onesb = sb("onesb", [128, 128], BF16)
    iota16 = sb("iota16", [128, 16], F32); invc = sb("invc", [128, 4, 16], F32); pow2 = sb("pow2", [128, 32], F32)
    B_cst = Buf("cst")
    biasT = sb("biasT", [128, 8, 2, 2, 128], BF16); B_bias = Buf("biasT")
    gqs = sb("gqs", [128, 2], F32); B_gqs = Buf("gqs")
    pscale = sb("pscale", [128, 8], F32)
    epst = sb("epst", [128, 1], F32)
    halo = sb("halo", [128, 8, 16], F32); B_halo = Buf("halo")
    stat = sb("stat", [128, 16], F32); B_stat = Buf("stat")
    ARENA = 96 * 1024
    arena = sb("arena", [128, ARENA // 4], F32)

    def av(off, shape, dtype):
        n = int(np.prod(shape)); esz = 4 if dtype in (F32, I32, U32) else 2
        assert off % 4 == 0 and off + n * esz <= ARENA, (off, shape)
        a = arena[:, off // 4:(off + n * esz) // 4]
        if dtype != F32:
            a = a.bitcast(dtype)
        if len(shape) == 2:
            a = a.rearrange("p (a b) -> p a b", a=shape[0])
        elif len(shape) == 3:
            a = a.rearrange("p (a b c) -> p a b c", a=shape[0], b=shape[1])
        elif len(shape) == 4:
            a = a.rearrange("p (a b c d) -> p a b c d", a=shape[0], b=shape[1], c=shape[2])
        return a

    class Lay:
        def __init__(self):
            self.off = 0

        def take(self, shape, dtype, name):
            n = int(np.prod(shape)); esz = 4 if dtype in (F32, I32, U32) else 2
            v = av(self.off, shape, dtype)
            self.off += (n * esz + 31) // 32 * 32
            return v, Buf(name)

    pb = [ps(f"pb{i}", [128, 512], F32) for i in range(8)]; B_pb = [Buf(f"pb{i}") for i in range(8)]

    wslot_ctr = [0]

    def load_chunk(ch):
        i = wslot_ctr[0] % NW; wslot_ctr[0] += 1
        dma("sp", wb[i], wsc_h[ch], reads=[B_wsc[ch]], writes=[B_wb[i]])
        return wb[i], B_wb[i]

    def mm(out, lhsT, rhs, start, stop, reads, writes):
        return op("pe", lambda e: e.matmul(out, lhsT, rhs, start=start, stop=stop), reads=reads, writes=writes)

    def bcast_row(h, off, n=D, parts=128):
        return AP(tensor=h, offset=off, ap=[[0, parts], [1, n]])

    B_wsc = [Buf(f"wsc{i}") for i in range(NCH)]
    B_modv = Buf("modv"); B_fd = Buf("fd")
    B_dbg = {n: Buf("dbg_" + n) for n in dbg}

    def dump(name, src_ap, src_bufs):
        if name in dbg_h:
            dma("sp", dbg_h[name].ap(), src_ap, reads=src_bufs, writes=[B_dbg[name]])

    for n, t in [("identb", identb), ("identbig", identbig), ("identf", identf), ("antif", antif),
                 ("cmask", cmask), ("onesb", onesb), ("iota16", iota16), ("invc", invc), ("pow2", pow2)]:
        dma("sp", t[:], cst_h[n].ap(), writes=[B_cst])
    dma("sp", pscale[:, :], pscale_h.ap(), writes=[B_cst])
    dma("sp", gqs[:, 0:1], gq_h.ap(), writes=[B_gqs])
    dma("sp", gqs[:, 1:2], gk_h.ap(), writes=[B_gqs])
    op("dve", lambda e: e.memset(epst[:, :], EPS), writes=[B_cst])
    op("dve", lambda e: e.tensor_scalar(gqs[:, 0:1], gqs[:, 0:1], float(128 ** -0.5), None, ALU.mult),
       reads=[B_gqs], writes=[B_gqs])
    op("dve", lambda e: e.memset(halo[:, :, :], 0.0), writes=[B_halo])
    op("pool", lambda e: e.memset(vaug[:, :, :, 128:130], 1.0), writes=[B_v])

    def cast_store(ch, loads):
        i = wslot_ctr[0] % NW; wslot_ctr[0] += 1
        for (o, src) in loads:
            dma("pool", o(wb[i]), src, writes=[B_wb[i]])
        dma("sp", wsc_h[ch], wb[i], reads=[B_wb[i]], writes=[B_wsc[ch]])

    def wsrc(h, ncolsW, c0, nk, ncols):
        return AP(tensor=h, offset=c0, ap=[[ncolsW, 128], [128 * ncolsW, nk], [1, ncols]])

    def v3(nk, ncols, c_lo=0, c_n=None):
        c_n = ncols if c_n is None else c_n
        return lambda w: w[:, 0:nk * ncols].rearrange("p (k c) -> p k c", k=nk)[:, :, c_lo:c_lo + c_n]

    def std_chunks(ch0, h, ncolsW, c0, n):
        for i in range(n):
            cast_store(ch0 + i, [(v3(16, 128), wsrc(h, ncolsW, c0 + i * 128, 16, 128))])

    std_chunks(CH_Q, win_h, INW, C_Q, 8); std_chunks(CH_K, win_h, INW, C_K, 2)
    std_chunks(CH_V, win_h, INW, C_V, 2); std_chunks(CH_IQ, win_h, INW, C_IQ, 8)
    cast_store(CH_IK, [(v3(16, 128, 0, 64), wsrc(win_h, INW, C_IK, 16, 64)),
                       (v3(16, 128, 64, 64), wsrc(win_h, INW, C_IK, 16, 64))])
    cast_store(CH_IW, [(v3(16, 16), wsrc(win_h, INW, C_IW, 16, 16))])
    std_chunks(CH_PL, win_h, INW, C_PL, 8); std_chunks(CH_GA, win_h, INW, C_GA, 16)
    std_chunks(CH_GP, win_h, INW, C_GP, 16)
    for i in range(8):
        cast_store(CH_AB + i, [(v3(8, 256), wsrc(wab_h, D, i * 256, 8, 256))])
        cast_store(CH_PB + i, [(v3(8, 256), wsrc(wpb_h, D, i * 256, 8, 256))])
    cast_store(CH_PW, [((lambda w, g=g: w[:, g * 512:(g + 1) * 512].rearrange("p (k c) -> p k c", k=2)),
                        AP(tensor=poolw_h, offset=g * 65536, ap=[[256, 128], [32768, 2], [1, 256]]))
                       for g in range(4)])
    std_chunks(CH_WO, wout_h, D, 0, 16); std_chunks(CH_PQ, wpq_h, D, 0, 16)

    L = Lay()
    wst = []; B_wst = []
    for i in range(2):
        v, b = L.take([16, 256], BF16, f"wst{i}"); wst.append(v); B_wst.append(b)
    modrow, B_modrow = L.take([1, 256], F32, "modrow")
    oh2v_full, B_oh2 = L.take([384], F32, "oh2")
    cact, B_cact = L.take([16], F32, "cact"); cactb, B_cactb = L.take([16], BF16, "cactb")
    keysn, B_keysn = L.take([16, 128], F32, "keysn")
    rb33, B_rb = L.take([8], F32, "rb33")
    rb31, B_rb31 = L.take([8], F32, "rb31")
    fdsb, B_fdsb = L.take([384], F32, "fdsb")
    hank, B_hank = L.take([2, 128], F32, "hank")
    brow, B_brow = L.take([256], F32, "brow")
    grow, B_grow = L.take([2, 2048], F32, "grow")

    dma("sp", keysn[:, :, :], AP(tensor=keys_h, offset=0, ap=[[128, 128], [16384, 16], [1, 128]]), writes=[B_keysn])
    ki = wslot_ctr[0] % NW; wslot_ctr[0] += 1
    for hp in range(16):
        bank = 2 + (hp % 2)
        op("pe", lambda e, hp=hp, bank=bank: e.transpose(pb[bank][:, 0:128], keysn[:, hp, :], identf[:, :]),
           reads=[B_keysn, B_cst], writes=[B_pb[bank]])
        op("act", lambda e, hp=hp, bank=bank: e.activation(wb[ki][:, hp * 128:(hp + 1) * 128], pb[bank][:, 0:128], AF.Copy),
           reads=[B_pb[bank]], writes=[B_wb[ki]])
    dma("sp", wsc_h[CH_KEYS], wb[ki], reads=[B_wb[ki]], writes=[B_wsc[CH_KEYS]])

    dma("sp", cact[:, :], cT_h.ap(), writes=[B_cact])
    op("act", lambda e: e.activation(cactb[:, :], cact[:, :], AF.Silu), reads=[B_cact], writes=[B_cactb])
    dma("sp", grow[0:1, 0, :], g1_h.ap(), writes=[B_grow])
    dma("sp", grow[0:1, 1, :], g2_h.ap(), writes=[B_grow])
    CGW = 256
    for cg in range(6 * D // CGW):
        i = cg % 2
        dma("pool", wst[i][:, :, :], AP(tensor=wada_h, offset=cg * CGW, ap=[[6 * D, 128], [128 * 6 * D, 16], [1, CGW]]),
            writes=[B_wst[i]])
        dma("sp", brow[0:1, :], AP(tensor=bada_h, offset=cg * CGW, ap=[[0, 1], [1, CGW]]), writes=[B_brow])
        for kc in range(16):
            mm(pb[0][0:1, 0:CGW], cactb[:, kc:kc + 1], wst[i][:, kc, :], kc == 0, kc == 15,
               reads=[B_cactb, B_wst[i]], writes=[B_pb[0]])
        op("dve", lambda e: e.tensor_tensor(modrow[0:1, 0, :], pb[0][0:1, 0:CGW], brow[0:1, :], ALU.add),
           reads=[B_pb[0], B_brow], writes=[B_modrow])
        seg = (cg * CGW) // D
        if seg in (1, 4):
            gsel = 0 if seg == 1 else 1
            cs = (cg * CGW) % D
            op("dve", lambda e, gsel=gsel, cs=cs: e.scalar_tensor_tensor(
                modrow[0:1, 0, :], modrow[0:1, 0, :], 1.0, grow[0:1, gsel, cs:cs + CGW], ALU.add, ALU.mult),
               reads=[B_modrow, B_grow], writes=[B_modrow])
        dma("sp", AP(tensor=modv_h, offset=cg * CGW, ap=[[0, 1], [1, CGW]]), modrow[0:1, 0, :],
            reads=[B_modrow], writes=[B_modv])
    MV_SH1, MV_S1, MV_GT1, MV_SH2, MV_S2, MV_GT2 = [i * D for i in range(6)]

    op("dve", lambda e: e.memset(rb33[0:33, :], 1.0), writes=[B_rb])
    dma("sp", rb33[0:32, :], relb_h.ap(), reads=[], writes=[B_rb])
    dma("sp", rb31[0:32, :], AP(tensor=relb_h, offset=31 * 8, ap=[[0, 32], [1, 8]]), writes=[B_rb31])
    op("dve", lambda e: e.tensor_tensor(rb33[0:32, :], rb33[0:32, :], rb31[0:32, :], ALU.subtract),
       reads=[B_rb, B_rb31], writes=[B_rb])
    oh2v = oh2v_full[0:33, :]
    dma("sp", oh2v, cst_h["oh2"].ap(), writes=[B_oh2])
    mm(pb[1][0:8, 0:384], rb33[0:33, :], oh2v, True, True, reads=[B_rb, B_oh2], writes=[B_pb[1]])
    op("act", lambda e: e.activation(fdsb[0:8, :], pb[1][0:8, 0:384], AF.Copy), reads=[B_pb[1]], writes=[B_fdsb])
    dma("sp", fd_h.ap(), fdsb[0:8, :], reads=[B_fdsb], writes=[B_fd])
    for h in range(8):
        for dl in range(2):
            k = (h * 2 + dl) % 2
            dma("sp", hank[:, k, :], AP(tensor=fd_h, offset=h * 384 + 128 * dl, ap=[[1, 128], [1, 128]]),
                reads=[B_fd], writes=[B_hank])
            bank = 2 + k
            mm(pb[bank][:, 0:128], hank[:, k, :], antif[:, :], True, True, reads=[B_hank, B_cst], writes=[B_pb[bank]])
            op("act", lambda e, h=h, dl=dl, bank=bank: e.activation(biasT[:, h, dl, 0, :], pb[bank][:, 0:128], AF.Copy),
               reads=[B_pb[bank]], writes=[B_bias])
            op("dve", lambda e, h=h, dl=dl, bank=bank: e.tensor_tensor(
                biasT[:, h, dl, 1, :], pb[bank][:, 0:128], biasT[:, h, dl, 0, :], ALU.subtract),
               reads=[B_pb[bank], B_bias], writes=[B_bias])
    dump("biasT", biasT[:, :, :, :, :], [B_bias])
    dump("modv", modv_h.ap(), [B_modv])

    sc.barrier()
    B_uv = Buf("uvtab")
    LU = Lay(); stg = []; B_stg = []
    for i in range(4):
        v_, b_ = LU.take([4, D], BF16, f"stg{i}"); stg.append(v_); B_stg.append(b_)
    uctr = 0
    for blk in range(32):
        for half, th in ((0, pu_h), (1, pv_h)):
            k = uctr % 4; uctr += 1
            dma("pool", stg[k][:, :, :], AP(tensor=th, offset=blk * 512 * D, ap=[[4 * D, 128], [D, 4], [1, D]]),
                writes=[B_stg[k]])
            dma("sp", AP(tensor=uv_h, offset=blk * 512 * 2 * D + half * D, ap=[[4 * 2 * D, 128], [2 * D, 4], [1, D]]),
                stg[k][:, :, :], reads=[B_stg[k]], writes=[B_uv])
    sc.barrier()

    LA = Lay()
    bcA, B_bcA = LA.take([D], F32, "bcA"); bcB, B_bcB = LA.take([D], F32, "bcB")
    qT, B_qT = LA.take([8, T], BF16, "qT"); iqT, B_iqT = LA.take([8, T], BF16, "iqT")
    attnT, B_attnT = LA.take([8, T], BF16, "attnT")
    acc, B_acc = LA.take([S], F32, "acc"); mneg, B_mneg = LA.take([S], BF16, "mneg")
    rl = []; B_rl = []; pT = []; B_pT = []
    for i in range(2):
        v, b = LA.take([512], BF16, f"rl{i}"); rl.append(v); B_rl.append(b)
        v, b = LA.take([512], BF16, f"pT{i}"); pT.append(v); B_pT.append(b)
    attn_tm, B_atm = LA.take([1024], BF16, "attn_tm")
    iw, B_iw = LA.take([NSUB, 16], F32, "iw")
    m8, B_m8 = LA.take([8], F32, "m8")
    sqt, B_sqt = LA.take([T], BF16, "sqt"); rtq, B_rtq = LA.take([T], F32, "rtq")
    junk8, B_junk8 = LA.take([S // 2], BF16, "junk8"); junk8 = junk8.bitcast(mybir.dt.uint8)
    rdn, _ = LA.take([4], F32, "rdn"); B_rdn = [Buf("rdn0"), Buf("rdn1")]
    bs, B_bs = LA.take([8], F32, "bs"); Wt, B_Wt = LA.take([32], F32, "Wt"); W2t, B_W2t = LA.take([32], F32, "W2t")
    plb = []; B_plb = []
    for i in range(2):
        v, b = LA.take([16 + T], F32, f"plb{i}"); plb.append(v); B_plb.append(b)
    ptmp = []; B_ptmp = []
    for i in range(2):
        v, b = LA.take([16 + T], F32, f"ptmp{i}"); ptmp.append(v); B_ptmp.append(b)
    praw, B_praw = LA.take([8, T], BF16, "praw"); pooledT, B_pooled = LA.take([8, T], BF16, "pooledT")
    sga, B_sga = LA.take([16, T], BF16, "sga"); sgp, B_sgp = LA.take([16, T], BF16, "sgp")
    qsb = []; B_qsb = []
    for i in range(2):
        v, b = LA.take([T], BF16, f"qsb{i}"); qsb.append(v); B_qsb.append(b)
    LB = Lay()
    LB.take([D], F32, "bcA"); LB.take([D], F32, "bcB")
    LB.take([8, T], BF16, "qT_"); LB.take([8, T], BF16, "iqT_"); LB.take([8, T], BF16, "attnT_")
    mergedT, B_mrg = LB.take([16, T], BF16, "mergedT")
    sg = []; B_sg = []
    for i in range(4):
        v, b = LB.take([T], F32, f"sg{i}"); sg.append(v); B_sg.append(b)
    rtmp, B_rtmp = LB.take([NSUB, 128], F32, "rtmp")
    assert LB.off <= 2 * 4 * D + 3 * 8 * T * 2 + 4 * S, LB.off
    LP = Lay()
    LP.take([D], F32, "bcA"); LP.take([D], F32, "bcB")
    mtop, B_mtop = LP.take([16, 16], F32, "mtop"); ixu, B_ixu = LP.take([16, 16], U32, "ixu")
    ixf, B_ixf = LP.take([16, 16], F32, "ixf")
    best, B_best = LP.take([8, 16], F32, "best"); posu, B_posu = LP.take([8, 16], U32, "posu")
    pa_u, B_pau = LP.take([128], U32, "pa_u"); pb_u, B_pbu = LP.take([128], U32, "pb_u")
    pa_f, B_paf = LP.take([128], F32, "pa_f"); pb_f, B_pbf = LP.take([128], F32, "pb_f")
    ia, B_ia = LP.take([128], F32, "ia"); ib, B_ib = LP.take([128], F32, "ib")
    eidf, B_eidf = LP.take([128], F32, "eidf")
    gsum, B_gsum = LP.take([8], F32, "gsum")
    eid = []; B_eid = []; gate = []; B_gate = []
    for j in range(NSUB):
        v, b = LP.take([128], I32, f"eid{j}"); eid.append(v); B_eid.append(b)
        v, b = LP.take([8, 16], F32, f"gate{j}"); gate.append(v); B_gate.append(b)
    dots, B_dots = LP.take([128], F32, "dots"); coef, B_coef = LP.take([128], F32, "coef")
    LPa = Lay(); LPa.off = LP.off
    qpT, B_qpT = LPa.take([16, T], BF16, "qpT")
    sub, B_sub = LPa.take([16, 128], F32, "sub")
    cand, B_cand = LPa.take([8, 256], F32, "cand")
    ohb, B_ohb = LPa.take([128, 16], F32, "ohb")
    LPb = Lay(); LPb.off = LPa.off
    prod = []; B_prod = []
    for i in range(2):
        v_, b_ = LPb.take([D], BF16, f"prod{i}"); prod.append(v_); B_prod.append(b_)
    NG = 8
    gb = []; B_gb = []
    gb.append(av(0, [2 * D], BF16)); B_gb.append([B_bcA])
    for i in range(1, 4):
        v_, b_ = LPb.take([2 * D], BF16, f"gb{i}"); gb.append(v_); B_gb.append([b_])
    for i in range(3):
        gb.append(wb_all[:, i * 2 * D:(i + 1) * 2 * D]); B_gb.append([B_wb[2 * i], B_wb[2 * i + 1]])
    gb.append(hT[:, :, :].rearrange("p k t -> p (k t)")); B_gb.append([B_hT])
    NPAR = 4
    dotg = []; B_dotg = []; gact = []; B_gact = []; dg = []; B_dg = []
    for i in range(NPAR):
        v_, b_ = LPb.take([2], F32, f"dotg{i}"); dotg.append(v_); B_dotg.append(b_)
        v_, b_ = LPb.take([2], F32, f"gact{i}"); gact.append(v_); B_gact.append(b_)
        v_, b_ = LPb.take([2, 128], BF16, f"dg{i}"); dg.append(v_); B_dg.append([Buf(f"dg{i}_0"), Buf(f"dg{i}_1")])
    rt2, B_rt2 = LPb.take([512], F32, "rt2")

    def flat(v):
        return v

    def rmsnorm_to_hT(j, s_off, b_off, first):
        c0 = 0 if first else 4
        op("act", lambda e: e.activation(hTM[:, j, :], xres[:, j, :], AF.Square, accum_out=stat[:, c0 + j:c0 + j + 1]),
           reads=[B_x[j]], writes=[B_hTM, B_stat])
        op("act", lambda e: e.activation(stat[:, 8 + j:9 + j], stat[:, c0 + j:c0 + j + 1], AF.Ln,
                                         bias=epst[:, 0:1], scale=1.0 / D), reads=[B_stat, B_cst], writes=[B_stat])
        op("act", lambda e: e.activation(stat[:, c0 + j:c0 + j + 1], stat[:, 8 + j:9 + j], AF.Exp, scale=-0.5),
           reads=[B_stat], writes=[B_stat])
        op("dve", lambda e: e.scalar_tensor_tensor(hTM[:, j, :], xres[:, j, :], stat[:, c0 + j:c0 + j + 1], bcA_ap(),
                                                   ALU.mult, ALU.mult), reads=[B_x[j], B_stat, B_bcA], writes=[B_hTM])
        op("dve", lambda e: e.tensor_tensor(hTM[:, j, :], hTM[:, j, :], bcB_ap(), ALU.add), reads=[B_hTM, B_bcB], writes=[B_hTM])
        for half in range(2):
            bank = 2 + half
            pbb = pb[bank][:, :].bitcast(BF16)
            for k8 in range(8):
                kc = half * 8 + k8
                op("pe", lambda e, kc=kc, k8=k8, pbb=pbb: e.transpose(pbb[:, k8 * 128:(k8 + 1) * 128],
                                                                       hTM[:, j, kc * 128:(kc + 1) * 128], identb[:, :]),
                   reads=[B_hTM, B_cst], writes=[B_pb[bank]])
            eng = "act" if half == 0 else "dve"
            src = pbb.rearrange("p (k t) -> p k t", k=8)
            dst = hT[:, half * 8:(half + 1) * 8, j * 128:(j + 1) * 128]
            if eng == "act":
                op("act", lambda e, src=src, dst=dst: e.activation(dst, src, AF.Copy), reads=[B_pb[bank]], writes=[B_hT])
            else:
                op("dve", lambda e, src=src, dst=dst: e.tensor_copy(dst, src), reads=[B_pb[bank]], writes=[B_hT])

    def bcA_ap():
        return arena[:, 0:D]

    def bcB_ap():
        return arena[:, D:2 * D]

    def load_bc(which, off):
        ap_ = bcA_ap() if which == 0 else bcB_ap()
        dma("sp", ap_, bcast_row(modv_h, off), reads=[B_modv], writes=[B_bcA if which == 0 else B_bcB])

    pbsel = [0]

    def next_bank01():
        pbsel[0] ^= 1
        return pbsel[0]

    def proj_fm(ch, nk, rhs_fn, rhs_bufs, ncols=T, col0=0, ldw=None):
        w, bw = ldw if ldw is not None else load_chunk(ch)
        bank = next_bank01()
        for kc in range(nk):
            lhsT = w[:, kc * 128:(kc + 1) * 128]
            mm(pb[bank][:, 0:ncols], lhsT, rhs_fn(kc), kc == 0, kc == nk - 1, reads=[bw] + rhs_bufs, writes=[B_pb[bank]])
        return bank

    hT_rhs = lambda kc: hT[:, kc, :]

    for ti in (tile_list if tile_list is not None else range(NT)):
        t0 = ti * T
        for j in range(NSUB):
            dma("sp", xres[:, j, :], x_h[t0 + j * 128:t0 + (j + 1) * 128, :], writes=[B_x[j]])
        load_bc(0, MV_S1); load_bc(1, MV_SH1)
        for j in range(NSUB):
            rmsnorm_to_hT(j, MV_S1, MV_SH1, True)
        if ti == 0:
            dump("hT", hT[:, :, :], [B_hT])

        if do_mix:
            for c in range(8):
                bank = proj_fm(CH_IQ + c, 16, hT_rhs, [B_hT])
                op("act", lambda e, bank=bank, c=c: e.activation(iqT[:, c, :], pb[bank][:, 0:T], AF.Copy),
                   reads=[B_pb[bank]], writes=[B_iqT])
            bank = proj_fm(CH_IK, 16, hT_rhs, [B_hT])
            op("act", lambda e, bank=bank: e.activation(ikT[:, t0:t0 + T], pb[bank][:, 0:T], AF.Copy),
               reads=[B_pb[bank]], writes=[B_ik])
            w, bw = load_chunk(CH_IW)
            for j in range(NSUB):
                bank = next_bank01()
                for kc in range(16):
                    mm(pb[bank][:, 0:16], hT[:, kc, j * 128:(j + 1) * 128], w[:, kc * 16:(kc + 1) * 16],
                       kc == 0, kc == 15, reads=[B_hT, bw], writes=[B_pb[bank]])
                op("act", lambda e, bank=bank, j=j: e.activation(iw[:, j, :], pb[bank][:, 0:16], AF.Copy),
                   reads=[B_pb[bank]], writes=[B_iw])

            def qkv_block(ti=ti, t0=t0):
                for c in range(10):
                    isq = c < 8
                    bank = proj_fm((CH_Q + c) if isq else (CH_K + c - 8), 16, hT_rhs, [B_hT])
                    op("act", lambda e, bank=bank: e.activation(sqt, pb[bank][:, 0:T], AF.Square),
                       reads=[B_pb[bank]], writes=[B_sqt])
                    mm(pb[2][:, 0:T], onesb[:, :], sqt, True, True, reads=[B_cst, B_sqt], writes=[B_pb[2]])
                    op("act", lambda e: e.activation(rtq, pb[2][:, 0:T], AF.Ln, bias=epst[:, 0:1], scale=1.0 / 128),
                       reads=[B_pb[2], B_cst], writes=[B_rtq])
                    op("act", lambda e: e.activation(rtq, rtq, AF.Exp, scale=-0.5), reads=[B_rtq], writes=[B_rtq])
                    if isq:
                        dst = qT[:, c, :]; bd = B_qT; gcol = 0
                    else:
                        dst = kT[:, c - 8, t0:t0 + T]; bd = B_kT; gcol = 1
                    qk = c % 2
                    op("act", lambda e, bank=bank, gcol=gcol, qk=qk: e.activation(qsb[qk], pb[bank][:, 0:T], AF.Copy,
                                                                                 scale=gqs[:, gcol:gcol + 1]),
                       reads=[B_pb[bank], B_gqs], writes=[B_qsb[qk]])
                    op("pool", lambda e, dst=dst, qk=qk: e.tensor_tensor(dst, qsb[qk], rtq, ALU.mult),
                       reads=[B_qsb[qk], B_rtq], writes=[bd])
                for g in range(2):
                    w, bw = load_chunk(CH_V + g)
                    for j in range(NSUB):
                        bank = next_bank01()
                        for kc in range(16):
                            mm(pb[bank][:, 0:128], hT[:, kc, j * 128:(j + 1) * 128], w[:, kc * 128:(kc + 1) * 128],
                               kc == 0, kc == 15, reads=[B_hT, bw], writes=[B_pb[bank]])
                        op("act", lambda e, bank=bank, g=g, j=j: e.activation(vaug[:, ti * NSUB + j, g, 0:128], pb[bank][:, 0:128], AF.Copy),
                           reads=[B_pb[bank]], writes=[B_v])
            if ti == 0:
                dump("iw", iw[:, :, :], [B_iw])

            def indexer(j, ti=ti):
                qi = ti * NSUB + j; Lk = (qi + 1) * 128
                qs = slice(j * 128, (j + 1) * 128)
                use_mask = qi >= 2
                for ih in range(16):
                    c = ih // 2; p0 = (ih % 2) * 64
                    for s0 in range(0, Lk, 512):
                        n = min(512, Lk - s0)
                        bank = next_bank01(); r = (ih + s0 // 512) % 2
                        mm(pb[bank][:, 0:n], iqT[p0:p0 + 64, c, qs], ikT[p0:p0 + 64, s0:s0 + n], True, True,
                           reads=[B_iqT, B_ik], writes=[B_pb[bank]])
                        op("act", lambda e, bank=bank, r=r, n=n: e.activation(rl[r][:, 0:n], pb[bank][:, 0:n], AF.Relu),
                           reads=[B_pb[bank]], writes=[B_rl[r]])
                        last = (s0 + n == Lk)
                        if ih == 0:
                            nb = n - 128 if last else n
                            if nb > 0:
                                op("dve", lambda e, r=r, s0=s0, nb=nb, j=j: e.tensor_scalar(
                                    acc[:, s0:s0 + nb], rl[r][:, 0:nb], iw[:, j, 0:1], None, ALU.mult),
                                   reads=[B_rl[r], B_iw], writes=[B_acc])
                            if last:
                                op("dve", lambda e, r=r, s0=s0, n=n, j=j: e.scalar_tensor_tensor(
                                    acc[:, s0 + n - 128:s0 + n], rl[r][:, n - 128:n], iw[:, j, 0:1], cmask[:, :],
                                    ALU.mult, ALU.add), reads=[B_rl[r], B_iw, B_cst], writes=[B_acc])
                        else:
                            op("dve", lambda e, r=r, s0=s0, n=n, j=j, ih=ih: e.scalar_tensor_tensor(
                                acc[:, s0:s0 + n], rl[r][:, 0:n], iw[:, j, ih:ih + 1], acc[:, s0:s0 + n],
                                ALU.mult, ALU.add), reads=[B_rl[r], B_iw, B_acc], writes=[B_acc])
                if qi == 2:
                    dump("acc", acc[:, 0:384], [B_acc])
            def topk(j, ti=ti):
                qi = ti * NSUB + j; Lk = (qi + 1) * 128
                qs = slice(j * 128, (j + 1) * 128)
                use_mask = qi >= 2
                if use_mask:
                    KB = 24
                    op("dve", lambda e: e.max(out=m8, in_=acc[:, 0:Lk]), reads=[B_acc], writes=[B_m8])
                    op("dve", lambda e: e.tensor_reduce(out=bs[:, 4:5], in_=acc[:, 0:Lk - 128], axis=AX.X, op=ALU.min),
                       reads=[B_acc], writes=[B_bs])
                    op("dve", lambda e: e.tensor_tensor(bs[:, 5:6], m8[:, 0:1], bs[:, 4:5], ALU.subtract), reads=[B_m8, B_bs], writes=[B_bs])
                    op("dve", lambda e: e.tensor_scalar(Wt[:, :], pow2[:, :], bs[:, 5:6], None, ALU.mult), reads=[B_cst, B_bs], writes=[B_Wt])
                    op("dve", lambda e: e.tensor_scalar(W2t[:, :], Wt[:, :], 2.0, None, ALU.mult), reads=[B_Wt], writes=[B_W2t])
                    op("dve", lambda e: e.tensor_tensor(bs[:, 0:1], bs[:, 4:5], Wt[:, 0:1], ALU.add), reads=[B_bs, B_Wt], writes=[B_bs])
                    for kb in range(KB):
                        op("dve", lambda e: e.tensor_scalar(junk8[:, 0:Lk], acc[:, 0:Lk], bs[:, 0:1], None, ALU.is_ge, ALU.add,
                                                            accum_out=bs[:, 1:2]), reads=[B_acc, B_bs], writes=[B_junk8, B_bs])
                        op("dve", lambda e, kb=kb: e.tensor_scalar(bs[:, 2:3], bs[:, 1:2], 256.0, W2t[:, kb + 1:kb + 2], ALU.is_ge, ALU.mult),
                           reads=[B_bs, B_W2t], writes=[B_bs])
                        op("dve", lambda e, kb=kb: e.scalar_tensor_tensor(bs[:, 0:1], bs[:, 2:3], Wt[:, kb + 1:kb + 2], bs[:, 0:1],
                                                                          ALU.subtract, ALU.add), reads=[B_bs, B_Wt], writes=[B_bs])
                    op("dve", lambda e: e.tensor_tensor(bs[:, 3:4], bs[:, 0:1], Wt[:, KB:KB + 1], ALU.subtract), reads=[B_bs, B_Wt], writes=[B_bs])
                    op("dve", lambda e: e.tensor_scalar(mneg[:, 0:Lk], acc[:, 0:Lk], bs[:, 3:4], -1.0, ALU.is_lt, ALU.mult),
                       reads=[B_acc, B_bs], writes=[B_mneg])
                    if qi == 2:
                        dump("mneg", mneg[:, 0:384], [B_mneg])
            def attention(j, ti=ti):
                qi = ti * NSUB + j; Lk = (qi + 1) * 128
                qs = slice(j * 128, (j + 1) * 128)
                use_mask = qi >= 2
                for h in range(8):
                    g = h // 4
                    pvb = 6 + (h % 2)
                    for grp in range(0, qi + 1, 4):
                        scs = list(range(grp, min(grp + 4, qi + 1)))
                        lbk = 4 + ((grp // 4) % 2); pr = (grp // 4) % 2
                        for sc_ in scs:
                            col = (sc_ - grp) * 128
                            o = pb[lbk][:, col:col + 128]
                            extra = (1 if use_mask else 0) + (2 if sc_ >= qi - 1 else 0)
                            mm(o, kT[:, g, sc_ * 128:(sc_ + 1) * 128], qT[:, h, qs], True, extra == 0,
                               reads=[B_kT, B_qT], writes=[B_pb[lbk]])
                            if use_mask:
                                extra -= 1
                                mm(o, mneg[:, sc_ * 128:(sc_ + 1) * 128], identbig[:, :], False, extra == 0,
                                   reads=[B_mneg, B_cst], writes=[B_pb[lbk]])
                            if sc_ >= qi - 1:
                                dl = qi - sc_
                                mm(o, biasT[:, h, dl, 0, :], identb[:, :], False, False, reads=[B_bias, B_cst], writes=[B_pb[lbk]])
                                mm(o, biasT[:, h, dl, 1, :], identb[:, :], False, True, reads=[B_bias, B_cst], writes=[B_pb[lbk]])
                        ncol = len(scs) * 128
                        op("act", lambda e, lbk=lbk, pr=pr, ncol=ncol: e.activation(pT[pr][:, 0:ncol], pb[lbk][:, 0:ncol], AF.Exp),
                           reads=[B_pb[lbk]], writes=[B_pT[pr]])
                        for sc_ in scs:
                            col = (sc_ - grp) * 128
                            mm(pb[pvb][:, 0:129], pT[pr][:, col:col + 128], vaug[:, sc_, g, 0:129], sc_ == 0, sc_ == qi,
                               reads=[B_pT[pr], B_v], writes=[B_pb[pvb]])
                    ra = h % 2
                    op("act", lambda e, pvb=pvb, ra=ra: e.activation(rdn[:, ra:ra + 1], pb[pvb][:, 128:129], AF.Ln),
                       reads=[B_pb[pvb]], writes=[B_rdn[ra]])
                    op("act", lambda e, ra=ra: e.activation(rdn[:, ra:ra + 1], rdn[:, ra:ra + 1], AF.Exp, scale=-1.0),
                       reads=[B_rdn[ra]], writes=[B_rdn[ra]])
                    op("act", lambda e, pvb=pvb, h=h, ra=ra: e.activation(attn_tm[:, h * 128:(h + 1) * 128], pb[pvb][:, 0:128], AF.Copy,
                                                                          scale=rdn[:, ra:ra + 1]),
                       reads=[B_pb[pvb], B_rdn[ra]], writes=[B_atm])
                pbb = pb[3][:, :].bitcast(BF16)
                for h in range(8):
                    op("pe", lambda e, h=h: e.transpose(pbb[:, h * 128:(h + 1) * 128], attn_tm[:, h * 128:(h + 1) * 128], identb[:, :]),
                       reads=[B_atm, B_cst], writes=[B_pb[3]])
                op("act", lambda e, j=j: e.activation(attnT[:, :, j * 128:(j + 1) * 128], pbb.rearrange("p (k t) -> p k t", k=8), AF.Copy),
                   reads=[B_pb[3]], writes=[B_attnT])

            def pool_block(ti=ti):
                for c in range(8):
                    g = c // 2; wdw = (2, 4, 8, 16)[g]
                    bank = proj_fm(CH_PL + c, 16, hT_rhs, [B_hT])
                    pbuf = plb[c % 2]; bpl = B_plb[c % 2]
                    op("act", lambda e, bank=bank, pbuf=pbuf: e.activation(pbuf[:, 16:16 + T], pb[bank][:, 0:T], AF.Copy),
                       reads=[B_pb[bank]], writes=[bpl])
                    op("pool", lambda e, pbuf=pbuf, c=c: e.tensor_copy(pbuf[:, 0:16], halo[:, c, :]),
                       reads=[B_halo], writes=[bpl])
                    op("pool", lambda e, pbuf=pbuf, c=c: e.tensor_copy(halo[:, c, :], pbuf[:, T:T + 16]),
                       reads=[bpl], writes=[B_halo])
                    cur = pbuf; bcur = bpl; k = 1; st = 0; lo = 1
                    while k < wdw:
                        nxt = ptmp[st % 2]; bn = B_ptmp[st % 2]
                        op("pool", lambda e, cur=cur, nxt=nxt, k=k, lo=lo: e.tensor_tensor(
                            nxt[:, lo:16 + T], cur[:, lo:16 + T], cur[:, lo - k:16 + T - k], ALU.add),
                           reads=[bcur], writes=[bn])
                        cur = nxt; bcur = bn; k *= 2; lo += k; st += 1
                    oth = ptmp[st % 2]; both = B_ptmp[st % 2]
                    op("pool", lambda e, cur=cur, oth=oth, wdw=wdw: e.tensor_scalar(oth[:, 16:16 + T], cur[:, 16:16 + T], 1.0 / wdw, 0.0, ALU.mult, ALU.add),
                       reads=[bcur], writes=[both])
                    op("pool", lambda e, oth=oth, pbuf=pbuf, c=c: e.tensor_tensor(praw[:, c, :], oth[:, 16:16 + T], pbuf[:, 16:16 + T], ALU.subtract),
                       reads=[both, bpl], writes=[B_praw])
                    if ti == 0:
                        op("pool", lambda e, cur=cur, oth=oth, g=g: e.tensor_tensor(oth[:, 16:32], cur[:, 16:32], invc[:, g, :], ALU.mult),
                           reads=[bcur, B_cst], writes=[both])
                        op("pool", lambda e, oth=oth, pbuf=pbuf, c=c: e.tensor_tensor(praw[:, c, 0:16], oth[:, 16:32], pbuf[:, 16:32], ALU.subtract),
                           reads=[both, bpl, B_praw], writes=[B_praw])
                w, bw = load_chunk(CH_PW)
                for g in range(4):
                    for eh in range(2):
                        bank = next_bank01()
                        for kc in range(2):
                            lhsT = w[:, g * 512 + kc * 256 + eh * 128: g * 512 + kc * 256 + eh * 128 + 128]
                            mm(pb[bank][:, 0:T], lhsT, praw[:, 2 * g + kc, :], kc == 0, kc == 1, reads=[bw, B_praw], writes=[B_pb[bank]])
                        op("act", lambda e, bank=bank, g=g, eh=eh: e.activation(pooledT[:, 2 * g + eh, :], pb[bank][:, 0:T], AF.Copy,
                                                                                scale=pscale[:, 2 * g + eh:2 * g + eh + 1]),
                           reads=[B_pb[bank], B_cst], writes=[B_pooled])
                if ti == 0:
                    dump("pooledT", pooledT[:, :, :], [B_pooled])


            def gates(c_lo, c_hi):
                for c in range(c_lo, c_hi):
                    bank = proj_fm(CH_GA + c, 16, hT_rhs, [B_hT])
                    op("act", lambda e, bank=bank, c=c: e.activation(sga[:, c, :], pb[bank][:, 0:T], AF.Sigmoid),
                       reads=[B_pb[bank]], writes=[B_sga])
                    bank = proj_fm(CH_GP + c, 16, hT_rhs, [B_hT])
                    op("act", lambda e, bank=bank, c=c: e.activation(sgp[:, c, :], pb[bank][:, 0:T], AF.Sigmoid),
                       reads=[B_pb[bank]], writes=[B_sgp])

            indexer(0); qkv_block(); pool_block(); gates(0, 8); topk(0)
            if ti == 0:
                dump("qT", qT[:, :, :], [B_qT]); dump("kT", kT[:, :, 0:T], [B_kT])
            for j in range(1, NSUB):
                indexer(j); attention(j - 1)
                if j == 1:
                    gates(8, 16)
                topk(j)
            attention(NSUB - 1)
            if ti == 0:
                dump("attnT", attnT[:, :, :], [B_attnT])

            sc.barrier()
            wab = wpbk = None
            for c in range(16):
                if c % 2 == 0:
                    wab = load_chunk(CH_AB + c // 2); wpbk = load_chunk(CH_PB + c // 2)
                col0 = (c % 2) * 128
                for kc in range(8):
                    mm(pb[4][:, 0:T], wab[0][:, kc * 256 + col0:kc * 256 + col0 + 128], attnT[:, kc, :], kc == 0, kc == 7,
                       reads=[wab[1], B_attnT], writes=[B_pb[4]])
                for kc in range(8):
                    mm(pb[5][:, 0:T], wpbk[0][:, kc * 256 + col0:kc * 256 + col0 + 128], pooledT[:, kc, :], kc == 0, kc == 7,
                       reads=[wpbk[1], B_pooled], writes=[B_pb[5]])
                sa = (c % 2) * 2
                op("dve", lambda e, sa=sa, c=c: e.tensor_tensor(sg[sa], sga[:, c, :], pb[4][:, 0:T], ALU.mult),
                   reads=[B_sga, B_pb[4]], writes=[B_sg[sa]])
                op("dve", lambda e, sa=sa, c=c: e.tensor_tensor(sg[sa + 1], sgp[:, c, :], pb[5][:, 0:T], ALU.mult),
                   reads=[B_sgp, B_pb[5]], writes=[B_sg[sa + 1]])
                op("dve", lambda e, sa=sa, c=c: e.tensor_tensor(mergedT[:, c, :], sg[sa], sg[sa + 1], ALU.add),
                   reads=[B_sg[sa], B_sg[sa + 1]], writes=[B_mrg])
            if ti == 0:
                dump("mergedT", mergedT[:, :, :], [B_mrg])

            load_bc(0, MV_GT1)
            for cc in range(16):
                w, bw = load_chunk(CH_WO + cc)
                bank = next_bank01()
                for j in range(NSUB):
                    for kc in range(16):
                        mm(pb[bank][:, j * 128:(j + 1) * 128], mergedT[:, kc, j * 128:(j + 1) * 128], w[:, kc * 128:(kc + 1) * 128],
                           kc == 0, kc == 15, reads=[B_mrg, bw], writes=[B_pb[bank]])
                gt = AP(tensor=arena, offset=cc * 128,
                        ap=[[ARENA // 4, 128], [0, NSUB], [1, 128]])
                op("dve", lambda e, bank=bank, gt=gt: e.tensor_tensor(
                    rtmp[:, :, :], pb[bank][:, 0:T].rearrange("p (j c) -> p j c", j=NSUB), gt, ALU.mult),
                   reads=[B_pb[bank], B_bcA], writes=[B_rtmp])
                op("dve", lambda e, cc=cc: e.tensor_tensor(xres[:, :, cc * 128:(cc + 1) * 128], xres[:, :, cc * 128:(cc + 1) * 128],
                                                           rtmp[:, :, :], ALU.add), reads=[B_rtmp] + B_x, writes=B_x)
        if ti == NT - 1:
            dump("x1", xres[:, :, :], B_x)
        sc.barrier()

        if do_peer:
            load_bc(0, MV_S2); load_bc(1, MV_SH2)
            for j in range(NSUB):
                rmsnorm_to_hT(j, MV_S2, MV_SH2, False)
            for c in range(16):
                bank = proj_fm(CH_PQ + c, 16, hT_rhs, [B_hT])
                op("act", lambda e, bank=bank, c=c: e.activation(qpT[:, c, :], pb[bank][:, 0:T], AF.Copy),
                   reads=[B_pb[bank]], writes=[B_qpT])
            wk, bwk = load_chunk(CH_KEYS)
            deferred = []; defer_on = [False]

            def dop(en, fn, reads=(), writes=()):
                if defer_on[0]:
                    deferred.append((en, fn, list(reads), list(writes)))
                else:
                    op(en, fn, reads=reads, writes=writes)

            def flush(n):
                while deferred and n > 0:
                    en, fn, r_, w_ = deferred.pop(0)
                    op(en, fn, reads=r_, writes=w_); n -= 1
            for j in range(NSUB):
                for hp in range(16):
                    bank = 4 + hp // 4
                    mm(pb[bank][:, (hp % 4) * 128:(hp % 4 + 1) * 128], qpT[:, hp, j * 128:(j + 1) * 128],
                       wk[:, hp * 128:(hp + 1) * 128], True, True, reads=[B_qpT, bwk], writes=[B_pb[bank]])
                for b4 in range(4):
                    op("act", lambda e, b4=b4: e.activation(sub[:, b4 * 4:(b4 + 1) * 4, :],
                                                            pb[4 + b4][:, :].rearrange("p (a n) -> p a n", a=4), AF.Copy),
                       reads=[B_pb[4 + b4]], writes=[B_sub])
                defer_on[0] = (NSUB > 1 and j == NSUB - 1)
                for hp in range(16):
                    sv = sub[:, hp, :]
                    dop("dve", lambda e, sv=sv, hp=hp: e.max(out=mtop[:, hp, 0:8], in_=sv), reads=[B_sub], writes=[B_mtop])
                    dop("dve", lambda e, sv=sv, hp=hp: e.max_index(out=ixu[:, hp, 0:8], in_max=mtop[:, hp, 0:8], in_values=sv),
                       reads=[B_sub, B_mtop], writes=[B_ixu])
                    dop("dve", lambda e, sv=sv, hp=hp: e.match_replace(out=sv, in_to_replace=mtop[:, hp, 0:8], in_values=sv, imm_value=-1.0e30),
                       reads=[B_sub, B_mtop], writes=[B_sub])
                    dop("dve", lambda e, sv=sv, hp=hp: e.max(out=mtop[:, hp, 8:16], in_=sv), reads=[B_sub], writes=[B_mtop])
                    dop("dve", lambda e, sv=sv, hp=hp: e.max_index(out=ixu[:, hp, 8:16], in_max=mtop[:, hp, 8:16], in_values=sv),
                       reads=[B_sub, B_mtop], writes=[B_ixu])
                dop("dve", lambda e: e.tensor_copy(ixf[:, :, :], ixu[:, :, :]), reads=[B_ixu], writes=[B_ixf])
                mt_t = mtop.tensor; mt_off = mtop.offset; PST = ARENA // 4
                s1b = AP(tensor=mt_t, offset=mt_off, ap=[[PST, 128], [32, 8], [1, 16], [0, 16]])
                s2b = AP(tensor=mt_t, offset=mt_off + 16, ap=[[PST, 128], [32, 8], [0, 16], [1, 16]])
                candv = cand[:, :, :].rearrange("p h (a b) -> p h a b", a=16)
                dop("dve", lambda e: e.tensor_tensor(candv, s1b, s2b, ALU.add), reads=[B_mtop], writes=[B_cand])
                for h in range(8):
                    cv = cand[:, h, :]
                    dop("dve", lambda e, cv=cv, h=h: e.max(out=best[:, h, 0:8], in_=cv), reads=[B_cand], writes=[B_best])
                    dop("dve", lambda e, cv=cv, h=h: e.max_index(out=posu[:, h, 0:8], in_max=best[:, h, 0:8], in_values=cv),
                       reads=[B_cand, B_best], writes=[B_posu])
                    dop("dve", lambda e, cv=cv, h=h: e.match_replace(out=cv, in_to_replace=best[:, h, 0:8], in_values=cv, imm_value=-1.0e30),
                       reads=[B_cand, B_best], writes=[B_cand])
                    dop("dve", lambda e, cv=cv, h=h: e.max(out=best[:, h, 8:16], in_=cv), reads=[B_cand], writes=[B_best])
                    dop("dve", lambda e, cv=cv, h=h: e.max_index(out=posu[:, h, 8:16], in_max=best[:, h, 8:16], in_values=cv),
                       reads=[B_cand, B_best], writes=[B_posu])
                bt_t = best.tensor; bt_off = best.offset
                b0 = AP(tensor=bt_t, offset=bt_off, ap=[[PST, 128], [16, 8], [0, 16]])
                gj = gate[j]
                dop("dve", lambda e, gj=gj: e.tensor_tensor(gj[:, :, :], best[:, :, :], b0, ALU.subtract), reads=[B_best], writes=[B_gate[j]])
                dop("act", lambda e, gj=gj: e.activation(gj[:, :, :], gj[:, :, :], AF.Exp), reads=[B_gate[j]], writes=[B_gate[j]])
                dop("dve", lambda e, gj=gj: e.tensor_reduce(out=gsum[:, :], in_=gj[:, :, :], axis=AX.X, op=ALU.add),
                   reads=[B_gate[j]], writes=[B_gsum])
                dop("dve", lambda e: e.reciprocal(gsum[:, :], gsum[:, :]), reads=[B_gsum], writes=[B_gsum])
                gs_b = AP(tensor=gsum.tensor, offset=gsum.offset, ap=[[PST, 128], [1, 8], [0, 16]])
                dop("dve", lambda e, gj=gj: e.tensor_tensor(gj[:, :, :], gj[:, :, :], gs_b, ALU.mult), reads=[B_gate[j], B_gsum], writes=[B_gate[j]])
                posf = posu[:, :, :].rearrange("p h r -> p (h r)")
                dop("dve", lambda e: e.tensor_single_scalar(pa_u[:, :], posf, 4, ALU.logical_shift_right), reads=[B_posu], writes=[B_pau])
                dop("dve", lambda e: e.tensor_single_scalar(pb_u[:, :], posf, 15, ALU.bitwise_and), reads=[B_posu], writes=[B_pbu])
                dop("dve", lambda e: e.tensor_copy(pa_f[:, :], pa_u[:, :]), reads=[B_pau], writes=[B_paf])
                dop("dve", lambda e: e.tensor_copy(pb_f[:, :], pb_u[:, :]), reads=[B_pbu], writes=[B_pbf])
                io_b = iota16[:, :].unsqueeze(1).to_broadcast([128, 128, 16])
                for (pf, Bpf, half, dst, Bdst) in ((pa_f, B_paf, 0, ia, B_ia), (pb_f, B_pbf, 1, ib, B_ib)):
                    pfb = pf[:, :].unsqueeze(2).to_broadcast([128, 128, 16])
                    dop("dve", lambda e, pfb=pfb: e.tensor_tensor(ohb[:, :, :], pfb, io_b, ALU.is_equal), reads=[Bpf, B_cst], writes=[B_ohb])
                    ixb = AP(tensor=ixf.tensor, offset=ixf.offset + 16 * half, ap=[[PST, 128], [32, 8], [0, 16], [1, 16]])
                    oh4 = ohb[:, :, :].rearrange("p (h r) a -> p h r a", h=8)
                    dop("dve", lambda e, oh4=oh4, ixb=ixb: e.tensor_tensor(oh4, oh4, ixb, ALU.mult), reads=[B_ohb, B_ixf], writes=[B_ohb])
                    dop("dve", lambda e, dst=dst: e.tensor_reduce(out=dst[:, :], in_=ohb[:, :, :], axis=AX.X, op=ALU.add),
                       reads=[B_ohb], writes=[Bdst])
                dop("dve", lambda e: e.scalar_tensor_tensor(eidf[:, :], ia[:, :], 128.0, ib[:, :], ALU.mult, ALU.add),
                   reads=[B_ia, B_ib], writes=[B_eidf])
                dop("dve", lambda e, j=j: e.tensor_copy(eid[j][:, :], eidf[:, :]), reads=[B_eidf], writes=[B_eid[j]])
                defer_on[0] = False
                if ti == 0 and j == 0:
                    dump("eid", eid[0][:, :], [B_eid[0]]); dump("gate", gate[0][:, :, :], [B_gate[0]])

            load_bc(1, MV_GT2)
            GS = 2; NGRP = 128 // GS
            glist = [(j, g) for j in range(NSUB) for g in range(NGRP)]
            kof = {}
            gctr = 0

            def stage_A(idx):
                j, g = glist[idx]; par = idx % NPAR
                nonlocal_k = []
                for i in range(GS):
                    slot = g * GS + i
                    k = (idx * GS + i) % NG
                    nonlocal_k.append(k)
                    dma("pool", gb[k], uv_h.ap(), reads=[B_eid[j], B_uv], writes=B_gb[k],
                        indirect=bass.IndirectOffsetOnAxis(ap=eid[j][:, slot:slot + 1], axis=0))
                    pk = (idx * GS + i) % 2
                    op("dve", lambda e, k=k, j=j, pk=pk: e.tensor_tensor(prod[pk][:, :], hTM[:, j, :], gb[k][:, 0:D], ALU.mult),
                       reads=[B_hTM] + B_gb[k], writes=[B_prod[pk]])
                    op("act", lambda e, i=i, par=par, pk=pk: e.activation(prod[pk][:, :], prod[pk][:, :], AF.Copy,
                                                                          accum_out=dotg[par][:, i:i + 1]),
                       reads=[B_prod[pk]], writes=[B_prod[pk], B_dotg[par]])
                kof[idx] = nonlocal_k
                op("act", lambda e, par=par: e.activation(gact[par][:, :], dotg[par][:, :], AF.Gelu),
                   reads=[B_dotg[par]], writes=[B_gact[par]])

            def stage_C(idx):
                j, g = glist[idx]; par = idx % NPAR
                gflat = gate[j][:, :, :].rearrange("p h r -> p (h r)")
                for i in range(GS):
                    slot = g * GS + i; k = kof[idx][i]
                    op("dve", lambda e, i=i, par=par, slot=slot, gflat=gflat: e.tensor_scalar(
                        dg[par][:, i, :], identb[:, :], gact[par][:, i:i + 1], gflat[:, slot:slot + 1], ALU.mult, ALU.mult),
                       reads=[B_cst, B_gact[par], B_gate[j]], writes=[B_dg[par][i]])
                    for q4 in range(4):
                        mm(pb[q4][:, :], dg[par][:, i, :], gb[k][:, D + q4 * 512:D + (q4 + 1) * 512], slot == 0, slot == 127,
                           reads=[B_dg[par][i]] + B_gb[k], writes=[B_pb[q4]])
                if g == NGRP - 1:
                    for q4 in range(4):
                        op("dve", lambda e, q4=q4: e.tensor_tensor(rt2[:, :], pb[q4][:, :], arena[:, D + q4 * 512:D + (q4 + 1) * 512], ALU.mult),
                           reads=[B_pb[q4], B_bcB], writes=[B_rt2])
                        op("dve", lambda e, q4=q4, j=j: e.tensor_tensor(xres[:, j, q4 * 512:(q4 + 1) * 512], xres[:, j, q4 * 512:(q4 + 1) * 512],
                                                                        rt2[:, :], ALU.add), reads=[B_x[j], B_rt2], writes=[B_x[j]])

            for idx in range(len(glist) + 1):
                if idx < len(glist):
                    if glist[idx][0] > 0:
                        flush(10 ** 9)
                    stage_A(idx)
                if idx >= 1:
                    stage_C(idx - 1)
                flush(3)
            flush(10 ** 9)
        for j in range(NSUB):
            dma("sp", out_h[t0 + j * 128:t0 + (j + 1) * 128, :], xres[:, j, :], reads=[B_x[j]])
        sc.barrier()

    sc.drain_dmas("sp")
    return nc, stack


def _noop():
    pass


_CACHE = {}


def make_in_maps(inputs):
    cst = host_consts()
    f = lambda a: np.ascontiguousarray(np.asarray(a, dtype=np.float32))
    x = f(inputs["x"]); c = f(inputs["c"])
    shared = {
        "w_ada": f(inputs["w_ada"][0]), "b_ada": f(inputs["b_ada"][0]).reshape(1, -1),
        "g1": f(inputs["g_norm1"][0]).reshape(1, -1), "g2": f(inputs["g_norm2"][0]).reshape(1, -1),
        "w_in": f(inputs["w_in"][0]), "gq": f(inputs["g_q"][0]).reshape(128, 1), "gk": f(inputs["g_k"][0]).reshape(128, 1),
        "rel_bias": f(inputs["rel_bias"]), "pool_w": f(inputs["pool_w"][0]),
        "pscale": np.ascontiguousarray(f(inputs["pool_scale"][0]).reshape(8, 128).T),
        "w_attn_br": f(inputs["w_attn_br"][0]), "w_pool_br": f(inputs["w_pool_br"][0]),
        "w_out": f(inputs["w_out"][0]), "w_peer_q": f(inputs["w_peer_q"][0]),
        "peer_keys": f(inputs["peer_keys"][0]).reshape(16, 128, 128),
        "peer_u": f(inputs["peer_u"][0]), "peer_v": f(inputs["peer_v"][0]),
    }
    shared.update(cst)
    maps = []
    for b in range(x.shape[0]):
        m = dict(shared)
        m["x"] = x[b]
        m["cT"] = np.ascontiguousarray(c[b].reshape(16, 128).T)
        maps.append(m)
    return maps


def kernel(**inputs):
    nc, stack = build()
    maps = make_in_maps(inputs)
    res = run_bass_kernel_spmd(nc, maps, core_ids=list(range(8)))
    out = np.stack([np.asarray(r["out"], dtype=np.float32) for r in res.results], axis=0)
    return out
```

```python
import math
from contextlib import ExitStack
import numpy as np
import ml_dtypes
import concourse.bass as bass
import concourse.mybir as mybir
from concourse.bass_utils import run_bass_kernel_spmd

F32 = mybir.dt.float32; BF16 = mybir.dt.bfloat16; I32 = mybir.dt.int32; U32 = mybir.dt.uint32
ALU = mybir.AluOpType; AF = mybir.ActivationFunctionType; AX = mybir.AxisListType

D = 2048; S = 4096; T = 256; NSUB = T // 128; NTILES = S // T
INW = 7760
C_Q, C_K, C_V, C_IQ, C_IK, C_IW, C_PL, C_GA, C_GP = 0, 1024, 1280, 1536, 2560, 2624, 2640, 3664, 5712
CH_Q = 0; CH_K = 8; CH_V = 10; CH_IQ = 12; CH_IK = 20; CH_IW = 21; CH_PL = 22; CH_GA = 30; CH_GP = 46
CH_AB = 62; CH_PB = 70; CH_PW = 78; CH_WO = 79; CH_PQ = 95; CH_KEYS = 111; NCH = 112
EPS = 1e-6
EPOCH = 20000
NDS = 8
NEGBIG = -3.0e38


class Buf:
    __slots__ = ("name", "lw", "rd")

    def __init__(self, name):
        self.name = name; self.lw = None; self.rd = {}


class Eng:
    def __init__(self, idx, name, h):
        self.idx = idx; self.name = name; self.h = h; self.seq = 0; self.sems = []; self.waited = {}
        self.dcount = 0; self.dvals = [0] * NDS; self.dsems = None


class Sched:
    def __init__(self, nc, stack):
        self.nc = nc; self.stack = stack; self.E = {}
        for i, (n, h) in enumerate([("pe", nc.tensor), ("act", nc.scalar), ("dve", nc.vector),
                                    ("pool", nc.gpsimd), ("sp", nc.sync)]):
            self.E[n] = Eng(i, n, h)
        self.elist = list(self.E.values()); self.dsem_list = []

    def _esem(self, E, ep):
        while len(E.sems) <= ep:
            E.sems.append(self.stack.enter_context(self.nc.semaphore(f"e_{E.name}_{len(E.sems)}")))
        return E.sems[ep]

    def _wait(self, E, ev):
        kind, k, v = ev
        key = (kind, k)
        if E.waited.get(key, 0) >= v:
            return
        E.waited[key] = v
        if kind == "e":
            E2 = self.elist[k]; ep, val = divmod(v - 1, EPOCH)
            E.h.wait_ge(self._esem(E2, ep), val + 1)
        else:
            E.h.wait_ge(self.dsem_list[k], v)

    def op(self, en, fn, reads=(), writes=()):
        E = self.E[en]; deps = []
        for r in reads:
            if r.lw is not None and not (en == "pe" and r.lw[0] == "e" and r.lw[1] == E.idx):
                deps.append(r.lw)
        pe = en == "pe"
        for w in writes:
            if w.lw is not None and not (pe and w.lw[0] == "e" and w.lw[1] == E.idx):
                deps.append(w.lw)
            for ev in w.rd.values():
                if not (pe and ev[0] == "e" and ev[1] == E.idx):
                    deps.append(ev)
        for ev in deps:
            self._wait(E, ev)
        inst = fn(E.h)
        E.seq += 1; ep, _ = divmod(E.seq - 1, EPOCH)
        inst.then_inc(self._esem(E, ep), 1)
        ev = ("e", E.idx, E.seq)
        for r in reads:
            r.rd[("e", E.idx)] = ev
        for w in writes:
            w.lw = ev; w.rd = {}
        return inst

    def dma(self, qn, out, in_, reads=(), writes=(), indirect=None, **kw):
        Q = self.E[qn]
        if Q.dsems is None:
            Q.dsems = []
            for i in range(NDS):
                sem = self.stack.enter_context(self.nc.semaphore(f"d_{qn}_{i}"))
                Q.dsems.append(len(self.dsem_list)); self.dsem_list.append(sem)
        slot = Q.dcount % NDS; Q.dcount += 1
        k = Q.dsems[slot]; pv = Q.dvals[slot]
        if pv > 0:
            self._wait(Q, ("d", k, pv))
        deps = []
        for r in reads:
            if r.lw is not None:
                deps.append(r.lw)
        for w in writes:
            if w.lw is not None:
                deps.append(w.lw)
            deps.extend(w.rd.values())
        for ev in deps:
            self._wait(Q, ev)
        if indirect is not None:
            inst = Q.h.indirect_dma_start(out=out, out_offset=None, in_=in_, in_offset=indirect)
        else:
            inst = Q.h.dma_start(out=out, in_=in_, **kw)
        nv = pv + 16; Q.dvals[slot] = nv
        inst.then_inc(self.dsem_list[k], 16)
        ev = ("d", k, nv)
        for r in reads:
            r.rd[("d", k)] = ev
        for w in writes:
            w.lw = ev; w.rd = {}

    def barrier(self):
        evs = [("e", E.idx, E.seq) for E in self.elist if E.seq > 0]
        for Q in self.elist:
            if Q.dsems is not None:
                for slot in range(NDS):
                    if Q.dvals[slot] > 0:
                        evs.append(("d", Q.dsems[slot], Q.dvals[slot]))
        for E in self.elist:
            for ev in evs:
                if not (ev[0] == "e" and ev[1] == E.idx):
                    self._wait(E, ev)

    def drain_dmas(self, en="sp"):
        E = self.E[en]
        for Q in self.elist:
            if Q.dsems is not None:
                for slot in range(NDS):
                    if Q.dvals[slot] > 0:
                        self._wait(E, ("d", Q.dsems[slot], Q.dvals[slot]))


def t5_bucket_np(n):
    n = np.asarray(n)
    nf = np.maximum(n, 1).astype(np.float32)
    large = 16 + (np.log(nf / np.float32(16)) / np.float32(math.log(8.0)) * np.float32(16)).astype(np.int32)
    large = np.minimum(large, 31)
    return np.where(n < 16, n, large)


def host_consts():
    c = {}
    eye = np.eye(128, dtype=np.float32)
    c["identb"] = eye.astype(ml_dtypes.bfloat16)
    c["identbig"] = (eye * 32768.0).astype(ml_dtypes.bfloat16)
    c["identf"] = eye
    c["antif"] = np.ascontiguousarray(eye[::-1])
    q = np.arange(128)[:, None]; s = np.arange(128)[None, :]
    c["cmask"] = np.where(s <= q, 0.0, -1.0e30).astype(np.float32)
    c["onesb"] = np.ones((128, 128), dtype=ml_dtypes.bfloat16)
    oh = np.zeros((33, 384), dtype=np.float32)
    for j in range(383):
        dist = j - 127
        if dist >= 0:
            oh[int(t5_bucket_np(dist)), j] = 1.0
        else:
            oh[32, j] = -30000.0
    c["oh2"] = oh
    c["iota16"] = np.tile(np.arange(16, dtype=np.float32)[None, :], (128, 1))
    invc = np.zeros((128, 4, 16), dtype=np.float32)
    for g, w in enumerate((2, 4, 8, 16)):
        for t in range(16):
            invc[:, g, t] = 1.0 / min(t + 1, w)
    c["invc"] = invc
    c["pow2"] = np.tile((2.0 ** -(np.arange(32, dtype=np.float64) + 1)).astype(np.float32)[None, :], (128, 1))
    return c


CONST_SPECS = [("identb", [128, 128], BF16), ("identbig", [128, 128], BF16), ("identf", [128, 128], F32),
               ("antif", [128, 128], F32), ("cmask", [128, 128], F32), ("onesb", [128, 128], BF16),
               ("oh2", [33, 384], F32), ("iota16", [128, 16], F32), ("invc", [128, 4, 16], F32), ("pow2", [128, 32], F32)]


def build(NT=NTILES, dbg=None, do_mix=True, do_peer=True, tile_list=None):
    dbg = dbg or {}
    nc = bass.Bass("TRN2", target_bir_lowering=False)
    stack = ExitStack()
    dt = lambda name, shape, dtype, kind="ExternalInput": nc.dram_tensor(name, shape, dtype, kind=kind)
    x_h = dt("x", [S, D], F32); cT_h = dt("cT", [128, 16], F32)
    wada_h = dt("w_ada", [D, 6 * D], F32); bada_h = dt("b_ada", [1, 6 * D], F32)
    g1_h = dt("g1", [1, D], F32); g2_h = dt("g2", [1, D], F32)
    win_h = dt("w_in", [D, INW], F32); gq_h = dt("gq", [128, 1], F32); gk_h = dt("gk", [128, 1], F32)
    relb_h = dt("rel_bias", [32, 8], F32); poolw_h = dt("pool_w", [4, 256, 256], F32)
    pscale_h = dt("pscale", [128, 8], F32)
    wab_h = dt("w_attn_br", [1024, D], F32); wpb_h = dt("w_pool_br", [1024, D], F32)
    wout_h = dt("w_out", [D, D], F32); wpq_h = dt("w_peer_q", [D, D], F32)
    keys_h = dt("peer_keys", [16, 128, 128], F32)
    pu_h = dt("peer_u", [16384, D], F32); pv_h = dt("peer_v", [16384, D], F32)
    cst_h = {n: dt(n, sh, ty) for n, sh, ty in CONST_SPECS}
    out_h = dt("out", [S, D], F32, kind="ExternalOutput")
    wsc_h = dt("wsc", [NCH, 128, 2048], BF16, kind="Internal")
    modv_h = dt("modv", [1, 6 * D], F32, kind="Internal")
    fd_h = dt("fd", [8, 384], F32, kind="Internal")
    uv_h = dt("uvtab", [16384, 2 * D], BF16, kind="Internal")
    dbg_h = {n: dt("dbg_" + n, sh, ty, kind="ExternalOutput") for n, (sh, ty) in dbg.items()}

    sb = lambda name, shape, dtype: stack.enter_context(nc.sbuf_tensor("s_" + name, shape, dtype))
    ps = lambda name, shape, dtype: stack.enter_context(nc.psum_tensor(name, shape, dtype))
    sc = Sched(nc, stack)
    op = sc.op; dma = sc.dma
    AP = bass.AP

    kT = sb("kT", [128, 2, S], BF16); B_kT = Buf("kT")
    vaug = sb("vaug", [128, 32, 2, 130], BF16); B_v = Buf("vaug")
    ikT = sb("ikT", [128, S], BF16); B_ik = Buf("ikT")
    xres = sb("xres", [128, NSUB, D], F32); B_x = [Buf(f"x{j}") for j in range(NSUB)]
    hTM = sb("hTM", [128, NSUB, D], BF16); B_hTM = Buf("hTM")
    hT = sb("hT", [128, 16, T], BF16); B_hT = Buf("hT")
    NW = 6
    wb_all = sb("wb_all", [128, NW * 2048], BF16)
    wb = [wb_all[:, i * 2048:(i + 1) * 2048] for i in range(NW)]; B_wb = [Buf(f"wb{i}") for i in range(NW)]
    identb = sb("identb", [128, 128], BF16); identbig = sb("identbig", [128, 128], BF16)
    identf = sb("identf", [128, 128], F32); antif = sb("antif", [128, 128], F32)
    cmask = sb("cmask", [128, 128], F32); onesb = sb("onesb", [128, 128], BF16)
    iota16 = sb("iota16", [128, 16], F32); invc = sb("invc", [128, 4, 16], F32); pow2 = sb("pow2", [128, 32], F32)
    B_cst = Buf("cst")
    biasT = sb("biasT", [128, 8, 2, 2, 128], BF16); B_bias = Buf("biasT")
    gqs = sb("gqs", [128, 2], F32); B_gqs = Buf("gqs")
    pscale = sb("pscale", [128, 8], F32)
    epst = sb("epst", [128, 1], F32)
    halo = sb("halo", [128, 8, 16], F32); B_halo = Buf("halo")
    stat = sb("stat", [128, 16], F32); B_stat = Buf("stat")
    ARENA = 96 * 1024
    arena = sb("arena", [128, ARENA // 4], F32)

    def av(off, shape, dtype):
        n = int(np.prod(shape)); esz = 4 if dtype in (F32, I32, U32) else 2
        assert off % 4 == 0 and off + n * esz <= ARENA, (off, shape)
        a = arena[:, off // 4:(off + n * esz) // 4]
        if dtype != F32:
            a = a.bitcast(dtype)
        if len(shape) == 2:
            a = a.rearrange("p (a b) -> p a b", a=shape[0])
        elif len(shape) == 3:
            a = a.rearrange("p (a b c) -> p a b c", a=shape[0], b=shape[1])
        elif len(shape) == 4:
            a = a.rearrange("p (a b c d) -> p a b c d", a=shape[0], b=shape[1], c=shape[2])
        return a

    class Lay:
        def __init__(self):
            self.off = 0

        def take(self, shape, dtype, name):
            n = int(np.prod(shape)); esz = 4 if dtype in (F32, I32, U32) else 2
            v = av(self.off, shape, dtype)
            self.off += (n * esz + 31) // 32 * 32
            return v, Buf(name)

    pb = [ps(f"pb{i}", [128, 512], F32) for i in range(8)]; B_pb = [Buf(f"pb{i}") for i in range(8)]

    wslot_ctr = [0]

    def load_chunk(ch):
        i = wslot_ctr[0] % NW; wslot_ctr[0] += 1
        dma("sp", wb[i], wsc_h[ch], reads=[B_wsc[ch]], writes=[B_wb[i]])
        return wb[i], B_wb[i]

    def mm(out, lhsT, rhs, start, stop, reads, writes):
        return op("pe", lambda e: e.matmul(out, lhsT, rhs, start=start, stop=stop), reads=reads, writes=writes)

    def bcast_row(h, off, n=D, parts=128):
        return AP(tensor=h, offset=off, ap=[[0, parts], [1, n]])

    B_wsc = [Buf(f"wsc{i}") for i in range(NCH)]
    B_modv = Buf("modv"); B_fd = Buf("fd")
    B_dbg = {n: Buf("dbg_" + n) for n in dbg}

    def dump(name, src_ap, src_bufs):
        if name in dbg_h:
            dma("sp", dbg_h[name].ap(), src_ap, reads=src_bufs, writes=[B_dbg[name]])

    for n, t in [("identb", identb), ("identbig", identbig), ("identf", identf), ("antif", antif),
                 ("cmask", cmask), ("onesb", onesb), ("iota16", iota16), ("invc", invc), ("pow2", pow2)]:
        dma("sp", t[:], cst_h[n].ap(), writes=[B_cst])
    dma("sp", pscale[:, :], pscale_h.ap(), writes=[B_cst])
    dma("sp", gqs[:, 0:1], gq_h.ap(), writes=[B_gqs])
    dma("sp", gqs[:, 1:2], gk_h.ap(), writes=[B_gqs])
    op("dve", lambda e: e.memset(epst[:, :], EPS), writes=[B_cst])
    op("dve", lambda e: e.tensor_scalar(gqs[:, 0:1], gqs[:, 0:1], float(128 ** -0.5), None, ALU.mult),
       reads=[B_gqs], writes=[B_gqs])
    op("dve", lambda e: e.memset(halo[:, :, :], 0.0), writes=[B_halo])
    op("pool", lambda e: e.memset(vaug[:, :, :, 128:130], 1.0), writes=[B_v])

    def cast_store(ch, loads):
        i = wslot_ctr[0] % NW; wslot_ctr[0] += 1
        for (o, src) in loads:
            dma("pool", o(wb[i]), src, writes=[B_wb[i]])
        dma("sp", wsc_h[ch], wb[i], reads=[B_wb[i]], writes=[B_wsc[ch]])

    def wsrc(h, ncolsW, c0, nk, ncols):
        return AP(tensor=h, offset=c0, ap=[[ncolsW, 128], [128 * ncolsW, nk], [1, ncols]])

    def v3(nk, ncols, c_lo=0, c_n=None):
        c_n = ncols if c_n is None else c_n
        return lambda w: w[:, 0:nk * ncols].rearrange("p (k c) -> p k c", k=nk)[:, :, c_lo:c_lo + c_n]

    def std_chunks(ch0, h, ncolsW, c0, n):
        for i in range(n):
            cast_store(ch0 + i, [(v3(16, 128), wsrc(h, ncolsW, c0 + i * 128, 16, 128))])

    std_chunks(CH_Q, win_h, INW, C_Q, 8); std_chunks(CH_K, win_h, INW, C_K, 2)
    std_chunks(CH_V, win_h, INW, C_V, 2); std_chunks(CH_IQ, win_h, INW, C_IQ, 8)
    cast_store(CH_IK, [(v3(16, 128, 0, 64), wsrc(win_h, INW, C_IK, 16, 64)),
                       (v3(16, 128, 64, 64), wsrc(win_h, INW, C_IK, 16, 64))])
    cast_store(CH_IW, [(v3(16, 16), wsrc(win_h, INW, C_IW, 16, 16))])
    std_chunks(CH_PL, win_h, INW, C_PL, 8); std_chunks(CH_GA, win_h, INW, C_GA, 16)
    std_chunks(CH_GP, win_h, INW, C_GP, 16)
    for i in range(8):
        cast_store(CH_AB + i, [(v3(8, 256), wsrc(wab_h, D, i * 256, 8, 256))])
        cast_store(CH_PB + i, [(v3(8, 256), wsrc(wpb_h, D, i * 256, 8, 256))])
    cast_store(CH_PW, [((lambda w, g=g: w[:, g * 512:(g + 1) * 512].rearrange("p (k c) -> p k c", k=2)),
                        AP(tensor=poolw_h, offset=g * 65536, ap=[[256, 128], [32768, 2], [1, 256]]))
                       for g in range(4)])
    std_chunks(CH_WO, wout_h, D, 0, 16); std_chunks(CH_PQ, wpq_h, D, 0, 16)

    L = Lay()
    wst = []; B_wst = []
    for i in range(2):
        v, b = L.take([16, 256], BF16, f"wst{i}"); wst.append(v); B_wst.append(b)
    modrow, B_modrow = L.take([1, 256], F32, "modrow")
    oh2v_full, B_oh2 = L.take([384], F32, "oh2")
    cact, B_cact = L.take([16], F32, "cact"); cactb, B_cactb = L.take([16], BF16, "cactb")
    keysn, B_keysn = L.take([16, 128], F32, "keysn")
    rb33, B_rb = L.take([8], F32, "rb33")
    rb31, B_rb31 = L.take([8], F32, "rb31")
    fdsb, B_fdsb = L.take([384], F32, "fdsb")
    hank, B_hank = L.take([2, 128], F32, "hank")
    brow, B_brow = L.take([256], F32, "brow")
    grow, B_grow = L.take([2, 2048], F32, "grow")

    dma("sp", keysn[:, :, :], AP(tensor=keys_h, offset=0, ap=[[128, 128], [16384, 16], [1, 128]]), writes=[B_keysn])
    ki = wslot_ctr[0] % NW; wslot_ctr[0] += 1
    for hp in range(16):
        bank = 2 + (hp % 2)
        op("pe", lambda e, hp=hp, bank=bank: e.transpose(pb[bank][:, 0:128], keysn[:, hp, :], identf[:, :]),
           reads=[B_keysn, B_cst], writes=[B_pb[bank]])
        op("act", lambda e, hp=hp, bank=bank: e.activation(wb[ki][:, hp * 128:(hp + 1) * 128], pb[bank][:, 0:128], AF.Copy),
           reads=[B_pb[bank]], writes=[B_wb[ki]])
    dma("sp", wsc_h[CH_KEYS], wb[ki], reads=[B_wb[ki]], writes=[B_wsc[CH_KEYS]])

    dma("sp", cact[:, :], cT_h.ap(), writes=[B_cact])
    op("act", lambda e: e.activation(cactb[:, :], cact[:, :], AF.Silu), reads=[B_cact], writes=[B_cactb])
    dma("sp", grow[0:1, 0, :], g1_h.ap(), writes=[B_grow])
    dma("sp", grow[0:1, 1, :], g2_h.ap(), writes=[B_grow])
    CGW = 256
    for cg in range(6 * D // CGW):
        i = cg % 2
        dma("pool", wst[i][:, :, :], AP(tensor=wada_h, offset=cg * CGW, ap=[[6 * D, 128], [128 * 6 * D, 16], [1, CGW]]),
            writes=[B_wst[i]])
        dma("sp", brow[0:1, :], AP(tensor=bada_h, offset=cg * CGW, ap=[[0, 1], [1, CGW]]), writes=[B_brow])
        for kc in range(16):
            mm(pb[0][0:1, 0:CGW], cactb[:, kc:kc + 1], wst[i][:, kc, :], kc == 0, kc == 15,
               reads=[B_cactb, B_wst[i]], writes=[B_pb[0]])
        op("dve", lambda e: e.tensor_tensor(modrow[0:1, 0, :], pb[0][0:1, 0:CGW], brow[0:1, :], ALU.add),
           reads=[B_pb[0], B_brow], writes=[B_modrow])
        seg = (cg * CGW) // D
        if seg in (1, 4):
            gsel = 0 if seg == 1 else 1
            cs = (cg * CGW) % D
            op("dve", lambda e, gsel=gsel, cs=cs: e.scalar_tensor_tensor(
                modrow[0:1, 0, :], modrow[0:1, 0, :], 1.0, grow[0:1, gsel, cs:cs + CGW], ALU.add, ALU.mult),
               reads=[B_modrow, B_grow], writes=[B_modrow])
        dma("sp", AP(tensor=modv_h, offset=cg * CGW, ap=[[0, 1], [1, CGW]]), modrow[0:1, 0, :],
            reads=[B_modrow], writes=[B_modv])
    MV_SH1, MV_S1, MV_GT1, MV_SH2, MV_S2, MV_GT2 = [i * D for i in range(6)]

    op("dve", lambda e: e.memset(rb33[0:33, :], 1.0), writes=[B_rb])
    dma("sp", rb33[0:32, :], relb_h.ap(), reads=[], writes=[B_rb])
    dma("sp", rb31[0:32, :], AP(tensor=relb_h, offset=31 * 8, ap=[[0, 32], [1, 8]]), writes=[B_rb31])
    op("dve", lambda e: e.tensor_tensor(rb33[0:32, :], rb33[0:32, :], rb31[0:32, :], ALU.subtract),
       reads=[B_rb, B_rb31], writes=[B_rb])
    oh2v = oh2v_full[0:33, :]
    dma("sp", oh2v, cst_h["oh2"].ap(), writes=[B_oh2])
    mm(pb[1][0:8, 0:384], rb33[0:33, :], oh2v, True, True, reads=[B_rb, B_oh2], writes=[B_pb[1]])
    op("act", lambda e: e.activation(fdsb[0:8, :], pb[1][0:8, 0:384], AF.Copy), reads=[B_pb[1]], writes=[B_fdsb])
    dma("sp", fd_h.ap(), fdsb[0:8, :], reads=[B_fdsb], writes=[B_fd])
    for h in range(8):
        for dl in range(2):
            k = (h * 2 + dl) % 2
            dma("sp", hank[:, k, :], AP(tensor=fd_h, offset=h * 384 + 128 * dl, ap=[[1, 128], [1, 128]]),
                reads=[B_fd], writes=[B_hank])
            bank = 2 + k
            mm(pb[bank][:, 0:128], hank[:, k, :], antif[:, :], True, True, reads=[B_hank, B_cst], writes=[B_pb[bank]])
            op("act", lambda e, h=h, dl=dl, bank=bank: e.activation(biasT[:, h, dl, 0, :], pb[bank][:, 0:128], AF.Copy),
               reads=[B_pb[bank]], writes=[B_bias])
            op("dve", lambda e, h=h, dl=dl, bank=bank: e.tensor_tensor(
                biasT[:, h, dl, 1, :], pb[bank][:, 0:128], biasT[:, h, dl, 0, :], ALU.subtract),
               reads=[B_pb[bank], B_bias], writes=[B_bias])
    dump("biasT", biasT[:, :, :, :, :], [B_bias])
    dump("modv", modv_h.ap(), [B_modv])

    sc.barrier()
    B_uv = Buf("uvtab")
    LU = Lay(); stg = []; B_stg = []
    for i in range(4):
        v_, b_ = LU.take([4, D], BF16, f"stg{i}"); stg.append(v_); B_stg.append(b_)
    uctr = 0
    for blk in range(32):
        for half, th in ((0, pu_h), (1, pv_h)):
            k = uctr % 4; uctr += 1
            dma("pool", stg[k][:, :, :], AP(tensor=th, offset=blk * 512 * D, ap=[[4 * D, 128], [D, 4], [1, D]]),
                writes=[B_stg[k]])
            dma("sp", AP(tensor=uv_h, offset=blk * 512 * 2 * D + half * D, ap=[[4 * 2 * D, 128], [2 * D, 4], [1, D]]),
                stg[k][:, :, :], reads=[B_stg[k]], writes=[B_uv])
    sc.barrier()

    LA = Lay()
    bcA, B_bcA = LA.take([D], F32, "bcA"); bcB, B_bcB = LA.take([D], F32, "bcB")
    qT, B_qT = LA.take([8, T], BF16, "qT"); iqT, B_iqT = LA.take([8, T], BF16, "iqT")
    attnT, B_attnT = LA.take([8, T], BF16, "attnT")
    acc, B_acc = LA.take([S], F32, "acc"); mneg, B_mneg = LA.take([S], BF16, "mneg")
    rl = []; B_rl = []; pT = []; B_pT = []
    for i in range(2):
        v, b = LA.take([512], BF16, f"rl{i}"); rl.append(v); B_rl.append(b)
        v, b = LA.take([512], BF16, f"pT{i}"); pT.append(v); B_pT.append(b)
    attn_tm, B_atm = LA.take([1024], BF16, "attn_tm")
    iw, B_iw = LA.take([NSUB, 16], F32, "iw")
    m8, B_m8 = LA.take([8], F32, "m8")
    sqt, B_sqt = LA.take([T], BF16, "sqt"); rtq, B_rtq = LA.take([T], F32, "rtq")
    junk8, B_junk8 = LA.take([S // 2], BF16, "junk8"); junk8 = junk8.bitcast(mybir.dt.uint8)
    rdn, _ = LA.take([4], F32, "rdn"); B_rdn = [Buf("rdn0"), Buf("rdn1")]
    bs, B_bs = LA.take([8], F32, "bs"); Wt, B_Wt = LA.take([32], F32, "Wt"); W2t, B_W2t = LA.take([32], F32, "W2t")
    plb = []; B_plb = []
    for i in range(2):
        v, b = LA.take([16 + T], F32, f"plb{i}"); plb.append(v); B_plb.append(b)
    ptmp = []; B_ptmp = []
    for i in range(2):
        v, b = LA.take([16 + T], F32, f"ptmp{i}"); ptmp.append(v); B_ptmp.append(b)
    praw, B_praw = LA.take([8, T], BF16, "praw"); pooledT, B_pooled = LA.take([8, T], BF16, "pooledT")
    sga, B_sga = LA.take([16, T], BF16, "sga"); sgp, B_sgp = LA.take([16, T], BF16, "sgp")
    qsb = []; B_qsb = []
    for i in range(2):
        v, b = LA.take([T], BF16, f"qsb{i}"); qsb.append(v); B_qsb.append(b)
    LB = Lay()
    LB.take([D], F32, "bcA"); LB.take([D], F32, "bcB")
    LB.take([8, T], BF16, "qT_"); LB.take([8, T], BF16, "iqT_"); LB.take([8, T], BF16, "attnT_")
    mergedT, B_mrg = LB.take([16, T], BF16, "mergedT")
    sg = []; B_sg = []
    for i in range(4):
        v, b = LB.take([T], F32, f"sg{i}"); sg.append(v); B_sg.append(b)
    rtmp, B_rtmp = LB.take([NSUB, 128], F32, "rtmp")
    assert LB.off <= 2 * 4 * D + 3 * 8 * T * 2 + 4 * S, LB.off
    LP = Lay()
    LP.take([D], F32, "bcA"); LP.take([D], F32, "bcB")
    mtop, B_mtop = LP.take([16, 16], F32, "mtop"); ixu, B_ixu = LP.take([16, 16], U32, "ixu")
    ixf, B_ixf = LP.take([16, 16], F32, "ixf")
    best, B_best = LP.take([8, 16], F32, "best"); posu, B_posu = LP.take([8, 16], U32, "posu")
    pa_u, B_pau = LP.take([128], U32, "pa_u"); pb_u, B_pbu = LP.take([128], U32, "pb_u")
    pa_f, B_paf = LP.take([128], F32, "pa_f"); pb_f, B_pbf = LP.take([128], F32, "pb_f")
    ia, B_ia = LP.take([128], F32, "ia"); ib, B_ib = LP.take([128], F32, "ib")
    eidf, B_eidf = LP.take([128], F32, "eidf")
    gsum, B_gsum = LP.take([8], F32, "gsum")
    eid = []; B_eid = []; gate = []; B_gate = []
    for j in range(NSUB):
        v, b = LP.take([128], I32, f"eid{j}"); eid.append(v); B_eid.append(b)
        v, b = LP.take([8, 16], F32, f"gate{j}"); gate.append(v); B_gate.append(b)
    dots, B_dots = LP.take([128], F32, "dots"); coef, B_coef = LP.take([128], F32, "coef")
    LPa = Lay(); LPa.off = LP.off
    qpT, B_qpT = LPa.take([16, T], BF16, "qpT")
    sub, B_sub = LPa.take([16, 128], F32, "sub")
    cand, B_cand = LPa.take([8, 256], F32, "cand")
    ohb, B_ohb = LPa.take([128, 16], F32, "ohb")
    LPb = Lay(); LPb.off = LPa.off
    prod = []; B_prod = []
    for i in range(2):
        v_, b_ = LPb.take([D], BF16, f"prod{i}"); prod.append(v_); B_prod.append(b_)
    NG = 8
    gb = []; B_gb = []
    gb.append(av(0, [2 * D], BF16)); B_gb.append([B_bcA])
    for i in range(1, 4):
        v_, b_ = LPb.take([2 * D], BF16, f"gb{i}"); gb.append(v_); B_gb.append([b_])
    for i in range(3):
        gb.append(wb_all[:, i * 2 * D:(i + 1) * 2 * D]); B_gb.append([B_wb[2 * i], B_wb[2 * i + 1]])
    gb.append(hT[:, :, :].rearrange("p k t -> p (k t)")); B_gb.append([B_hT])
    NPAR = 4
    dotg = []; B_dotg = []; gact = []; B_gact = []; dg = []; B_dg = []
    for i in range(NPAR):
        v_, b_ = LPb.take([2], F32, f"dotg{i}"); dotg.append(v_); B_dotg.append(b_)
        v_, b_ = LPb.take([2], F32, f"gact{i}"); gact.append(v_); B_gact.append(b_)
        v_, b_ = LPb.take([2, 128], BF16, f"dg{i}"); dg.append(v_); B_dg.append([Buf(f"dg{i}_0"), Buf(f"dg{i}_1")])
    rt2, B_rt2 = LPb.take([512], F32, "rt2")

    def flat(v):
        return v

    def rmsnorm_to_hT(j, s_off, b_off, first):
        c0 = 0 if first else 4
        op("act", lambda e: e.activation(hTM[:, j, :], xres[:, j, :], AF.Square, accum_out=stat[:, c0 + j:c0 + j + 1]),
           reads=[B_x[j]], writes=[B_hTM, B_stat])
        op("act", lambda e: e.activation(stat[:, 8 + j:9 + j], stat[:, c0 + j:c0 + j + 1], AF.Sqrt,
                                         bias=epst[:, 0:1], scale=1.0 / D), reads=[B_stat, B_cst], writes=[B_stat])
        op("dve", lambda e: e.reciprocal(stat[:, c0 + j:c0 + j + 1], stat[:, 8 + j:9 + j]), reads=[B_stat], writes=[B_stat])
        op("dve", lambda e: e.scalar_tensor_tensor(hTM[:, j, :], xres[:, j, :], stat[:, c0 + j:c0 + j + 1], bcA_ap(),
                                                   ALU.mult, ALU.mult), reads=[B_x[j], B_stat, B_bcA], writes=[B_hTM])
        op("dve", lambda e: e.tensor_tensor(hTM[:, j, :], hTM[:, j, :], bcB_ap(), ALU.add), reads=[B_hTM, B_bcB], writes=[B_hTM])
        for half in range(2):
            bank = 2 + half
            pbb = pb[bank][:, :].bitcast(BF16)
            for k8 in range(8):
                kc = half * 8 + k8
                op("pe", lambda e, kc=kc, k8=k8, pbb=pbb: e.transpose(pbb[:, k8 * 128:(k8 + 1) * 128],
                                                                       hTM[:, j, kc * 128:(kc + 1) * 128], identb[:, :]),
                   reads=[B_hTM, B_cst], writes=[B_pb[bank]])
            eng = "act" if half == 0 else "dve"
            src = pbb.rearrange("p (k t) -> p k t", k=8)
            dst = hT[:, half * 8:(half + 1) * 8, j * 128:(j + 1) * 128]
            if eng == "act":
                op("act", lambda e, src=src, dst=dst: e.activation(dst, src, AF.Copy), reads=[B_pb[bank]], writes=[B_hT])
            else:
                op("dve", lambda e, src=src, dst=dst: e.tensor_copy(dst, src), reads=[B_pb[bank]], writes=[B_hT])

    def bcA_ap():
        return arena[:, 0:D]

    def bcB_ap():
        return arena[:, D:2 * D]

    def load_bc(which, off):
        ap_ = bcA_ap() if which == 0 else bcB_ap()
        dma("sp", ap_, bcast_row(modv_h, off), reads=[B_modv], writes=[B_bcA if which == 0 else B_bcB])

    pbsel = [0]

    def next_bank01():
        pbsel[0] ^= 1
        return pbsel[0]

    def proj_fm(ch, nk, rhs_fn, rhs_bufs, ncols=T, col0=0, ldw=None):
        w, bw = ldw if ldw is not None else load_chunk(ch)
        bank = next_bank01()
        for kc in range(nk):
            lhsT = w[:, kc * 128:(kc + 1) * 128]
            mm(pb[bank][:, 0:ncols], lhsT, rhs_fn(kc), kc == 0, kc == nk - 1, reads=[bw] + rhs_bufs, writes=[B_pb[bank]])
        return bank

    hT_rhs = lambda kc: hT[:, kc, :]

    for ti in (tile_list if tile_list is not None else range(NT)):
        t0 = ti * T
        for j in range(NSUB):
            dma("sp", xres[:, j, :], x_h[t0 + j * 128:t0 + (j + 1) * 128, :], writes=[B_x[j]])
        load_bc(0, MV_S1); load_bc(1, MV_SH1)
        for j in range(NSUB):
            rmsnorm_to_hT(j, MV_S1, MV_SH1, True)
        if ti == 0:
            dump("hT", hT[:, :, :], [B_hT])

        if do_mix:
            for c in range(8):
                bank = proj_fm(CH_IQ + c, 16, hT_rhs, [B_hT])
                op("act", lambda e, bank=bank, c=c: e.activation(iqT[:, c, :], pb[bank][:, 0:T], AF.Copy),
                   reads=[B_pb[bank]], writes=[B_iqT])
            bank = proj_fm(CH_IK, 16, hT_rhs, [B_hT])
            op("act", lambda e, bank=bank: e.activation(ikT[:, t0:t0 + T], pb[bank][:, 0:T], AF.Copy),
               reads=[B_pb[bank]], writes=[B_ik])
            w, bw = load_chunk(CH_IW)
            for j in range(NSUB):
                bank = next_bank01()
                for kc in range(16):
                    mm(pb[bank][:, 0:16], hT[:, kc, j * 128:(j + 1) * 128], w[:, kc * 16:(kc + 1) * 16],
                       kc == 0, kc == 15, reads=[B_hT, bw], writes=[B_pb[bank]])
                op("act", lambda e, bank=bank, j=j: e.activation(iw[:, j, :], pb[bank][:, 0:16], AF.Copy),
                   reads=[B_pb[bank]], writes=[B_iw])

            def qkv_block(ti=ti, t0=t0):
                for c in range(10):
                    isq = c < 8
                    bank = proj_fm((CH_Q + c) if isq else (CH_K + c - 8), 16, hT_rhs, [B_hT])
                    op("act", lambda e, bank=bank: e.activation(sqt, pb[bank][:, 0:T], AF.Square),
                       reads=[B_pb[bank]], writes=[B_sqt])
                    mm(pb[2][:, 0:T], onesb[:, :], sqt, True, True, reads=[B_cst, B_sqt], writes=[B_pb[2]])
                    op("act", lambda e: e.activation(rtq, pb[2][:, 0:T], AF.Ln, bias=epst[:, 0:1], scale=1.0 / 128),
                       reads=[B_pb[2], B_cst], writes=[B_rtq])
                    op("act", lambda e: e.activation(rtq, rtq, AF.Exp, scale=-0.5), reads=[B_rtq], writes=[B_rtq])
                    if isq:
                        dst = qT[:, c, :]; bd = B_qT; gcol = 0
                    else:
                        dst = kT[:, c - 8, t0:t0 + T]; bd = B_kT; gcol = 1
                    qk = c % 2
                    op("act", lambda e, bank=bank, gcol=gcol, qk=qk: e.activation(qsb[qk], pb[bank][:, 0:T], AF.Copy,
                                                                                 scale=gqs[:, gcol:gcol + 1]),
                       reads=[B_pb[bank], B_gqs], writes=[B_qsb[qk]])
                    op("pool", lambda e, dst=dst, qk=qk: e.tensor_tensor(dst, qsb[qk], rtq, ALU.mult),
                       reads=[B_qsb[qk], B_rtq], writes=[bd])
                for g in range(2):
                    w, bw = load_chunk(CH_V + g)
                    for j in range(NSUB):
                        bank = next_bank01()
                        for kc in range(16):
                            mm(pb[bank][:, 0:128], hT[:, kc, j * 128:(j + 1) * 128], w[:, kc * 128:(kc + 1) * 128],
                               kc == 0, kc == 15, reads=[B_hT, bw], writes=[B_pb[bank]])
                        op("act", lambda e, bank=bank, g=g, j=j: e.activation(vaug[:, ti * NSUB + j, g, 0:128], pb[bank][:, 0:128], AF.Copy),
                           reads=[B_pb[bank]], writes=[B_v])
            if ti == 0:
                dump("iw", iw[:, :, :], [B_iw])

            def indexer(j, ti=ti):
                qi = ti * NSUB + j; Lk = (qi + 1) * 128
                qs = slice(j * 128, (j + 1) * 128)
                use_mask = qi >= 2
                for ih in range(16):
                    c = ih // 2; p0 = (ih % 2) * 64
                    for s0 in range(0, Lk, 512):
                        n = min(512, Lk - s0)
                        bank = next_bank01(); r = (ih + s0 // 512) % 2
                        mm(pb[bank][:, 0:n], iqT[p0:p0 + 64, c, qs], ikT[p0:p0 + 64, s0:s0 + n], True, True,
                           reads=[B_iqT, B_ik], writes=[B_pb[bank]])
                        op("act", lambda e, bank=bank, r=r, n=n: e.activation(rl[r][:, 0:n], pb[bank][:, 0:n], AF.Relu),
                           reads=[B_pb[bank]], writes=[B_rl[r]])
                        last = (s0 + n == Lk)
                        if ih == 0:
                            nb = n - 128 if last else n
                            if nb > 0:
                                op("dve", lambda e, r=r, s0=s0, nb=nb, j=j: e.tensor_scalar(
                                    acc[:, s0:s0 + nb], rl[r][:, 0:nb], iw[:, j, 0:1], None, ALU.mult),
                                   reads=[B_rl[r], B_iw], writes=[B_acc])
                            if last:
                                op("dve", lambda e, r=r, s0=s0, n=n, j=j: e.scalar_tensor_tensor(
                                    acc[:, s0 + n - 128:s0 + n], rl[r][:, n - 128:n], iw[:, j, 0:1], cmask[:, :],
                                    ALU.mult, ALU.add), reads=[B_rl[r], B_iw, B_cst], writes=[B_acc])
                        else:
                            op("dve", lambda e, r=r, s0=s0, n=n, j=j, ih=ih: e.scalar_tensor_tensor(
                                acc[:, s0:s0 + n], rl[r][:, 0:n], iw[:, j, ih:ih + 1], acc[:, s0:s0 + n],
                                ALU.mult, ALU.add), reads=[B_rl[r], B_iw, B_acc], writes=[B_acc])
                if qi == 2:
                    dump("acc", acc[:, 0:384], [B_acc])
            def topk(j, ti=ti):
                qi = ti * NSUB + j; Lk = (qi + 1) * 128
                qs = slice(j * 128, (j + 1) * 128)
                use_mask = qi >= 2
                if use_mask:
                    KB = 24
                    op("dve", lambda e: e.max(out=m8, in_=acc[:, 0:Lk]), reads=[B_acc], writes=[B_m8])
                    op("dve", lambda e: e.tensor_reduce(out=bs[:, 4:5], in_=acc[:, 0:Lk - 128], axis=AX.X, op=ALU.min),
                       reads=[B_acc], writes=[B_bs])
                    op("dve", lambda e: e.tensor_tensor(bs[:, 5:6], m8[:, 0:1], bs[:, 4:5], ALU.subtract), reads=[B_m8, B_bs], writes=[B_bs])
                    op("dve", lambda e: e.tensor_scalar(Wt[:, :], pow2[:, :], bs[:, 5:6], None, ALU.mult), reads=[B_cst, B_bs], writes=[B_Wt])
                    op("dve", lambda e: e.tensor_scalar(W2t[:, :], Wt[:, :], 2.0, None, ALU.mult), reads=[B_Wt], writes=[B_W2t])
                    op("dve", lambda e: e.tensor_tensor(bs[:, 0:1], bs[:, 4:5], Wt[:, 0:1], ALU.add), reads=[B_bs, B_Wt], writes=[B_bs])
                    for kb in range(KB):
                        op("dve", lambda e: e.tensor_scalar(junk8[:, 0:Lk], acc[:, 0:Lk], bs[:, 0:1], None, ALU.is_ge, ALU.add,
                                                            accum_out=bs[:, 1:2]), reads=[B_acc, B_bs], writes=[B_junk8, B_bs])
                        op("dve", lambda e, kb=kb: e.tensor_scalar(bs[:, 2:3], bs[:, 1:2], 256.0, W2t[:, kb + 1:kb + 2], ALU.is_ge, ALU.mult),
                           reads=[B_bs, B_W2t], writes=[B_bs])
                        op("dve", lambda e, kb=kb: e.scalar_tensor_tensor(bs[:, 0:1], bs[:, 2:3], Wt[:, kb + 1:kb + 2], bs[:, 0:1],
                                                                          ALU.subtract, ALU.add), reads=[B_bs, B_Wt], writes=[B_bs])
                    op("dve", lambda e: e.tensor_tensor(bs[:, 3:4], bs[:, 0:1], Wt[:, KB:KB + 1], ALU.subtract), reads=[B_bs, B_Wt], writes=[B_bs])
                    op("dve", lambda e: e.tensor_scalar(mneg[:, 0:Lk], acc[:, 0:Lk], bs[:, 3:4], -1.0, ALU.is_lt, ALU.mult),
                       reads=[B_acc, B_bs], writes=[B_mneg])
                    if qi == 2:
                        dump("mneg", mneg[:, 0:384], [B_mneg])
            def attention(j, ti=ti):
                qi = ti * NSUB + j; Lk = (qi + 1) * 128
                qs = slice(j * 128, (j + 1) * 128)
                use_mask = qi >= 2
                items = [(h, grp) for h in range(8) for grp in range(0, qi + 1, 4)]

                def QK(i):
                    h, grp = items[i]; g = h // 4
                    scs = list(range(grp, min(grp + 4, qi + 1)))
                    lbk = 4 + (i % 2)
                    for sc_ in scs:
                        col = (sc_ - grp) * 128
                        o = pb[lbk][:, col:col + 128]
                        extra = (1 if use_mask else 0) + (2 if sc_ >= qi - 1 else 0)
                        mm(o, kT[:, g, sc_ * 128:(sc_ + 1) * 128], qT[:, h, qs], True, extra == 0,
                           reads=[B_kT, B_qT], writes=[B_pb[lbk]])
                        if use_mask:
                            extra -= 1
                            mm(o, mneg[:, sc_ * 128:(sc_ + 1) * 128], identbig[:, :], False, extra == 0,
                               reads=[B_mneg, B_cst], writes=[B_pb[lbk]])
                        if sc_ >= qi - 1:
                            dl = qi - sc_
                            mm(o, biasT[:, h, dl, 0, :], identb[:, :], False, False, reads=[B_bias, B_cst], writes=[B_pb[lbk]])
                            mm(o, biasT[:, h, dl, 1, :], identb[:, :], False, True, reads=[B_bias, B_cst], writes=[B_pb[lbk]])

                def EXP_PV(i):
                    h, grp = items[i]; g = h // 4
                    pvb = 6 + (h % 2)
                    scs = list(range(grp, min(grp + 4, qi + 1)))
                    lbk = 4 + (i % 2); pr = i % 2
                    ncol = len(scs) * 128
                    op("act", lambda e, lbk=lbk, pr=pr, ncol=ncol: e.activation(pT[pr][:, 0:ncol], pb[lbk][:, 0:ncol], AF.Exp),
                       reads=[B_pb[lbk]], writes=[B_pT[pr]])
                    for sc_ in scs:
                        col = (sc_ - grp) * 128
                        mm(pb[pvb][:, 0:129], pT[pr][:, col:col + 128], vaug[:, sc_, g, 0:129], sc_ == 0, sc_ == qi,
                           reads=[B_pT[pr], B_v], writes=[B_pb[pvb]])
                    if scs[-1] == qi:
                        ra = h % 2
                        op("act", lambda e, pvb=pvb, ra=ra: e.activation(rdn[:, ra:ra + 1], pb[pvb][:, 128:129], AF.Ln),
                           reads=[B_pb[pvb]], writes=[B_rdn[ra]])
                        op("act", lambda e, ra=ra: e.activation(rdn[:, ra:ra + 1], rdn[:, ra:ra + 1], AF.Exp, scale=-1.0),
                           reads=[B_rdn[ra]], writes=[B_rdn[ra]])
                        op("act", lambda e, pvb=pvb, h=h, ra=ra: e.activation(attn_tm[:, h * 128:(h + 1) * 128], pb[pvb][:, 0:128], AF.Copy,
                                                                              scale=rdn[:, ra:ra + 1]),
                           reads=[B_pb[pvb], B_rdn[ra]], writes=[B_atm])

                QK(0)
                for i in range(len(items)):
                    if i + 1 < len(items):
                        QK(i + 1)
                    EXP_PV(i)
                pbb = pb[3][:, :].bitcast(BF16)
                for h in range(8):
                    op("pe", lambda e, h=h: e.transpose(pbb[:, h * 128:(h + 1) * 128], attn_tm[:, h * 128:(h + 1) * 128], identb[:, :]),
                       reads=[B_atm, B_cst], writes=[B_pb[3]])
                op("act", lambda e, j=j: e.activation(attnT[:, :, j * 128:(j + 1) * 128], pbb.rearrange("p (k t) -> p k t", k=8), AF.Copy),
                   reads=[B_pb[3]], writes=[B_attnT])

            def pool_block(ti=ti):
                for c in range(8):
                    g = c // 2; wdw = (2, 4, 8, 16)[g]
                    bank = proj_fm(CH_PL + c, 16, hT_rhs, [B_hT])
                    pbuf = plb[c % 2]; bpl = B_plb[c % 2]
                    op("act", lambda e, bank=bank, pbuf=pbuf: e.activation(pbuf[:, 16:16 + T], pb[bank][:, 0:T], AF.Copy),
                       reads=[B_pb[bank]], writes=[bpl])
                    op("pool", lambda e, pbuf=pbuf, c=c: e.tensor_copy(pbuf[:, 0:16], halo[:, c, :]),
                       reads=[B_halo], writes=[bpl])
                    op("pool", lambda e, pbuf=pbuf, c=c: e.tensor_copy(halo[:, c, :], pbuf[:, T:T + 16]),
                       reads=[bpl], writes=[B_halo])
                    cur = pbuf; bcur = bpl; k = 1; st = 0; lo = 1
                    while k < wdw:
                        nxt = ptmp[st % 2]; bn = B_ptmp[st % 2]
                        op("pool", lambda e, cur=cur, nxt=nxt, k=k, lo=lo: e.tensor_tensor(
                            nxt[:, lo:16 + T], cur[:, lo:16 + T], cur[:, lo - k:16 + T - k], ALU.add),
                           reads=[bcur], writes=[bn])
                        cur = nxt; bcur = bn; k *= 2; lo += k; st += 1
                    oth = ptmp[st % 2]; both = B_ptmp[st % 2]
                    op("pool", lambda e, cur=cur, oth=oth, wdw=wdw: e.tensor_scalar(oth[:, 16:16 + T], cur[:, 16:16 + T], 1.0 / wdw, 0.0, ALU.mult, ALU.add),
                       reads=[bcur], writes=[both])
                    op("pool", lambda e, oth=oth, pbuf=pbuf, c=c: e.tensor_tensor(praw[:, c, :], oth[:, 16:16 + T], pbuf[:, 16:16 + T], ALU.subtract),
                       reads=[both, bpl], writes=[B_praw])
                    if ti == 0:
                        op("pool", lambda e, cur=cur, oth=oth, g=g: e.tensor_tensor(oth[:, 16:32], cur[:, 16:32], invc[:, g, :], ALU.mult),
                           reads=[bcur, B_cst], writes=[both])
                        op("pool", lambda e, oth=oth, pbuf=pbuf, c=c: e.tensor_tensor(praw[:, c, 0:16], oth[:, 16:32], pbuf[:, 16:32], ALU.subtract),
                           reads=[both, bpl, B_praw], writes=[B_praw])
                w, bw = load_chunk(CH_PW)
                for g in range(4):
                    for eh in range(2):
                        bank = next_bank01()
                        for kc in range(2):
                            lhsT = w[:, g * 512 + kc * 256 + eh * 128: g * 512 + kc * 256 + eh * 128 + 128]
                            mm(pb[bank][:, 0:T], lhsT, praw[:, 2 * g + kc, :], kc == 0, kc == 1, reads=[bw, B_praw], writes=[B_pb[bank]])
                        op("act", lambda e, bank=bank, g=g, eh=eh: e.activation(pooledT[:, 2 * g + eh, :], pb[bank][:, 0:T], AF.Copy,
                                                                                scale=pscale[:, 2 * g + eh:2 * g + eh + 1]),
                           reads=[B_pb[bank], B_cst], writes=[B_pooled])
                if ti == 0:
                    dump("pooledT", pooledT[:, :, :], [B_pooled])


            def gates(c_lo, c_hi):
                for c in range(c_lo, c_hi):
                    bank = proj_fm(CH_GA + c, 16, hT_rhs, [B_hT])
                    op("act", lambda e, bank=bank, c=c: e.activation(sga[:, c, :], pb[bank][:, 0:T], AF.Sigmoid),
                       reads=[B_pb[bank]], writes=[B_sga])
                    bank = proj_fm(CH_GP + c, 16, hT_rhs, [B_hT])
                    op("act", lambda e, bank=bank, c=c: e.activation(sgp[:, c, :], pb[bank][:, 0:T], AF.Sigmoid),
                       reads=[B_pb[bank]], writes=[B_sgp])

            indexer(0); qkv_block(); pool_block(); gates(0, 8); topk(0)
            if ti == 0:
                dump("qT", qT[:, :, :], [B_qT]); dump("kT", kT[:, :, 0:T], [B_kT])
            for j in range(1, NSUB):
                indexer(j); attention(j - 1)
                if j == 1:
                    gates(8, 16)
                topk(j)
            attention(NSUB - 1)
            if ti == 0:
                dump("attnT", attnT[:, :, :], [B_attnT])

            wab = wpbk = None
            for c in range(16):
                if c % 2 == 0:
                    wab = load_chunk(CH_AB + c // 2); wpbk = load_chunk(CH_PB + c // 2)
                col0 = (c % 2) * 128
                for kc in range(8):
                    mm(pb[4][:, 0:T], wab[0][:, kc * 256 + col0:kc * 256 + col0 + 128], attnT[:, kc, :], kc == 0, kc == 7,
                       reads=[wab[1], B_attnT], writes=[B_pb[4]])
                for kc in range(8):
                    mm(pb[5][:, 0:T], wpbk[0][:, kc * 256 + col0:kc * 256 + col0 + 128], pooledT[:, kc, :], kc == 0, kc == 7,
                       reads=[wpbk[1], B_pooled], writes=[B_pb[5]])
                sa = (c % 2) * 2
                op("dve", lambda e, sa=sa, c=c: e.tensor_tensor(sg[sa], sga[:, c, :], pb[4][:, 0:T], ALU.mult),
                   reads=[B_sga, B_pb[4]], writes=[B_sg[sa], B_acc])
                op("dve", lambda e, sa=sa, c=c: e.tensor_tensor(sg[sa + 1], sgp[:, c, :], pb[5][:, 0:T], ALU.mult),
                   reads=[B_sgp, B_pb[5]], writes=[B_sg[sa + 1], B_acc])
                op("dve", lambda e, sa=sa, c=c: e.tensor_tensor(mergedT[:, c, :], sg[sa], sg[sa + 1], ALU.add),
                   reads=[B_sg[sa], B_sg[sa + 1]], writes=[B_mrg, B_acc])
            if ti == 0:
                dump("mergedT", mergedT[:, :, :], [B_mrg])

            load_bc(0, MV_GT1)
            for cc in range(16):
                w, bw = load_chunk(CH_WO + cc)
                bank = next_bank01()
                for j in range(NSUB):
                    for kc in range(16):
                        mm(pb[bank][:, j * 128:(j + 1) * 128], mergedT[:, kc, j * 128:(j + 1) * 128], w[:, kc * 128:(kc + 1) * 128],
                           kc == 0, kc == 15, reads=[B_mrg, bw], writes=[B_pb[bank]])
                gt = AP(tensor=arena, offset=cc * 128,
                        ap=[[ARENA // 4, 128], [0, NSUB], [1, 128]])
                op("dve", lambda e, bank=bank, gt=gt: e.tensor_tensor(
                    rtmp[:, :, :], pb[bank][:, 0:T].rearrange("p (j c) -> p j c", j=NSUB), gt, ALU.mult),
                   reads=[B_pb[bank], B_bcA], writes=[B_rtmp])
                op("dve", lambda e, cc=cc: e.tensor_tensor(xres[:, :, cc * 128:(cc + 1) * 128], xres[:, :, cc * 128:(cc + 1) * 128],
                                                           rtmp[:, :, :], ALU.add), reads=[B_rtmp] + B_x, writes=B_x)
        if ti == NT - 1:
            dump("x1", xres[:, :, :], B_x)
        sc.barrier()

        if do_peer:
            load_bc(0, MV_S2); load_bc(1, MV_SH2)
            for j in range(NSUB):
                rmsnorm_to_hT(j, MV_S2, MV_SH2, False)
            for c in range(16):
                bank = proj_fm(CH_PQ + c, 16, hT_rhs, [B_hT])
                op("act", lambda e, bank=bank, c=c: e.activation(qpT[:, c, :], pb[bank][:, 0:T], AF.Copy),
                   reads=[B_pb[bank]], writes=[B_qpT])
            wk, bwk = load_chunk(CH_KEYS)
            deferred = []; defer_on = [False]

            def dop(en, fn, reads=(), writes=()):
                if defer_on[0]:
                    deferred.append((en, fn, list(reads), list(writes)))
                else:
                    op(en, fn, reads=reads, writes=writes)

            def flush(n):
                while deferred and n > 0:
                    en, fn, r_, w_ = deferred.pop(0)
                    op(en, fn, reads=r_, writes=w_); n -= 1
            for j in range(NSUB):
                for hp in range(16):
                    bank = 4 + hp // 4
                    mm(pb[bank][:, (hp % 4) * 128:(hp % 4 + 1) * 128], qpT[:, hp, j * 128:(j + 1) * 128],
                       wk[:, hp * 128:(hp + 1) * 128], True, True, reads=[B_qpT, bwk], writes=[B_pb[bank]])
                for b4 in range(4):
                    op("act", lambda e, b4=b4: e.activation(sub[:, b4 * 4:(b4 + 1) * 4, :],
                                                            pb[4 + b4][:, :].rearrange("p (a n) -> p a n", a=4), AF.Copy),
                       reads=[B_pb[4 + b4]], writes=[B_sub])
                defer_on[0] = (NSUB > 1 and j == NSUB - 1)
                for hp in range(16):
                    sv = sub[:, hp, :]
                    dop("dve", lambda e, sv=sv, hp=hp: e.max(out=mtop[:, hp, 0:8], in_=sv), reads=[B_sub], writes=[B_mtop])
                    dop("dve", lambda e, sv=sv, hp=hp: e.max_index(out=ixu[:, hp, 0:8], in_max=mtop[:, hp, 0:8], in_values=sv),
                       reads=[B_sub, B_mtop], writes=[B_ixu])
                    dop("dve", lambda e, sv=sv, hp=hp: e.match_replace(out=sv, in_to_replace=mtop[:, hp, 0:8], in_values=sv, imm_value=-1.0e30),
                       reads=[B_sub, B_mtop], writes=[B_sub])
                    dop("dve", lambda e, sv=sv, hp=hp: e.max(out=mtop[:, hp, 8:16], in_=sv), reads=[B_sub], writes=[B_mtop])
                    dop("dve", lambda e, sv=sv, hp=hp: e.max_index(out=ixu[:, hp, 8:16], in_max=mtop[:, hp, 8:16], in_values=sv),
                       reads=[B_sub, B_mtop], writes=[B_ixu])
                dop("dve", lambda e: e.tensor_copy(ixf[:, :, :], ixu[:, :, :]), reads=[B_ixu], writes=[B_ixf])
                mt_t = mtop.tensor; mt_off = mtop.offset; PST = ARENA // 4
                s1b = AP(tensor=mt_t, offset=mt_off, ap=[[PST, 128], [32, 8], [1, 16], [0, 16]])
                s2b = AP(tensor=mt_t, offset=mt_off + 16, ap=[[PST, 128], [32, 8], [0, 16], [1, 16]])
                candv = cand[:, :, :].rearrange("p h (a b) -> p h a b", a=16)
                dop("dve", lambda e: e.tensor_tensor(candv, s1b, s2b, ALU.add), reads=[B_mtop], writes=[B_cand])
                for h in range(8):
                    cv = cand[:, h, :]
                    dop("dve", lambda e, cv=cv, h=h: e.max(out=best[:, h, 0:8], in_=cv), reads=[B_cand], writes=[B_best])
                    dop("dve", lambda e, cv=cv, h=h: e.max_index(out=posu[:, h, 0:8], in_max=best[:, h, 0:8], in_values=cv),
                       reads=[B_cand, B_best], writes=[B_posu])
                    dop("dve", lambda e, cv=cv, h=h: e.match_replace(out=cv, in_to_replace=best[:, h, 0:8], in_values=cv, imm_value=-1.0e30),
                       reads=[B_cand, B_best], writes=[B_cand])
                    dop("dve", lambda e, cv=cv, h=h: e.max(out=best[:, h, 8:16], in_=cv), reads=[B_cand], writes=[B_best])
                    dop("dve", lambda e, cv=cv, h=h: e.max_index(out=posu[:, h, 8:16], in_max=best[:, h, 8:16], in_values=cv),
                       reads=[B_cand, B_best], writes=[B_posu])
                bt_t = best.tensor; bt_off = best.offset
                b0 = AP(tensor=bt_t, offset=bt_off, ap=[[PST, 128], [16, 8], [0, 16]])
                gj = gate[j]
                dop("dve", lambda e, gj=gj: e.tensor_tensor(gj[:, :, :], best[:, :, :], b0, ALU.subtract), reads=[B_best], writes=[B_gate[j]])
                dop("act", lambda e, gj=gj: e.activation(gj[:, :, :], gj[:, :, :], AF.Exp), reads=[B_gate[j]], writes=[B_gate[j]])
                dop("dve", lambda e, gj=gj: e.tensor_reduce(out=gsum[:, :], in_=gj[:, :, :], axis=AX.X, op=ALU.add),
                   reads=[B_gate[j]], writes=[B_gsum])
                dop("dve", lambda e: e.reciprocal(gsum[:, :], gsum[:, :]), reads=[B_gsum], writes=[B_gsum])
                gs_b = AP(tensor=gsum.tensor, offset=gsum.offset, ap=[[PST, 128], [1, 8], [0, 16]])
                dop("dve", lambda e, gj=gj: e.tensor_tensor(gj[:, :, :], gj[:, :, :], gs_b, ALU.mult), reads=[B_gate[j], B_gsum], writes=[B_gate[j]])
                posf = posu[:, :, :].rearrange("p h r -> p (h r)")
                dop("dve", lambda e: e.tensor_single_scalar(pa_u[:, :], posf, 4, ALU.logical_shift_right), reads=[B_posu], writes=[B_pau])
                dop("dve", lambda e: e.tensor_single_scalar(pb_u[:, :], posf, 15, ALU.bitwise_and), reads=[B_posu], writes=[B_pbu])
                dop("dve", lambda e: e.tensor_copy(pa_f[:, :], pa_u[:, :]), reads=[B_pau], writes=[B_paf])
                dop("dve", lambda e: e.tensor_copy(pb_f[:, :], pb_u[:, :]), reads=[B_pbu], writes=[B_pbf])
                io_b = iota16[:, :].unsqueeze(1).to_broadcast([128, 128, 16])
                for (pf, Bpf, half, dst, Bdst) in ((pa_f, B_paf, 0, ia, B_ia), (pb_f, B_pbf, 1, ib, B_ib)):
                    pfb = pf[:, :].unsqueeze(2).to_broadcast([128, 128, 16])
                    dop("dve", lambda e, pfb=pfb: e.tensor_tensor(ohb[:, :, :], pfb, io_b, ALU.is_equal), reads=[Bpf, B_cst], writes=[B_ohb])
                    ixb = AP(tensor=ixf.tensor, offset=ixf.offset + 16 * half, ap=[[PST, 128], [32, 8], [0, 16], [1, 16]])
                    oh4 = ohb[:, :, :].rearrange("p (h r) a -> p h r a", h=8)
                    dop("dve", lambda e, oh4=oh4, ixb=ixb: e.tensor_tensor(oh4, oh4, ixb, ALU.mult), reads=[B_ohb, B_ixf], writes=[B_ohb])
                    dop("dve", lambda e, dst=dst: e.tensor_reduce(out=dst[:, :], in_=ohb[:, :, :], axis=AX.X, op=ALU.add),
                       reads=[B_ohb], writes=[Bdst])
                dop("dve", lambda e: e.scalar_tensor_tensor(eidf[:, :], ia[:, :], 128.0, ib[:, :], ALU.mult, ALU.add),
                   reads=[B_ia, B_ib], writes=[B_eidf])
                dop("dve", lambda e, j=j: e.tensor_copy(eid[j][:, :], eidf[:, :]), reads=[B_eidf], writes=[B_eid[j]])
                defer_on[0] = False
                if ti == 0 and j == 0:
                    dump("eid", eid[0][:, :], [B_eid[0]]); dump("gate", gate[0][:, :, :], [B_gate[0]])

            load_bc(1, MV_GT2)
            GS = 2; NGRP = 128 // GS
            glist = [(j, g) for j in range(NSUB) for g in range(NGRP)]
            kof = {}
            gctr = 0

            def stage_A(idx):
                j, g = glist[idx]; par = idx % NPAR
                nonlocal_k = []
                for i in range(GS):
                    slot = g * GS + i
                    k = (idx * GS + i) % NG
                    nonlocal_k.append(k)
                    dma("pool", gb[k], uv_h.ap(), reads=[B_eid[j], B_uv], writes=B_gb[k],
                        indirect=bass.IndirectOffsetOnAxis(ap=eid[j][:, slot:slot + 1], axis=0))
                    pk = (idx * GS + i) % 2
                    op("dve", lambda e, k=k, j=j, pk=pk: e.tensor_tensor(prod[pk][:, :], hTM[:, j, :], gb[k][:, 0:D], ALU.mult),
                       reads=[B_hTM] + B_gb[k], writes=[B_prod[pk]])
                    op("act", lambda e, i=i, par=par, pk=pk: e.activation(prod[pk][:, :], prod[pk][:, :], AF.Copy,
                                                                          accum_out=dotg[par][:, i:i + 1]),
                       reads=[B_prod[pk]], writes=[B_prod[pk], B_dotg[par]])
                kof[idx] = nonlocal_k
                op("act", lambda e, par=par: e.activation(gact[par][:, :], dotg[par][:, :], AF.Gelu),
                   reads=[B_dotg[par]], writes=[B_gact[par]])

            def stage_C(idx):
                j, g = glist[idx]; par = idx % NPAR
                gflat = gate[j][:, :, :].rearrange("p h r -> p (h r)")
                for i in range(GS):
                    slot = g * GS + i; k = kof[idx][i]
                    op("dve", lambda e, i=i, par=par, slot=slot, gflat=gflat: e.tensor_scalar(
                        dg[par][:, i, :], identb[:, :], gact[par][:, i:i + 1], gflat[:, slot:slot + 1], ALU.mult, ALU.mult),
                       reads=[B_cst, B_gact[par], B_gate[j]], writes=[B_dg[par][i]])
                    for q4 in range(4):
                        mm(pb[q4][:, :], dg[par][:, i, :], gb[k][:, D + q4 * 512:D + (q4 + 1) * 512], slot == 0, slot == 127,
                           reads=[B_dg[par][i]] + B_gb[k], writes=[B_pb[q4]])
                if g == NGRP - 1:
                    for q4 in range(4):
                        op("dve", lambda e, q4=q4: e.tensor_tensor(rt2[:, :], pb[q4][:, :], arena[:, D + q4 * 512:D + (q4 + 1) * 512], ALU.mult),
                           reads=[B_pb[q4], B_bcB], writes=[B_rt2])
                        op("dve", lambda e, q4=q4, j=j: e.tensor_tensor(xres[:, j, q4 * 512:(q4 + 1) * 512], xres[:, j, q4 * 512:(q4 + 1) * 512],
                                                                        rt2[:, :], ALU.add), reads=[B_x[j], B_rt2], writes=[B_x[j]])

            for idx in range(len(glist) + 1):
                if idx < len(glist):
                    if glist[idx][0] > 0:
                        flush(10 ** 9)
                    stage_A(idx)
                if idx >= 1:
                    stage_C(idx - 1)
                flush(3)
            flush(10 ** 9)
        for j in range(NSUB):
            dma("sp", out_h[t0 + j * 128:t0 + (j + 1) * 128, :], xres[:, j, :], reads=[B_x[j]])
        sc.barrier()

    sc.drain_dmas("sp")
    return nc, stack


def _noop():
    pass


_CACHE = {}


def make_in_maps(inputs):
    cst = host_consts()
    f = lambda a: np.ascontiguousarray(np.asarray(a, dtype=np.float32))
    x = f(inputs["x"]); c = f(inputs["c"])
    shared = {
        "w_ada": f(inputs["w_ada"][0]), "b_ada": f(inputs["b_ada"][0]).reshape(1, -1),
        "g1": f(inputs["g_norm1"][0]).reshape(1, -1), "g2": f(inputs["g_norm2"][0]).reshape(1, -1),
        "w_in": f(inputs["w_in"][0]), "gq": f(inputs["g_q"][0]).reshape(128, 1), "gk": f(inputs["g_k"][0]).reshape(128, 1),
        "rel_bias": f(inputs["rel_bias"]), "pool_w": f(inputs["pool_w"][0]),
        "pscale": np.ascontiguousarray(f(inputs["pool_scale"][0]).reshape(8, 128).T),
        "w_attn_br": f(inputs["w_attn_br"][0]), "w_pool_br": f(inputs["w_pool_br"][0]),
        "w_out": f(inputs["w_out"][0]), "w_peer_q": f(inputs["w_peer_q"][0]),
        "peer_keys": f(inputs["peer_keys"][0]).reshape(16, 128, 128),
        "peer_u": f(inputs["peer_u"][0]), "peer_v": f(inputs["peer_v"][0]),
    }
    shared.update(cst)
    maps = []
    for b in range(x.shape[0]):
        m = dict(shared)
        m["x"] = x[b]
        m["cT"] = np.ascontiguousarray(c[b].reshape(16, 128).T)
        maps.append(m)
    return maps


def kernel(**inputs):
    nc, stack = build()
    maps = make_in_maps(inputs)
    res = run_bass_kernel_spmd(nc, maps, core_ids=list(range(8)))
    out = np.stack([np.asarray(r["out"], dtype=np.float32) for r in res.results], axis=0)
    return out
```
